# Optimizing a Trainium2 kernel written in Bass

```python
import math
import jax
import jax.numpy as jnp
from jax import lax
import numpy as np

D_MODEL = 2048
BATCH = 2
SEQ = 8192
DEPTH = 2

N_META = 16
CHUNK = 64
CONV_K = 4
MIX_WIDTH = D_MODEL

S5_WIDTH = D_MODEL // 2
S5_GROUP = 16
S5_GROUPS = S5_WIDTH // S5_GROUP
S5_STATE = 64

GLA_HEADS = 4
GLA_DV = (D_MODEL // 2) // GLA_HEADS
GLA_DK = GLA_DV // 2
GLA_RANK = 16
GLA_TAU = 16.0

GDN_HEADS = 8
GDN_DK = (D_MODEL // 2) // GDN_HEADS
GDN_DV = GDN_DK

LRU_WIDTH = D_MODEL // 2
LRU_BLOCKS = 8
LRU_BW = LRU_WIDTH // LRU_BLOCKS
LRU_C = 8.0

MOE_GROUPS = 4
MOE_EPG = 8
MOE_TOPK = 2
MOE_FF = D_MODEL // 8

AB_SPLITS = (S5_WIDTH, GLA_HEADS * GLA_DK, GLA_HEADS * GLA_DK, GLA_HEADS * GLA_DV, GLA_RANK, GLA_HEADS * GLA_DV)
AB_IN = sum(AB_SPLITS)
CD_SPLITS = (2 * GDN_HEADS * GDN_DK + GDN_HEADS * GDN_DV, GDN_HEADS * GDN_DV, GDN_HEADS, GDN_HEADS, LRU_WIDTH, LRU_WIDTH)
CD_IN = sum(CD_SPLITS)

N_AB = (DEPTH + 1) // 2
N_CD = DEPTH // 2
DN_ALPHA = (2.0 * DEPTH) ** 0.25
DN_BETA = (8.0 * DEPTH) ** -0.25
LN_EPS = 1e-5
NORM_EPS = 1e-6

kernel_name = 'hybrid_s5_gla_gdn_rglru_hmoe'


def split_cols(h, sizes):
    offs = [int(s) for s in np.cumsum(sizes)[:-1]]
    return jnp.split(h, offs, axis=-1)


def layer_norm(x, g, b):
    xf = x.astype(jnp.float32)
    mu = jnp.mean(xf, axis=-1, keepdims=True)
    xc = xf - mu
    var = jnp.mean(xc * xc, axis=-1, keepdims=True)
    y = xc * lax.rsqrt(var + LN_EPS) * g.astype(jnp.float32) + b.astype(jnp.float32)
    return y.astype(x.dtype)


def rms_norm_heads(o, g):
    return o * lax.rsqrt(jnp.mean(o * o, axis=-1, keepdims=True) + NORM_EPS) * g.astype(jnp.float32)


def l2_normalize(t):
    return t * lax.rsqrt(jnp.sum(t * t, axis=-1, keepdims=True) + NORM_EPS)


def causal_conv(x, w, b=None):
    L = x.shape[1]
    xp = jnp.pad(x, ((0, 0), (CONV_K - 1, 0), (0, 0)))
    y = xp[:, 0:L] * w[0]
    for j in range(1, CONV_K):
        y = y + xp[:, j:j + L] * w[j]
    if b is not None:
        y = y + b
    return y


def linear_combine(left, right):
    a_l, b_l = left
    a_r, b_r = right
    return a_r * a_l, a_r * b_l + b_r


def to_chunks(t):
    pad = CHUNK - N_META
    t = jnp.pad(t.astype(jnp.float32), [(0, 0), (pad, 0)] + [(0, 0)] * (t.ndim - 2))
    bsz, lp = t.shape[:2]
    t = t.reshape((bsz, lp // CHUNK, CHUNK) + t.shape[2:])
    return jnp.moveaxis(t, 3, 1)


def from_chunks(t):
    bsz, nh, nc, c = t.shape[:4]
    t = jnp.moveaxis(t, 1, 3).reshape((bsz, nc * c, nh) + t.shape[4:])
    return t[:, CHUNK - N_META:]


def s5_mixer(u, a_re, a_im, log_dt, b_re, b_im, c_re, c_im, d, w_glu, b_glu):
    f32 = jnp.float32
    bsz, L, _ = u.shape
    uf = u.astype(f32).reshape(bsz, L, S5_GROUPS, S5_GROUP)
    a = lax.complex(a_re.astype(f32), a_im.astype(f32))
    dt = jnp.exp(log_dt.astype(f32))[:, None]
    a_bar = jnp.exp(a * dt)
    zoh = (a_bar - 1.0) / a
    bu = lax.complex(jnp.einsum('blgc,gpc->blgp', uf, b_re.astype(f32)),
                     jnp.einsum('blgc,gpc->blgp', uf, b_im.astype(f32))) * zoh
    a_seq = jnp.broadcast_to(a_bar, bu.shape)
    _, state = lax.associative_scan(linear_combine, (a_seq, bu), axis=1)
    y = (jnp.einsum('blgp,gcp->blgc', state.real, c_re.astype(f32))
         - jnp.einsum('blgp,gcp->blgc', state.imag, c_im.astype(f32))
         + d.astype(f32).reshape(S5_GROUPS, S5_GROUP) * uf)
    z = jax.nn.gelu(y.reshape(bsz, L, S5_WIDTH))
    out = z * jax.nn.sigmoid(z @ w_glu.astype(f32) + b_glu.astype(f32))
    return out.astype(u.dtype)


def gla_chunked(q, k, v, g):
    q, k, v, g = to_chunks(q), to_chunks(k), to_chunks(v), to_chunks(g)
    bsz, nh, _, _, dk = q.shape
    dv = v.shape[-1]
    bcum = jnp.cumsum(g, axis=3)
    b_last = bcum[:, :, :, -1:, :]
    q_in = q * jnp.exp(bcum)
    k_in = k * jnp.exp(-bcum)
    k_st = k * jnp.exp(b_last - bcum)
    causal = jnp.tril(jnp.ones((CHUNK, CHUNK), dtype=bool))
    att = jnp.where(causal, jnp.einsum('bhncd,bhnsd->bhncs', q_in, k_in), 0.0)
    o_intra = jnp.einsum('bhncs,bhnsv->bhncv', att, v)

    def step(s, inp):
        q_n, k_n, v_n, dec_n = inp
        o_n = jnp.einsum('bhcd,bhdv->bhcv', q_n, s)
        s = s * dec_n[..., None] + jnp.einsum('bhcd,bhcv->bhdv', k_n, v_n)
        return s, o_n

    s0 = jnp.zeros((bsz, nh, dk, dv), jnp.float32)
    xs = (jnp.moveaxis(q_in, 2, 0), jnp.moveaxis(k_st, 2, 0), jnp.moveaxis(v, 2, 0),
          jnp.moveaxis(jnp.exp(b_last[:, :, :, 0]), 2, 0))
    _, o_inter = lax.scan(step, s0, xs)
    return from_chunks(o_intra + jnp.moveaxis(o_inter, 0, 2))


def gla_mixer(q, k, v, g_low, r, w_gate, b_gate, norm_g):
    bsz, L, _ = q.shape
    logf = jax.nn.log_sigmoid((g_low @ w_gate).astype(jnp.float32) + b_gate.astype(jnp.float32)) / GLA_TAU
    qh = q.astype(jnp.float32).reshape(bsz, L, GLA_HEADS, GLA_DK) * (GLA_DK ** -0.5)
    kh = k.reshape(bsz, L, GLA_HEADS, GLA_DK)
    vh = v.reshape(bsz, L, GLA_HEADS, GLA_DV)
    o = gla_chunked(qh, kh, vh, logf.reshape(bsz, L, GLA_HEADS, GLA_DK))
    o = rms_norm_heads(o, norm_g) * jax.nn.silu(r.astype(jnp.float32).reshape(bsz, L, GLA_HEADS, GLA_DV))
    return o.reshape(bsz, L, GLA_HEADS * GLA_DV).astype(q.dtype)


def gdn_chunked(q, k, v, beta, g):
    q, k, v, beta, g = to_chunks(q), to_chunks(k), to_chunks(v), to_chunks(beta), to_chunks(g)
    bsz, nh, _, _, dk = q.shape
    dv = v.shape[-1]
    gc = jnp.cumsum(g, axis=-1)
    tril = jnp.tril(jnp.ones((CHUNK, CHUNK), dtype=bool))
    strict = jnp.tril(jnp.ones((CHUNK, CHUNK), dtype=bool), -1)
    gam = jnp.exp(jnp.where(tril, gc[..., :, None] - gc[..., None, :], -jnp.inf))
    kb = k * beta[..., None]
    m = jnp.where(strict, jnp.einsum('bhncd,bhnsd->bhncs', kb, k) * gam, 0.0)
    eye = jnp.eye(CHUNK, dtype=jnp.float32)
    rhs = jnp.concatenate([v * beta[..., None], kb * jnp.exp(gc)[..., None]], axis=-1)
    sol = lax.linalg.triangular_solve(eye + m, rhs, left_side=True, lower=True, unit_diagonal=True)
    u_c, w_c = sol[..., :dv], sol[..., dv:]
    a_qk = jnp.einsum('bhncd,bhnsd->bhncs', q, k) * gam
    q_dec = q * jnp.exp(gc)[..., None]
    g_last = gc[..., -1]
    k_st = k * jnp.exp(g_last[..., None] - gc)[..., None]

    def step(s, inp):
        q_n, k_n, u_n, w_n, a_n, dec_n = inp
        v_new = u_n - jnp.einsum('bhcd,bhdv->bhcv', w_n, s)
        o_n = jnp.einsum('bhcd,bhdv->bhcv', q_n, s) + jnp.einsum('bhcs,bhsv->bhcv', a_n, v_new)
        s = s * dec_n[..., None, None] + jnp.einsum('bhcd,bhcv->bhdv', k_n, v_new)
        return s, o_n

    s0 = jnp.zeros((bsz, nh, dk, dv), jnp.float32)
    xs = tuple(jnp.moveaxis(t, 2, 0) for t in (q_dec, k_st, u_c, w_c, a_qk, jnp.exp(g_last)))
    _, o = lax.scan(step, s0, xs)
    return from_chunks(jnp.moveaxis(o, 0, 2))


def gated_deltanet(qkv, z, beta_logit, a_logit, conv_w, a_log, dt_bias, norm_g):
    f32 = jnp.float32
    bsz, L, _ = qkv.shape
    qkv = jax.nn.silu(causal_conv(qkv, conv_w).astype(f32))
    q, k, v = split_cols(qkv, (GDN_HEADS * GDN_DK, GDN_HEADS * GDN_DK, GDN_HEADS * GDN_DV))
    q = l2_normalize(q.reshape(bsz, L, GDN_HEADS, GDN_DK)) * (GDN_DK ** -0.5)
    k = l2_normalize(k.reshape(bsz, L, GDN_HEADS, GDN_DK))
    v = v.reshape(bsz, L, GDN_HEADS, GDN_DV)
    beta = jax.nn.sigmoid(beta_logit.astype(f32))
    g = -jnp.exp(a_log.astype(f32)) * jax.nn.softplus(a_logit.astype(f32) + dt_bias.astype(f32))
    o = gdn_chunked(q, k, v, beta, g)
    o = rms_norm_heads(o, norm_g) * jax.nn.silu(z.astype(f32).reshape(bsz, L, GDN_HEADS, GDN_DV))
    return o.reshape(bsz, L, GDN_HEADS * GDN_DV).astype(z.dtype)


def rglru_mixer(xb, gate, conv_w, conv_b, w_a, b_a, w_x, b_x, lam):
    f32 = jnp.float32
    bsz, L, _ = xb.shape
    xc = causal_conv(xb, conv_w, conv_b).astype(f32).reshape(bsz, L, LRU_BLOCKS, LRU_BW)
    r = jax.nn.sigmoid(jnp.einsum('blhi,hij->blhj', xc, w_a.astype(f32)) + b_a.astype(f32).reshape(LRU_BLOCKS, LRU_BW))
    i = jax.nn.sigmoid(jnp.einsum('blhi,hij->blhj', xc, w_x.astype(f32)) + b_x.astype(f32).reshape(LRU_BLOCKS, LRU_BW))
    log_a = -LRU_C * jax.nn.softplus(-lam.astype(f32)).reshape(LRU_BLOCKS, LRU_BW) * r
    a = jnp.exp(log_a)
    bx = jnp.sqrt(-jnp.expm1(2.0 * log_a)) * (i * xc)
    _, hs = lax.associative_scan(linear_combine, (a, bx), axis=1)
    y = hs.reshape(bsz, L, LRU_WIDTH) * jax.nn.gelu(gate.astype(f32))
    return y.astype(xb.dtype)


def mixer_ab(x, w_in, a_re, a_im, log_dt, b_re, b_im, c_re, c_im, d, w_glu, b_glu,
             gla_w_gate, gla_b_gate, gla_norm, w_out):
    h = x @ w_in
    u, q, k, v, g_low, r = split_cols(h, AB_SPLITS)
    y_s5 = s5_mixer(u, a_re, a_im, log_dt, b_re, b_im, c_re, c_im, d, w_glu, b_glu)
    y_gla = gla_mixer(q, k, v, g_low, r, gla_w_gate, gla_b_gate, gla_norm)
    return jnp.concatenate([y_s5, y_gla], axis=-1) @ w_out


def mixer_cd(x, w_in, conv_w, a_log, dt_bias, gdn_norm, lru_conv_w, lru_conv_b,
             lru_w_a, lru_b_a, lru_w_x, lru_b_x, lru_lambda, w_out):
    h = x @ w_in
    qkv, z, beta_logit, a_logit, lru_x, lru_gate = split_cols(h, CD_SPLITS)
    y_gdn = gated_deltanet(qkv, z, beta_logit, a_logit, conv_w, a_log, dt_bias, gdn_norm)
    y_lru = rglru_mixer(lru_x, lru_gate, lru_conv_w, lru_conv_b, lru_w_a, lru_b_a, lru_w_x, lru_b_x, lru_lambda)
    return jnp.concatenate([y_gdn, y_lru], axis=-1) @ w_out


def hier_moe(x, w_rg, b_rg, w_re, b_re, w_gate, w_up, w_down):
    f32 = jnp.float32
    bsz, L, dm = x.shape
    xt = x.reshape(bsz * L, dm)
    lg = (xt @ w_rg).astype(f32) + b_rg.astype(f32)
    pg = jax.nn.softmax(lg, axis=-1)
    p_top, g_top = lax.top_k(pg, 1)
    le = jnp.einsum('td,dge->tge', xt, w_re).astype(f32) + b_re.astype(f32)
    le_sel = jnp.take_along_axis(le, g_top[:, :, None], axis=1)[:, 0]
    v_top, e_top = lax.top_k(le_sel, MOE_TOPK)
    w_tok = p_top * jax.nn.softmax(v_top, axis=-1)
    comb_e = jnp.einsum('tk,tke->te', w_tok, jax.nn.one_hot(e_top, MOE_EPG, dtype=f32))
    comb = (jax.nn.one_hot(g_top[:, 0], MOE_GROUPS, dtype=f32)[:, :, None] * comb_e[:, None, :]).astype(x.dtype)
    out = jnp.zeros_like(xt)
    for gi in range(MOE_GROUPS):
        hg = jnp.einsum('td,edf->tef', xt, w_gate[gi])
        hu = jnp.einsum('td,edf->tef', xt, w_up[gi])
        hh = jax.nn.silu(hg) * hu * comb[:, gi, :, None]
        out = out + jnp.einsum('tef,efd->td', hh, w_down[gi])
    return out.reshape(bsz, L, dm)


def setup_inputs(seed: int = 0) -> dict:
    key = jax.random.key(seed)
    ks = iter(jax.random.split(key, 64))
    f32 = jnp.float32

    def nrm(shape, scale):
        return jax.random.normal(next(ks), shape, f32) * scale

    def unif(shape, lo, hi):
        return jax.random.uniform(next(ks), shape, f32, lo, hi)

    x = nrm((BATCH, SEQ, D_MODEL), 1.0)
    meta_tokens = nrm((N_META, D_MODEL), 1.0)
    ab_w_in = nrm((N_AB, D_MODEL, AB_IN), D_MODEL ** -0.5)
    ab_s5_a_re = -0.5 * jnp.exp(nrm((N_AB, S5_GROUPS, S5_STATE), 0.05))
    ab_s5_a_im = math.pi * jnp.arange(S5_STATE, dtype=f32) + nrm((N_AB, S5_GROUPS, S5_STATE), 0.01)
    ab_s5_log_dt = unif((N_AB, S5_GROUPS), math.log(1e-3), math.log(1e-1))
    ab_s5_b_re = nrm((N_AB, S5_GROUPS, S5_STATE, S5_GROUP), (2 * S5_GROUP) ** -0.5)
    ab_s5_b_im = nrm((N_AB, S5_GROUPS, S5_STATE, S5_GROUP), (2 * S5_GROUP) ** -0.5)
    ab_s5_c_re = nrm((N_AB, S5_GROUPS, S5_GROUP, S5_STATE), S5_STATE ** -0.5)
    ab_s5_c_im = nrm((N_AB, S5_GROUPS, S5_GROUP, S5_STATE), S5_STATE ** -0.5)
    ab_s5_d = nrm((N_AB, S5_WIDTH), 1.0)
    ab_s5_w_glu = nrm((N_AB, S5_WIDTH, S5_WIDTH), S5_WIDTH ** -0.5)
    ab_s5_b_glu = nrm((N_AB, S5_WIDTH), 0.01)
    ab_gla_w_gate = nrm((N_AB, GLA_RANK, GLA_HEADS * GLA_DK), GLA_RANK ** -0.5)
    ab_gla_b_gate = nrm((N_AB, GLA_HEADS * GLA_DK), 0.01)
    ab_gla_norm = 1.0 + nrm((N_AB, GLA_DV), 0.02)
    ab_w_out = nrm((N_AB, MIX_WIDTH, D_MODEL), (MIX_WIDTH ** -0.5) * DN_BETA)
    cd_w_in = nrm((N_CD, D_MODEL, CD_IN), D_MODEL ** -0.5)
    cd_conv_w = nrm((N_CD, CONV_K, CD_SPLITS[0]), CONV_K ** -0.5)
    cd_gdn_a_log = jnp.log(unif((N_CD, GDN_HEADS), 1.0, 16.0))
    dt0 = jnp.exp(unif((N_CD, GDN_HEADS), math.log(1e-3), math.log(1e-1)))
    cd_gdn_dt_bias = dt0 + jnp.log(-jnp.expm1(-dt0))
    cd_gdn_norm = 1.0 + nrm((N_CD, GDN_DV), 0.02)
    cd_lru_conv_w = nrm((N_CD, CONV_K, LRU_WIDTH), CONV_K ** -0.5)
    cd_lru_conv_b = nrm((N_CD, LRU_WIDTH), 0.01)
    cd_lru_w_a = nrm((N_CD, LRU_BLOCKS, LRU_BW, LRU_BW), LRU_BW ** -0.5)
    cd_lru_b_a = nrm((N_CD, LRU_WIDTH), 0.01)
    cd_lru_w_x = nrm((N_CD, LRU_BLOCKS, LRU_BW, LRU_BW), LRU_BW ** -0.5)
    cd_lru_b_x = nrm((N_CD, LRU_WIDTH), 0.01)
    a_c = unif((N_CD, LRU_WIDTH), 0.9, 0.999) ** (1.0 / LRU_C)
    cd_lru_lambda = jnp.log(a_c) - jnp.log1p(-a_c)
    cd_w_out = nrm((N_CD, MIX_WIDTH, D_MODEL), (MIX_WIDTH ** -0.5) * DN_BETA)
    moe_w_router_g = nrm((DEPTH, D_MODEL, MOE_GROUPS), D_MODEL ** -0.5)
    moe_b_router_g = nrm((DEPTH, MOE_GROUPS), 0.01)
    moe_w_router_e = nrm((DEPTH, D_MODEL, MOE_GROUPS, MOE_EPG), D_MODEL ** -0.5)
    moe_b_router_e = nrm((DEPTH, MOE_GROUPS, MOE_EPG), 0.01)
    moe_w_gate = nrm((DEPTH, MOE_GROUPS, MOE_EPG, D_MODEL, MOE_FF), D_MODEL ** -0.5)
    moe_w_up = nrm((DEPTH, MOE_GROUPS, MOE_EPG, D_MODEL, MOE_FF), D_MODEL ** -0.5)
    moe_w_down = nrm((DEPTH, MOE_GROUPS, MOE_EPG, MOE_FF, D_MODEL), (MOE_FF ** -0.5) * DN_BETA)
    ln_mix_g = 1.0 + nrm((DEPTH, D_MODEL), 0.02)
    ln_mix_b = nrm((DEPTH, D_MODEL), 0.01)
    ln_ffn_g = 1.0 + nrm((DEPTH, D_MODEL), 0.02)
    ln_ffn_b = nrm((DEPTH, D_MODEL), 0.01)
    return {'x': x, 'meta_tokens': meta_tokens, 'ab_w_in': ab_w_in,
            'ab_s5_a_re': ab_s5_a_re, 'ab_s5_a_im': ab_s5_a_im, 'ab_s5_log_dt': ab_s5_log_dt,
            'ab_s5_b_re': ab_s5_b_re, 'ab_s5_b_im': ab_s5_b_im, 'ab_s5_c_re': ab_s5_c_re,
            'ab_s5_c_im': ab_s5_c_im, 'ab_s5_d': ab_s5_d, 'ab_s5_w_glu': ab_s5_w_glu,
            'ab_s5_b_glu': ab_s5_b_glu, 'ab_gla_w_gate': ab_gla_w_gate, 'ab_gla_b_gate': ab_gla_b_gate,
            'ab_gla_norm': ab_gla_norm, 'ab_w_out': ab_w_out, 'cd_w_in': cd_w_in,
            'cd_conv_w': cd_conv_w, 'cd_gdn_a_log': cd_gdn_a_log, 'cd_gdn_dt_bias': cd_gdn_dt_bias,
            'cd_gdn_norm': cd_gdn_norm, 'cd_lru_conv_w': cd_lru_conv_w, 'cd_lru_conv_b': cd_lru_conv_b,
            'cd_lru_w_a': cd_lru_w_a, 'cd_lru_b_a': cd_lru_b_a, 'cd_lru_w_x': cd_lru_w_x,
            'cd_lru_b_x': cd_lru_b_x, 'cd_lru_lambda': cd_lru_lambda, 'cd_w_out': cd_w_out,
            'moe_w_router_g': moe_w_router_g, 'moe_b_router_g': moe_b_router_g,
            'moe_w_router_e': moe_w_router_e, 'moe_b_router_e': moe_b_router_e,
            'moe_w_gate': moe_w_gate, 'moe_w_up': moe_w_up, 'moe_w_down': moe_w_down,
            'ln_mix_g': ln_mix_g, 'ln_mix_b': ln_mix_b, 'ln_ffn_g': ln_ffn_g, 'ln_ffn_b': ln_ffn_b}


def reference(x, meta_tokens, ab_w_in, ab_s5_a_re, ab_s5_a_im, ab_s5_log_dt, ab_s5_b_re, ab_s5_b_im,
              ab_s5_c_re, ab_s5_c_im, ab_s5_d, ab_s5_w_glu, ab_s5_b_glu, ab_gla_w_gate, ab_gla_b_gate,
              ab_gla_norm, ab_w_out, cd_w_in, cd_conv_w, cd_gdn_a_log, cd_gdn_dt_bias, cd_gdn_norm,
              cd_lru_conv_w, cd_lru_conv_b, cd_lru_w_a, cd_lru_b_a, cd_lru_w_x, cd_lru_b_x, cd_lru_lambda,
              cd_w_out, moe_w_router_g, moe_b_router_g, moe_w_router_e, moe_b_router_e, moe_w_gate,
              moe_w_up, moe_w_down, ln_mix_g, ln_mix_b, ln_ffn_g, ln_ffn_b):
    bsz = x.shape[0]
    meta = jnp.broadcast_to(meta_tokens.astype(x.dtype)[None], (bsz, N_META, D_MODEL))
    h = jnp.concatenate([meta, x], axis=1)
    for layer in range(DEPTH):
        j = layer // 2
        if layer % 2 == 0:
            mix = mixer_ab(h, ab_w_in[j], ab_s5_a_re[j], ab_s5_a_im[j], ab_s5_log_dt[j], ab_s5_b_re[j],
                           ab_s5_b_im[j], ab_s5_c_re[j], ab_s5_c_im[j], ab_s5_d[j], ab_s5_w_glu[j],
                           ab_s5_b_glu[j], ab_gla_w_gate[j], ab_gla_b_gate[j], ab_gla_norm[j], ab_w_out[j])
        else:
            mix = mixer_cd(h, cd_w_in[j], cd_conv_w[j], cd_gdn_a_log[j], cd_gdn_dt_bias[j], cd_gdn_norm[j],
                           cd_lru_conv_w[j], cd_lru_conv_b[j], cd_lru_w_a[j], cd_lru_b_a[j], cd_lru_w_x[j],
                           cd_lru_b_x[j], cd_lru_lambda[j], cd_w_out[j])
        h = layer_norm(DN_ALPHA * h + mix, ln_mix_g[layer], ln_mix_b[layer])
        ffn = hier_moe(h, moe_w_router_g[layer], moe_b_router_g[layer], moe_w_router_e[layer],
                       moe_b_router_e[layer], moe_w_gate[layer], moe_w_up[layer], moe_w_down[layer])
        h = layer_norm(DN_ALPHA * h + ffn, ln_ffn_g[layer], ln_ffn_b[layer])
    return h[:, N_META:]
```

```python
import numpy as np
import os
from contextlib import ExitStack
import concourse.bass as bass
import concourse.mybir as mybir
from concourse.bass_utils import run_bass_kernel_spmd

F32 = mybir.dt.float32
BF16 = mybir.dt.bfloat16
ALU = mybir.AluOpType
AF = mybir.ActivationFunctionType
AX = mybir.AxisListType

ENGS = ['pe', 'act', 'dve', 'pool', 'sp']
N_DMA_SEMS = 12
SAME_ENGINE_SYNC = True


class Prog:
    def __init__(self):
        self.nc = bass.Bass("TRN2", target_bir_lowering=False)
        self.es = ExitStack()
        self.ops = {e: [] for e in ENGS}
        self.cnt = {e: 0 for e in ENGS}
        self.seen = {e: {} for e in ENGS}
        self.last_w = {}
        self.readers = {}
        self.dma_cnt = [0] * N_DMA_SEMS
        self.dma_rr = 0
        self.sems = {}
        self.n_ops = 0
        self.phase = 0
        self.barrier_toks = {e: [] for e in ENGS}
        self.scopes = []
        self.cc_cnt = 0
        self._alloc_sems()

    def _alloc_sems(self):
        p = self.phase
        for e in ENGS:
            self.sems[(e, p)] = self.es.enter_context(self.nc.semaphore("s_%s_%d" % (e, p)))
        for j in range(N_DMA_SEMS):
            self.sems[(('dma', j), p)] = self.es.enter_context(self.nc.semaphore("s_dma%d_%d" % (j, p)))

    def new_phase(self):
        p = self.phase
        toks = [((e, p), self.cnt[e]) for e in ENGS if self.cnt[e] > 0]
        toks += [((('dma', j), p), self.dma_cnt[j]) for j in range(N_DMA_SEMS) if self.dma_cnt[j] > 0]
        if self.cc_cnt > 0:
            toks.append((('cc', 0), self.cc_cnt))
        for e in ENGS:
            self.barrier_toks[e] = self.barrier_toks[e] + toks
        self.phase += 1
        self._alloc_sems()
        self.cnt = {e: 0 for e in ENGS}
        self.dma_cnt = [0] * N_DMA_SEMS
        self.last_w = {}
        self.readers = {}

    def scope(self):
        prog = self

        class _S:
            def __enter__(self_):
                prog.scopes.append(ExitStack())

            def __exit__(self_, *a):
                prog.scopes.pop().close()
                return False
        return _S()

    def sb(self, name, shape, dt=F32):
        es = self.scopes[-1] if self.scopes else self.es
        return es.enter_context(self.nc.sbuf_tensor("p%d_%s" % (self.phase, name), list(shape), dt))

    def ps(self, name, shape, dt=F32):
        return self.es.enter_context(self.nc.psum_tensor(name, list(shape), dt))

    def dram(self, name, shape, dt=F32, kind="ExternalInput"):
        return self.nc.dram_tensor(name, list(shape), dt, kind=kind).ap()

    def tok(self, name, val):
        return ((name, self.phase), val)

    def _deps(self, R, W):
        toks = []
        for k in R:
            if k in self.last_w:
                toks.append(self.last_w[k])
        for k in W:
            if k in self.last_w:
                toks.append(self.last_w[k])
            for t in self.readers.get(k, {}).items():
                toks.append(t)
        return toks

    def _mark(self, tok, R, W):
        for k in R:
            self.readers.setdefault(k, {})[tok[0]] = tok[1]
        for k in W:
            self.last_w[k] = tok
            self.readers[k] = {}

    def _waits(self, eng, toks):
        need = {}
        for s, v in toks:
            if s[0] == eng and (eng in ('pe', 'sp') or not SAME_ENGINE_SYNC):
                continue
            if self.seen[eng].get(s, 0) >= v:
                continue
            need[s] = max(need.get(s, 0), v)
        for s, v in need.items():
            self.seen[eng][s] = v
        return list(need.items())

    @staticmethod
    def _excl(R, W):
        ps = [k for k in R if isinstance(k, str) and k.startswith('pb')]
        if ps:
            R = [k for k in R if k not in ps]
            W = list(W) + ps
        return R, W

    def _bar(self, eng):
        b = self.barrier_toks[eng]
        self.barrier_toks[eng] = []
        return b

    def op(self, eng, fn, R=(), W=()):
        R, W = self._excl(R, W)
        waits = self._waits(eng, self._deps(R, W) + self._bar(eng))
        self.cnt[eng] += 1
        tok = self.tok(eng, self.cnt[eng])
        self.ops[eng].append((waits, fn, ((eng, self.phase), 1)))
        self._mark(tok, R, W)
        self.n_ops += 1

    def dma(self, out, in_, R=(), W=(), q='sp', **kw):
        if q == 'pool':
            q = 'sp'
        j = self.dma_rr
        self.dma_rr = (self.dma_rr + 1) % N_DMA_SEMS
        toks = self._deps(R, W) + self._bar(q)
        if self.dma_cnt[j] > 0:
            toks.append(self.tok(('dma', j), self.dma_cnt[j]))
        waits = self._waits(q, toks)
        self.dma_cnt[j] += 16
        tok = self.tok(('dma', j), self.dma_cnt[j])
        self.ops[q].append((waits, lambda e: e.dma_start(out=out, in_=in_, **kw), ((('dma', j), self.phase), 16)))
        self._mark(tok, R, W)
        self.n_ops += 1

    def allgather(self, out, in_, groups, R=(), W=()):
        if ('cc', 0) not in self.sems:
            self.sems[('cc', 0)] = self.es.enter_context(self.nc.semaphore("s_cc"))
        toks = self._deps(R, W) + self._bar('pool')
        waits = self._waits('pool', toks)
        self.cc_cnt += 1
        tok = (('cc', 0), self.cc_cnt)
        self.ops['pool'].append((waits, lambda e: e.collective_compute(
            "AllGather", ALU.bypass, replica_groups=groups, ins=[in_], outs=[out]), (('cc', 0), None)))
        self._mark(tok, R, W)
        self.n_ops += 1

    def mm(self, out, lhsT, rhs, start=True, stop=True, R=(), W=()):
        self.op('pe', lambda e: e.matmul(out, lhsT, rhs, start=start, stop=stop), R, W)

    def transpose(self, out, in_, ident, R=(), W=()):
        self.op('pe', lambda e: e.transpose(out, in_, ident), R, W)

    def act(self, out, in_, func, bias=None, scale=1.0, R=(), W=(), accum_out=None):
        kw = {}
        if bias is not None:
            kw['bias'] = bias
        if accum_out is not None:
            kw['accum_out'] = accum_out
        self.op('act', lambda e: e.activation(out=out, in_=in_, func=func, scale=scale, **kw), R, W)

    def copy(self, eng, out, in_, R=(), W=()):
        if eng == 'act':
            self.op('act', lambda e: e.copy(out=out, in_=in_), R, W)
        else:
            self.op(eng, lambda e: e.tensor_copy(out=out, in_=in_), R, W)

    def tt(self, eng, out, a, b, op, R=(), W=()):
        self.op(eng, lambda e: e.tensor_tensor(out=out, in0=a, in1=b, op=op), R, W)

    def ts(self, eng, out, a, s1, op0, s2=None, op1=None, R=(), W=()):
        if op1 is None:
            self.op(eng, lambda e: e.tensor_scalar(out=out, in0=a, scalar1=s1, scalar2=None, op0=op0), R, W)
        else:
            self.op(eng, lambda e: e.tensor_scalar(out=out, in0=a, scalar1=s1, scalar2=s2, op0=op0, op1=op1), R, W)

    def stt(self, eng, out, a, s, b, op0, op1, R=(), W=()):
        eng = 'dve'
        self.op(eng, lambda e: e.scalar_tensor_tensor(out=out, in0=a, scalar=s, in1=b, op0=op0, op1=op1), R, W)

    def memset(self, eng, ap, val, W=()):
        self.op(eng, lambda e: e.memset(ap, val), (), W)

    def finalize(self):
        nc = self.nc
        fin = list(self.barrier_toks['sp'])
        for j in range(N_DMA_SEMS):
            if self.dma_cnt[j] > 0:
                fin.append(self.tok(('dma', j), self.dma_cnt[j]))
        for e in ENGS:
            if e != 'sp' and self.cnt[e] > 0:
                fin.append(self.tok(e, self.cnt[e]))
        if self.cc_cnt > 0:
            fin.append((('cc', 0), self.cc_cnt))
        ops = self.ops
        sems = self.sems

        def run(engobj, name):
            for waits, fn, (s, inc) in ops[name]:
                for ws, wv in waits:
                    engobj.wait_ge(sems[ws], wv)
                ins = fn(engobj)
                if inc is None:
                    ins.then_inc(sems[s])
                else:
                    ins.then_inc(sems[s], inc)
            if name == 'sp':
                for ws, wv in fin:
                    engobj.wait_ge(sems[ws], wv)

        with nc.Block() as block:
            @block.sync
            def _(e):
                run(e, 'sp')

            if ops['pe']:
                @block.tensor
                def _(e):
                    run(e, 'pe')
            if ops['act']:
                @block.scalar
                def _(e):
                    run(e, 'act')
            if ops['dve']:
                @block.vector
                def _(e):
                    run(e, 'dve')
            if ops['pool']:
                @block.gpsimd
                def _(e):
                    run(e, 'pool')
        self.es.close()
        return nc

import math

D = 2048
CH = 512
PAD = 496


def gelu_tanh(P, out, x, tA, tB, N, kx, kout, ktmp):
    P.tt('pool', tA[:, :N], x, x, ALU.mult, R=[kx], W=[ktmp[0]])
    P.ts('pool', tA[:, :N], tA[:, :N], 0.044715, ALU.mult, 1.0, ALU.add, R=[ktmp[0]], W=[ktmp[0]])
    P.tt('pool', tA[:, :N], tA[:, :N], x, ALU.mult, R=[ktmp[0], kx], W=[ktmp[0]])
    P.act(tB[:, :N], tA[:, :N], AF.Sigmoid, scale=1.5957691216057308, R=[ktmp[0]], W=[ktmp[1]])
    P.tt('pool', out, x, tB[:, :N], ALU.mult, R=[kx, ktmp[1]], W=[kout])


def inproj_setup(P, w_in, ncols, cast_engs=('act', 'pool')):
    wb = P.sb("win_b", [128, 16, ncols], BF16)
    stg = [P.sb("win_st%d" % i, [128, ncols]) for i in range(2)]
    for dt in range(16):
        i = dt % 2
        P.dma(stg[i][:], w_in[dt * 128:(dt + 1) * 128, :], W=[('win_st', i)])
        P.copy(cast_engs[dt % 2], wb[:, dt, :], stg[i][:], R=[('win_st', i)], W=['win_b'])
    return wb


def load_x_chunk(P, hT, c, xst, xb):
    for dt in range(16):
        i = dt % 4
        P.dma(xst[i][:], hT[dt * 128:(dt + 1) * 128, c * CH:(c + 1) * CH], W=[('xst', i)], q='sp')
        P.copy(('act', 'pool')[dt % 2], xb[:, dt, :], xst[i][:], R=[('xst', i)], W=[('xb', dt)])


def proj_tile(P, wb, col0, M, xb, ps, pskey):
    for dt in range(16):
        P.mm(ps[:M, :CH], wb[:, dt, col0:col0 + M], xb[:, dt, :], start=(dt == 0), stop=(dt == 15),
             R=['win_b', ('xb', dt)], W=[pskey])


def phase_ab(P, pb, pk, NCHUNK, hT, y_loc, y_all, groups, pre="a_"):
    Tp = NCHUNK * CH
    NCOL = 1040
    w_in = P.dram(pre + "w_in", [D, NCOL])
    s5par = P.dram(pre + "s5par", [128, 8, 3])
    s5BT = P.dram(pre + "s5BT", [128, 2, 8, 128])
    s5CT = P.dram(pre + "s5CT", [128, 2, 8, 128])
    s5d = P.dram(pre + "s5d", [128, 2])
    gla_wg = P.dram(pre + "gla_wg", [16, 128]); gla_bg = P.dram(pre + "gla_bg", [128, 1]); gla_ng = P.dram(pre + "gla_ng", [128, 256])
    c_U = P.dram(pre + "c_U", [128, 128]); c_ident = P.dram(pre + "c_ident", [128, 128])
    ykeys = []
    wb = inproj_setup(P, w_in, NCOL)
    xst = [P.sb("xst%d" % i, [128, CH]) for i in range(4)]
    xb = P.sb("xb", [128, 16, CH], BF16)
    names = ['u0', 'u1', 'q', 'k', 'v0', 'v1', 'r0', 'r1']
    pt = {n: P.sb("pt_" + n, [128, CH]) for n in names}
    glT = P.sb("glT", [16, CH])
    U = P.sb("U", [128, 128]); ident = P.sb("ident", [128, 128]); identb = None
    onescol = P.sb("onescol", [128, 1])
    P.dma(U[:], c_U, W=['U']); P.dma(ident[:], c_ident, W=['ident'])
    P.memset('dve', onescol[:], 1.0, W=['onescol'])

    par = P.sb("s5par_sb", [128, 8, 3]); P.dma(par[:], s5par, W=['par'])
    BTb = P.sb("BTb", [128, 2, 8, 128], BF16); CTb = P.sb("CTb", [128, 2, 8, 128], BF16)
    stg128 = [P.sb("stg128_%d" % i, [128, 128]) for i in range(4)]
    dcol = P.sb("dcol", [128, 2]); P.dma(dcol[:], s5d, W=['dcol'])
    Er = P.sb("Er", [128, 8, CH]); Ei = P.sb("Ei", [128, 8, CH])
    sc = {k: P.sb("sc_" + k, [128, 8]) for k in
          ['dt', 'th', 'r', 'c', 's', 'c2', 's2', 't', 'cr', 'ci', 'nr', 'ni', 'den', 'zr', 'zi', 'x', 'y']}
    SK = lambda *n: ['sc_' + a for a in n]
    halfpi = P.sb("halfpi", [128, 1]); P.memset('dve', halfpi[:], math.pi / 2, W=['halfpi'])
    P.act(sc['dt'][:], par[:, :, 2], AF.Exp, R=['par'], W=SK('dt'))
    P.tt('dve', sc['th'][:], par[:, :, 1], sc['dt'][:], ALU.mult, R=['par'] + SK('dt'), W=SK('th'))
    P.tt('dve', sc['r'][:], par[:, :, 0], sc['dt'][:], ALU.mult, R=['par'] + SK('dt'), W=SK('r'))
    P.act(sc['r'][:], sc['r'][:], AF.Exp, R=SK('r'), W=SK('r'))
    P.act(sc['s'][:], sc['th'][:], AF.Sin, scale=1.0 / 16, R=SK('th'), W=SK('s'))
    P.act(sc['c'][:], sc['th'][:], AF.Sin, scale=1.0 / 16, bias=halfpi[:], R=SK('th') + ['halfpi'], W=SK('c'))
    for _ in range(4):
        P.tt('dve', sc['c2'][:], sc['c'][:], sc['c'][:], ALU.mult, R=SK('c'), W=SK('c2'))
        P.tt('dve', sc['s2'][:], sc['s'][:], sc['s'][:], ALU.mult, R=SK('s'), W=SK('s2'))
        P.tt('dve', sc['t'][:], sc['c'][:], sc['s'][:], ALU.mult, R=SK('c', 's'), W=SK('t'))
        P.tt('dve', sc['c'][:], sc['c2'][:], sc['s2'][:], ALU.subtract, R=SK('c2', 's2'), W=SK('c'))
        P.ts('dve', sc['s'][:], sc['t'][:], 2.0, ALU.mult, R=SK('t'), W=SK('s'))
    P.tt('dve', sc['nr'][:], sc['r'][:], sc['c'][:], ALU.mult, R=SK('r', 'c'), W=SK('nr'))
    P.ts('dve', sc['nr'][:], sc['nr'][:], -1.0, ALU.add, R=SK('nr'), W=SK('nr'))
    P.tt('dve', sc['ni'][:], sc['r'][:], sc['s'][:], ALU.mult, R=SK('r', 's'), W=SK('ni'))
    P.tt('dve', sc['den'][:], par[:, :, 0], par[:, :, 0], ALU.mult, R=['par'], W=SK('den'))
    P.tt('dve', sc['x'][:], par[:, :, 1], par[:, :, 1], ALU.mult, R=['par'], W=SK('x'))
    P.tt('dve', sc['den'][:], sc['den'][:], sc['x'][:], ALU.add, R=SK('den', 'x'), W=SK('den'))
    P.op('dve', lambda e: e.reciprocal(out=sc['den'][:], in_=sc['den'][:]), R=SK('den'), W=SK('den'))
    P.tt('dve', sc['x'][:], sc['nr'][:], par[:, :, 0], ALU.mult, R=SK('nr') + ['par'], W=SK('x'))
    P.tt('dve', sc['y'][:], sc['ni'][:], par[:, :, 1], ALU.mult, R=SK('ni') + ['par'], W=SK('y'))
    P.tt('dve', sc['zr'][:], sc['x'][:], sc['y'][:], ALU.add, R=SK('x', 'y'), W=SK('zr'))
    P.tt('dve', sc['zr'][:], sc['zr'][:], sc['den'][:], ALU.mult, R=SK('zr', 'den'), W=SK('zr'))
    P.tt('dve', sc['x'][:], sc['ni'][:], par[:, :, 0], ALU.mult, R=SK('ni') + ['par'], W=SK('x'))
    P.tt('dve', sc['y'][:], sc['nr'][:], par[:, :, 1], ALU.mult, R=SK('nr') + ['par'], W=SK('y'))
    P.tt('dve', sc['zi'][:], sc['x'][:], sc['y'][:], ALU.subtract, R=SK('x', 'y'), W=SK('zi'))
    P.tt('dve', sc['zi'][:], sc['zi'][:], sc['den'][:], ALU.mult, R=SK('zi', 'den'), W=SK('zi'))
    tmpc = P.sb("tmpc", [128, 256])
    for st in range(8):
        c1 = sc['c'][:, st:st + 1]; s1 = sc['s'][:, st:st + 1]
        eng = 'dve' if st % 2 == 0 else 'pool'
        P.copy(eng, Er[:, st, 0:1], c1, R=SK('c'), W=['Er']); P.copy(eng, Ei[:, st, 0:1], s1, R=SK('s'), W=['Ei'])
        m = 1
        while m < CH:
            ar = Er[:, st, m - 1:m]; ai = Ei[:, st, m - 1:m]
            P.ts(eng, tmpc[:, :m], Ei[:, st, 0:m], ai, ALU.mult, R=['Ei'], W=['tmpc'])
            P.stt(eng, Er[:, st, m:2 * m], Er[:, st, 0:m], ar, tmpc[:, :m], ALU.mult, ALU.subtract, R=['Er', 'tmpc'], W=['Er'])
            P.ts(eng, tmpc[:, :m], Ei[:, st, 0:m], ar, ALU.mult, R=['Ei', 'Er'], W=['tmpc'])
            P.stt(eng, Ei[:, st, m:2 * m], Er[:, st, 0:m], ai, tmpc[:, :m], ALU.mult, ALU.add, R=['Er', 'tmpc', 'Ei'], W=['Ei'])
            m *= 2
        zr = sc['zr'][:, st:st + 1]; zi = sc['zi'][:, st:st + 1]
        for ri in range(2):
            P.dma(stg128[ri][:], s5BT[:, ri, st, :], W=[('stg128', ri)], q='sp')
            P.copy('act', BTb[:, ri, st, :], stg128[ri][:], R=[('stg128', ri)], W=['BTb'])
        for ri in range(2):
            P.dma(stg128[2 + ri][:], s5CT[:, ri, st, :], W=[('stg128', 2 + ri)], q='sp')
        P.ts(eng, tmpc[:, :128], stg128[3][:], zi, ALU.mult, R=[('stg128', 3)] + SK('zi'), W=['tmpc'])
        P.stt(eng, CTb[:, 0, st, :], stg128[2][:], zr, tmpc[:, :128], ALU.mult, ALU.subtract,
              R=[('stg128', 2), 'tmpc'] + SK('zr'), W=['CTb'])
        P.ts(eng, tmpc[:, :128], stg128[3][:], zr, ALU.mult, R=[('stg128', 3), 'CTb'] + SK('zr'), W=['tmpc'])
        P.stt(eng, tmpc[:, 128:256], stg128[2][:], zi, tmpc[:, :128], ALU.mult, ALU.add,
              R=[('stg128', 2), 'tmpc'] + SK('zi'), W=['tmpc'])
        P.ts(eng, CTb[:, 1, st, :], tmpc[:, 128:256], -1.0, ALU.mult, R=['tmpc'], W=['CTb'])
    carry = P.sb("carry", [128, 8, 2]); P.memset('dve', carry[:], 0.0, W=['carry'])
    s5t = {k: [P.sb("s5_%s%d" % (k, i), [128, CH]) for i in range(2)] for k in ['a', 'b', 'c', 'd']}
    sre = [P.sb("sre%d" % i, [128, CH]) for i in range(2)]; sim = [P.sb("sim%d" % i, [128, CH]) for i in range(2)]
    sreb = [P.sb("sreb%d" % i, [128, CH], BF16) for i in range(2)]; simb = [P.sb("simb%d" % i, [128, CH], BF16) for i in range(2)]
    ub = [P.sb("ub%d" % i, [128, CH], BF16) for i in range(2)]
    zt = [P.sb("zt%d" % i, [128, CH]) for i in range(2)]
    ztb = [P.sb("ztb%d" % i, [128, CH], BF16) for i in range(2)]
    obc = P.sb("obc", [128, 2, CH], BF16)
    gA = P.sb("gA", [128, CH]); gB = P.sb("gB", [128, CH])

    wg = P.sb("wg", [16, 128]); nbg = P.sb("nbg", [128, 1]); ngrep = P.sb("ngrep", [128, 256])
    P.dma(wg[:], gla_wg, W=['wg']); P.dma(nbg[:], gla_bg, W=['nbg']); P.dma(ngrep[:], gla_ng, W=['ngrep'])
    P.ts('dve', nbg[:], nbg[:], -1.0, ALU.mult, R=['nbg'], W=['nbg'])
    S = P.sb("S", [128, 256]); Sb = P.sb("Sb", [128, 256], BF16)
    P.memset('dve', S[:], 0.0, W=['S']); P.memset('dve', Sb[:], 0.0, W=['Sb'])
    gk = P.sb("gk", [128, CH]); bcum = P.sb("bcum", [128, CH])
    gt = {k: P.sb("g_" + k, [128, 128]) for k in ['eb', 'enb', 'ekst', 'kstT']}
    gtb = {k: P.sb("gb_" + k, [128, 128], BF16) for k in ['qin', 'kin', 'kst', 'att']}
    vtok = P.sb("vtok", [128, 256], BF16); rtok = P.sb("rtok", [128, 256]); osb = P.sb("osb", [128, 256])
    rsil = [P.sb("rsil%d" % i, [128, CH]) for i in range(2)]
    gsm = {k: P.sb("gsm_" + k, [128, 1]) for k in ['dec', 'ss', 'rstd', 'junk']}
    junk = P.sb("junk", [128, 256])
    GK = lambda *n: ['g_' + a for a in n]

    for c in range(NCHUNK):
        load_x_chunk(P, hT, c, xst, xb)
        for i, n in enumerate(names):
            bank = i % 2
            proj_tile(P, wb, i * 128, 128, xb, pb[bank], pk[bank])
            P.copy('act' if i % 2 == 0 else 'dve', pt[n][:], pb[bank][:, :CH], R=[pk[bank]], W=['pt_' + n])
        proj_tile(P, wb, 1024, 16, xb, pb[0], pk[0])
        P.copy('dve', glT[:], pb[0][:16, :CH], R=[pk[0]], W=['glT'])

        def s5_chain():
            for ut in range(2):
                un = 'u%d' % ut
                P.copy('act', ub[ut][:], pt[un][:], R=['pt_' + un], W=[('ub', ut)])
                for s4 in range(4):
                    st = ut * 4 + s4
                    i2 = st % 2
                    xr = pb[2]; xi = pb[3]
                    P.mm(xr[:, :CH], BTb[:, 0, st, :], ub[ut][:], R=['BTb', ('ub', ut)], W=[pk[2]])
                    P.mm(xi[:, :CH], BTb[:, 1, st, :], ub[ut][:], R=['BTb', ('ub', ut)], W=[pk[3]])
                    a = s5t['a'][i2]; b = s5t['b'][i2]; cc = s5t['c'][i2]; d = s5t['d'][i2]
                    ka, kb_, kc, kd = ('s5a', i2), ('s5b', i2), ('s5c', i2), ('s5d', i2)
                    P.tt('dve', a[:], xr[:, :CH], Er[:, st, :], ALU.mult, R=[pk[2], 'Er'], W=[ka])
                    P.tt('dve', b[:], xi[:, :CH], Ei[:, st, :], ALU.mult, R=[pk[3], 'Ei'], W=[kb_])
                    P.tt('pool', a[:], a[:], b[:], ALU.add, R=[ka, kb_], W=[ka])
                    P.tt('dve', cc[:], xi[:, :CH], Er[:, st, :], ALU.mult, R=[pk[3], 'Er'], W=[kc])
                    P.tt('dve', d[:], xr[:, :CH], Ei[:, st, :], ALU.mult, R=[pk[2], 'Ei'], W=[kd])
                    P.tt('pool', cc[:], cc[:], d[:], ALU.subtract, R=[kc, kd], W=[kc])
                    yield
                    P.op('dve', lambda e, a=a, b=b, st=st: e.tensor_tensor_scan(
                        out=b[:], data0=sc['r'][:, st:st + 1].to_broadcast([128, CH]), data1=a[:], initial=carry[:, st, 0:1], op0=ALU.mult, op1=ALU.add),
                        R=[ka, 'carry'] + SK('r'), W=[kb_])
                    P.op('dve', lambda e, cc=cc, d=d, st=st: e.tensor_tensor_scan(
                        out=d[:], data0=sc['r'][:, st:st + 1].to_broadcast([128, CH]), data1=cc[:], initial=carry[:, st, 1:2], op0=ALU.mult, op1=ALU.add),
                        R=[kc, 'carry'] + SK('r'), W=[kd])
                    sr = sre[i2]; si = sim[i2]
                    P.tt('pool', a[:], b[:], Er[:, st, :], ALU.mult, R=[kb_, 'Er'], W=[ka])
                    P.tt('pool', cc[:], d[:], Ei[:, st, :], ALU.mult, R=[kd, 'Ei'], W=[kc])
                    P.tt('dve', sr[:], a[:], cc[:], ALU.subtract, R=[ka, kc], W=[('sre', i2)])
                    P.tt('pool', a[:], b[:], Ei[:, st, :], ALU.mult, R=[kb_, 'Ei'], W=[ka])
                    P.tt('pool', cc[:], d[:], Er[:, st, :], ALU.mult, R=[kd, 'Er'], W=[kc])
                    P.tt('dve', si[:], a[:], cc[:], ALU.add, R=[ka, kc], W=[('sim', i2)])
                    P.copy('dve', carry[:, st, 0:1], sr[:, CH - 1:CH], R=[('sre', i2), 'carry'], W=['carry'])
                    P.copy('dve', carry[:, st, 1:2], si[:, CH - 1:CH], R=[('sim', i2), 'carry'], W=['carry'])
                    P.copy('act', sreb[i2][:], sr[:], R=[('sre', i2)], W=[('sreb', i2)])
                    P.copy('act', simb[i2][:], si[:], R=[('sim', i2)], W=[('simb', i2)])
                    P.mm(pb[4 + ut][:, :CH], CTb[:, 0, st, :], sreb[i2][:], start=(s4 == 0), stop=False,
                         R=['CTb', ('sreb', i2)], W=[pk[4 + ut]])
                    P.mm(pb[4 + ut][:, :CH], CTb[:, 1, st, :], simb[i2][:], start=False, stop=(s4 == 3),
                         R=['CTb', ('simb', i2)], W=[pk[4 + ut]])
                    yield
                P.stt('dve', zt[ut][:], pt[un][:], dcol[:, ut:ut + 1], pb[4 + ut][:, :CH], ALU.mult, ALU.add,
                      R=['pt_' + un, 'dcol', pk[4 + ut]], W=[('zt', ut)])
                gelu_tanh(P, zt[ut][:], zt[ut][:], gA, gB, CH, ('zt', ut), ('zt', ut), ['gA', 'gB'])
                P.copy('act', ztb[ut][:], zt[ut][:], R=[('zt', ut)], W=[('ztb', ut)])
                P.dma(y_loc[c][ut * 128:(ut + 1) * 128, :], ztb[ut][:], R=[('ztb', ut)], W=[('y_loc', c, ut)], q='pool')
                ykeys.append(('y_loc', c, ut))
                yield


        def gla_chain():
            P.mm(pb[7][:, :CH], wg[:], glT[:], R=['wg', 'glT'], W=[pk[7]])
            P.act(gk[:], pb[7][:, :CH], AF.Exp, scale=-1.0, bias=nbg[:], R=[pk[7], 'nbg'], W=['gk'])
            P.ts('dve', gk[:], gk[:], 1.0, ALU.add, R=['gk'], W=['gk'])
            P.act(gk[:], gk[:], AF.Ln, R=['gk'], W=['gk'])
            P.ts('dve', gk[:], gk[:], -1.0 / 16, ALU.mult, R=['gk'], W=['gk'])
            for i in range(2):
                P.act(rsil[i][:], pt['r%d' % i][:], AF.Silu, R=['pt_r%d' % i], W=[('rsil', i)])
            for sb_ in range(4):
                if (c * CH + (sb_ + 1) * 128) <= PAD:
                    continue
                ss = slice(sb_ * 128, (sb_ + 1) * 128)
                P.op('dve', lambda e, ss=ss: e.tensor_tensor_scan(out=bcum[:, ss], data0=onescol[:, 0:1].to_broadcast([128, 128]), data1=gk[:, ss],
                                                                 initial=0.0, op0=ALU.mult, op1=ALU.add),
                     R=['gk', 'onescol'], W=['bcum'])
                P.act(gt['eb'][:], bcum[:, ss], AF.Exp, R=['bcum'], W=GK('eb'))
                P.act(gt['enb'][:], bcum[:, ss], AF.Exp, scale=-1.0, R=['bcum'], W=GK('enb'))
                P.act(gt['ekst'][:], bcum[:, ss], AF.Exp, scale=-1.0, bias=bcum[:, sb_ * 128 + 127:sb_ * 128 + 128],
                      R=['bcum'], W=GK('ekst'))
                P.act(gsm['dec'][:], bcum[:, sb_ * 128 + 127:sb_ * 128 + 128], AF.Exp, R=['bcum'], W=['gsm_dec'])
                P.stt('dve', gtb['qin'][:], pt['q'][:, ss], 128.0 ** -0.5, gt['eb'][:], ALU.mult, ALU.mult,
                      R=['pt_q'] + GK('eb'), W=['gb_qin'])
                P.tt('dve', gtb['kin'][:], pt['k'][:, ss], gt['enb'][:], ALU.mult, R=['pt_k'] + GK('enb'), W=['gb_kin'])
                P.tt('dve', gt['kstT'][:], pt['k'][:, ss], gt['ekst'][:], ALU.mult, R=['pt_k'] + GK('ekst'), W=GK('kstT'))
                yield
                P.transpose(pb[6][:, 0:128], gt['kstT'][:], ident[:], R=GK('kstT') + ['ident'], W=[pk[6]])
                for i in range(2):
                    P.transpose(pb[6][:, 128 + i * 128:256 + i * 128], pt['v%d' % i][:, ss], ident[:],
                                R=['pt_v%d' % i, 'ident'], W=[pk[6]])
                P.copy('act', gtb['kst'][:], pb[6][:, 0:128], R=[pk[6]], W=['gb_kst'])
                P.copy('act', vtok[:], pb[6][:, 128:384], R=[pk[6]], W=['vtok'])
                yield
                for i in range(2):
                    P.transpose(pb[7][:, i * 128:(i + 1) * 128], rsil[i][:, ss], ident[:], R=[('rsil', i), 'ident'], W=[pk[7]])
                P.copy('act', rtok[:], pb[7][:, :256], R=[pk[7]], W=['rtok'])
                P.mm(pb[6][:, 384:512], gtb['kin'][:], gtb['qin'][:], R=['gb_kin', 'gb_qin'], W=[pk[6]])
                P.tt('dve', gtb['att'][:], pb[6][:, 384:512], U[:], ALU.mult, R=[pk[6], 'U'], W=['gb_att'])
                yield
                P.mm(pb[7][:, 256:512], gtb['att'][:], vtok[:], start=True, stop=False, R=['gb_att', 'vtok'], W=[pk[7]])
                P.mm(pb[7][:, 256:512], gtb['qin'][:], Sb[:], start=False, stop=True, R=['gb_qin', 'Sb'], W=[pk[7]])
                P.mm(pb[6][:, 0:256], gtb['kst'][:], vtok[:], R=['gb_kst', 'vtok'], W=[pk[6]])
                P.stt('dve', S[:], S[:], gsm['dec'][:], pb[6][:, 0:256], ALU.mult, ALU.add, R=['S', 'gsm_dec', pk[6]], W=['S'])
                P.copy('act', Sb[:], S[:], R=['S'], W=['Sb'])
                yield
                P.act(junk[:], pb[7][:, 256:512], AF.Square, accum_out=gsm['ss'][:], R=[pk[7]], W=['junk', 'gsm_ss'])
                P.ts('dve', gsm['rstd'][:], gsm['ss'][:], 1.0 / 256, ALU.mult, 1e-6, ALU.add, R=['gsm_ss'], W=['gsm_rstd'])
                P.act(gsm['rstd'][:], gsm['rstd'][:], AF.Sqrt, R=['gsm_rstd'], W=['gsm_rstd'])
                P.op('dve', lambda e: e.reciprocal(out=gsm['rstd'][:], in_=gsm['rstd'][:]), R=['gsm_rstd'], W=['gsm_rstd'])
                P.tt('pool', rtok[:], rtok[:], ngrep[:], ALU.mult, R=['rtok', 'ngrep'], W=['rtok'])
                P.stt('dve', osb[:], pb[7][:, 256:512], gsm['rstd'][:], rtok[:], ALU.mult, ALU.mult,
                      R=[pk[7], 'gsm_rstd', 'rtok'], W=['osb'])
                yield
                for i in range(2):
                    P.transpose(pb[7][:, i * 128:(i + 1) * 128], osb[:, i * 128:(i + 1) * 128], ident[:], R=['osb', 'ident'], W=[pk[7]])
                for i in range(2):
                    P.copy('act', obc[:, i, ss], pb[7][:, i * 128:(i + 1) * 128], R=[pk[7]], W=['obc'])
                yield
        gens = [s5_chain(), gla_chain()]
        while gens:
            for g in list(gens):
                try:
                    next(g)
                except StopIteration:
                    gens.remove(g)
        for i in range(2):
            P.dma(y_loc[c][256 + i * 128:256 + (i + 1) * 128, :], obc[:, i, :], R=['obc'], W=[('y_loc', c, 2 + i)], q='pool')
            ykeys.append(('y_loc', c, 2 + i))
        P.allgather(y_all[c], y_loc[c], groups, R=[('y_loc', c, k) for k in range(4)], W=[('y_all', c)])
    return ykeys

import math, os


def phase_cd(P, pb, pk, NCHUNK, h_all, y_loc, y_all, groups, pre="c_", stage=99, PN=342):
    Tp = NCHUNK * CH
    NCOL = 12 * 128 + 4
    T2 = 2052
    w_in = P.dram(pre + "w_in", [D, NCOL])
    convw = P.dram(pre + "convw", [128, 8, 4]); convb = P.dram(pre + "convb", [128, 8])
    gpar = P.dram(pre + "gpar", [4, 4])
    gdn_ng = P.dram(pre + "gdn_ng", [128, 128])
    lwa = P.dram(pre + "lwa", [128, 2, 128]); lwx = P.dram(pre + "lwx", [128, 2, 128]); lpar = P.dram(pre + "lpar", [128, 2, 3])
    c_U = P.dram(pre + "c_U", [128, 128]); c_Us = P.dram(pre + "c_Us", [128, 128]); c_ident = P.dram(pre + "c_ident", [128, 128])
    c_sel4 = P.dram(pre + "c_sel4", [4, 512])
    ykeys = []
    R_ = lambda b, i: pk[6] if (b, i) == (3, 3) else pk[b]
    wb = inproj_setup(P, w_in, NCOL)
    xb = P.sb("xb", [128, 16, CH], BF16)
    gobc = P.sb("gobc", [128, 2, CH], BF16); ylb = P.sb("ylb", [128, 2, CH], BF16)
    praw = [P.sb("praw%d" % i, [128, CH + 3]) for i in range(8)]
    pc = [P.sb("pc%d" % i, [128, CH]) for i in range(8)]
    pz = [P.sb("pz%d" % i, [128, CH]) for i in range(4)]
    baT = P.sb("baT", [4, CH])
    U = P.sb("U", [128, 128]); Us = P.sb("Us", [128, 128]); ident = P.sb("ident", [128, 128])
    ones_f = P.sb("ones_f", [128, 128]); onescol = P.sb("onescol", [128, 1]); sel4 = P.sb("sel4", [4, 512])
    P.dma(U[:], c_U, W=['U']); P.dma(Us[:], c_Us, W=['Us']); P.dma(ident[:], c_ident, W=['ident'])
    P.dma(sel4[:], c_sel4, W=['sel4'])
    P.memset('dve', ones_f[:], 1.0, W=['ones_f']); P.memset('dve', onescol[:], 1.0, W=['onescol'])
    cw = P.sb("cw", [128, 8, 4]); cb = P.sb("cb", [128, 8])
    P.dma(cw[:], convw, W=['cw']); P.dma(cb[:], convb, W=['cb'])
    for i in range(8):
        P.memset('pool', praw[i][:, 0:3], 0.0, W=[('praw', i)])
    gp = P.sb("gp", [4, 4]); P.dma(gp[:], gpar, W=['gp'])
    negA = P.sb("negA", [4, 1])
    P.act(negA[:], gp[:, 1:2], AF.Exp, R=['gp'], W=['negA'])
    P.ts('dve', negA[:], negA[:], -1.0, ALU.mult, R=['negA'], W=['negA'])
    ngrep = P.sb("ngrep", [128, 128]); P.dma(ngrep[:], gdn_ng, W=['ngrep'])
    S = [P.sb("S%d" % h, [128, 128]) for h in range(2)]; Sb = [P.sb("Sb%d" % h, [128, 128], BF16) for h in range(2)]
    for h in range(2):
        P.memset('dve', S[h][:], 0.0, W=[('S', h)]); P.memset('dve', Sb[h][:], 0.0, W=[('Sb', h)])
    sig4 = P.sb("sig4", [4, CH]); g4 = P.sb("g4", [4, CH]); bg4 = P.sb("bg4", [4, CH])
    Brep = [P.sb("Brep%d" % h, [128, CH]) for h in range(2)]; Grep = [P.sb("Grep%d" % h, [128, CH]) for h in range(2)]
    qnb = [P.sb("qnb%d" % h, [128, CH], BF16) for h in range(2)]; knb = [P.sb("knb%d" % h, [128, CH], BF16) for h in range(2)]
    knf = [P.sb("knf%d" % h, [128, CH]) for h in range(2)]
    t512 = [P.sb("t512_%d" % i, [128, CH]) for i in range(3)]
    f2 = [{k: P.sb("f%d_%s" % (h, k), [128, 128]) for k in
           ['R1', 'DT', 'Gam', 'GamU', 'P0', 'BU', 'Pa', 'Pb', 'Qa', 'Qb', 'R', 'egr', 'zs', 'osb', 'junk', 'ktok', 'vtok']}
          for h in range(2)]
    bq2 = [{k: P.sb("b%d_%s" % (h, k), [128, 128], BF16) for k in ['Rb', 'Aqk', 'kbg', 'kst', 'vb', 'nwc', 'vnew', 'qdec']}
           for h in range(2)]
    sm2 = [{k: P.sb("sm%d_%s" % (h, k), [128, 1]) for k in ['gcc', 'glast', 'egc', 'bege', 'ekl', 'dec', 'ss', 'rstd']}
           for h in range(2)]
    tok4 = P.sb("tok4", [128, 4])
    FK = lambda *n: ['f_' + a for a in n]
    BK = lambda *n: ['b_' + a for a in n]
    MK = lambda *n: ['sm_' + a for a in n]
    wa_f = P.sb("wa_f", [128, 2, 128]); wx_f = P.sb("wx_f", [128, 2, 128]); lp = P.sb("lp", [128, 2, 3])
    wa_b = P.sb("wa_b", [128, 2, 128], BF16); wx_b = P.sb("wx_b", [128, 2, 128], BF16)
    P.dma(wa_f[:], lwa, W=['wa_f']); P.dma(wx_f[:], lwx, W=['wx_f']); P.dma(lp[:], lpar, W=['lp'])
    P.copy('dve', wa_b[:], wa_f[:], R=['wa_f'], W=['wa_b']); P.copy('dve', wx_b[:], wx_f[:], R=['wx_f'], W=['wx_b'])
    ccol = P.sb("ccol", [128, 2])
    P.act(ccol[:], lp[:, :, 2], AF.Exp, scale=-1.0, R=['lp'], W=['ccol'])
    P.ts('dve', ccol[:], ccol[:], 1.0, ALU.add, R=['ccol'], W=['ccol'])
    P.act(ccol[:], ccol[:], AF.Ln, R=['ccol'], W=['ccol'])
    P.ts('dve', ccol[:], ccol[:], -8.0, ALU.mult, R=['ccol'], W=['ccol'])
    hprev = P.sb("hprev", [128, 2]); P.memset('dve', hprev[:], 0.0, W=['hprev'])
    xcb = P.sb("xcb", [128, CH], BF16)
    L = {k: P.sb("l_" + k, [128, CH]) for k in ['r', 'i', 'a', 'bx', 'h', 'gA', 'gB', 'y']}
    LK = lambda *n: ['l_' + a for a in n]

    for c in range(NCHUNK):
        xk = [('xb', dt) for dt in range(16)]
        p0 = c * CH
        if p0 < PAD:
            P.memset('pool', xb[:, :, 0:PAD - p0], 0.0, W=xk)
        pos = max(p0, PAD)
        while pos < p0 + CH:
            t = pos - PAD
            r = t // T2
            col = t - r * T2
            pc_ = col // PN
            off = col - pc_ * PN
            n = min(p0 + CH - pos, PN - off)
            for half in range(2):
                P.dma(xb[:, 8 * half:8 * half + 8, pos - p0:pos - p0 + n],
                      h_all[pc_][half][r * 1024:(r + 1) * 1024, off:off + n].rearrange("(t p) n -> p t n", p=128),
                      W=xk, q='sp')
            pos += n
        for i in range(12):
            bank = i % 2
            proj_tile(P, wb, i * 128, 128, xb, pb[bank], pk[bank])
            if i < 8:
                P.copy('act', praw[i][:, 3:CH + 3], pb[bank][:, :CH], R=[pk[bank]], W=[('praw', i)])
                P.ts('dve', pc[i][:], praw[i][:, 0:CH], cw[:, i, 0:1], ALU.mult, cb[:, i:i + 1], ALU.add,
                     R=[('praw', i), 'cw', 'cb'], W=[('pc', i)])
                for jj in range(1, 4):
                    P.stt('dve', pc[i][:], praw[i][:, jj:CH + jj], cw[:, i, jj:jj + 1], pc[i][:], ALU.mult, ALU.add,
                          R=[('praw', i), 'cw', ('pc', i)], W=[('pc', i)])
                P.copy('pool', praw[i][:, 0:3], praw[i][:, CH:CH + 3], R=[('praw', i)], W=[('praw', i)])
                if i < 6:
                    P.act(pc[i][:], pc[i][:], AF.Silu, R=[('pc', i)], W=[('pc', i)])
            else:
                P.copy('act', pz[i - 8][:], pb[bank][:, :CH], R=[pk[bank]], W=[('pz', i - 8)])
        proj_tile(P, wb, 12 * 128, 4, xb, pb[0], pk[0])
        P.copy('act', baT[:], pb[0][:4, :CH], R=[pk[0]], W=['baT'])

        for b in range(2):
            xc = pc[6 + b]
            P.copy('act', xcb[:], xc[:], R=[('pc', 6 + b)], W=['xcb'])
            P.mm(pb[6][:, :CH], wa_b[:, b, :], xcb[:], R=['wa_b', 'xcb'], W=[pk[6]])
            P.mm(pb[7][:, :CH], wx_b[:, b, :], xcb[:], R=['wx_b', 'xcb'], W=[pk[7]])
            P.act(L['r'][:], pb[6][:, :CH], AF.Sigmoid, bias=lp[:, b, 0:1], R=[pk[6], 'lp'], W=LK('r'))
            P.act(L['i'][:], pb[7][:, :CH], AF.Sigmoid, bias=lp[:, b, 1:2], R=[pk[7], 'lp'], W=LK('i'))
            P.act(L['a'][:], L['r'][:], AF.Exp, scale=ccol[:, b:b + 1], R=LK('r') + ['ccol'], W=LK('a'))
            P.tt('pool', L['bx'][:], L['a'][:], L['a'][:], ALU.mult, R=LK('a'), W=LK('bx'))
            P.ts('pool', L['bx'][:], L['bx'][:], -1.0, ALU.mult, 1.0, ALU.add, R=LK('bx'), W=LK('bx'))
            P.act(L['bx'][:], L['bx'][:], AF.Sqrt, R=LK('bx'), W=LK('bx'))
            P.tt('pool', L['bx'][:], L['bx'][:], L['i'][:], ALU.mult, R=LK('bx', 'i'), W=LK('bx'))
            P.tt('pool', L['bx'][:], L['bx'][:], xc[:], ALU.mult, R=LK('bx') + [('pc', 6 + b)], W=LK('bx'))
            c0 = PAD if c == 0 else 0
            if c == 0:
                P.memset('dve', L['h'][:, :PAD], 0.0, W=LK('h'))
            P.op('dve', lambda e, b=b, c0=c0: e.tensor_tensor_scan(
                out=L['h'][:, c0:], data0=L['a'][:, c0:], data1=L['bx'][:, c0:], initial=hprev[:, b:b + 1],
                op0=ALU.mult, op1=ALU.add), R=LK('a', 'bx') + ['hprev'], W=LK('h'))
            P.copy('dve', hprev[:, b:b + 1], L['h'][:, CH - 1:CH], R=LK('h') + ['hprev'], W=['hprev'])
            gelu_tanh(P, L['y'][:], pz[2 + b][:], L['gA'], L['gB'], CH, ('pz', 2 + b), 'l_y', LK('gA', 'gB'))
            P.tt('pool', L['y'][:], L['y'][:], L['h'][:], ALU.mult, R=LK('y', 'h'), W=LK('y'))
            P.copy('act', ylb[:, b, :], L['y'][:], R=LK('y'), W=['ylb'])
            P.dma(y_loc[c][256 + b * 128:256 + (b + 1) * 128, :], ylb[:, b, :], R=['ylb'], W=[('y_loc', c, 2 + b)], q='pool')
            ykeys.append(('y_loc', c, 2 + b))

        if stage < 1:
            continue
        P.act(sig4[:], baT[:], AF.Sigmoid, R=['baT'], W=['sig4'])
        P.act(g4[:], baT[:], AF.Exp, bias=gp[:, 0:1], R=['baT', 'gp'], W=['g4'])
        P.ts('dve', g4[:], g4[:], 1.0, ALU.add, R=['g4'], W=['g4'])
        P.act(g4[:], g4[:], AF.Ln, R=['g4'], W=['g4'])
        P.ts('dve', g4[:], g4[:], negA[:, 0:1], ALU.mult, R=['g4', 'negA'], W=['g4'])
        P.ts('dve', bg4[:], sig4[:], gp[:, 2:3], ALU.mult, R=['sig4', 'gp'], W=['bg4'])
        P.stt('dve', bg4[:], g4[:], gp[:, 3:4], bg4[:], ALU.mult, ALU.add, R=['g4', 'gp', 'bg4'], W=['bg4'])
        for h in range(2):
            P.mm(pb[6][:, :CH], sel4[:, h * 128:(h + 1) * 128], sig4[:], R=['sel4', 'sig4'], W=[pk[6]])
            P.copy('act', Brep[h][:], pb[6][:, :CH], R=[pk[6]], W=[('Brep', h)])
            P.mm(pb[7][:, :CH], sel4[:, (2 + h) * 128:(3 + h) * 128], g4[:], R=['sel4', 'g4'], W=[pk[7]])
            P.copy('act', Grep[h][:], pb[7][:, :CH], R=[pk[7]], W=[('Grep', h)])
            for which, src, scale_ in (('q', pc[h], 128.0 ** -0.5), ('k', pc[2 + h], 1.0)):
                P.act(t512[0][:], src[:], AF.Square, R=[('pc', h if which == 'q' else 2 + h)], W=[('t512', 0)])
                P.mm(pb[6][:, :CH], ones_f[:], t512[0][:], R=['ones_f', ('t512', 0)], W=[pk[6]])
                P.ts('dve', t512[1][:], pb[6][:, :CH], 1e-6, ALU.add, R=[pk[6]], W=[('t512', 1)])
                P.act(t512[1][:], t512[1][:], AF.Sqrt, R=[('t512', 1)], W=[('t512', 1)])
                P.op('dve', lambda e: e.reciprocal(out=t512[1][:], in_=t512[1][:]), R=[('t512', 1)], W=[('t512', 1)])
                if which == 'q':
                    P.stt('dve', qnb[h][:], src[:], scale_, t512[1][:], ALU.mult, ALU.mult, R=[('pc', h), ('t512', 1)], W=[('qnb', h)])
                else:
                    P.tt('dve', knf[h][:], src[:], t512[1][:], ALU.mult, R=[('pc', 2 + h), ('t512', 1)], W=[('knf', h)])
                    P.copy('act', knb[h][:], knf[h][:], R=[('knf', h)], W=[('knb', h)])

        if stage < 2:
            continue
        def head_chain(h, sb_, ss):
            f = f2[h]; bq = bq2[h]; sm = sm2[h]
            FK = lambda *n: ['f%d_%s' % (h, a) for a in n]
            BK = lambda *n: ['b%d_%s' % (h, a) for a in n]
            MK = lambda *n: ['sm%d_%s' % (h, a) for a in n]
            bA, bB, bC = (2, 3, 4) if h == 0 else (5, 6, 7)
            pA, pB, pC = pb[bA], pb[bB], pb[bC]
            kA, kB, kC = pk[bA], pk[bB], pk[bC]
            bcol = tok4[:, h:h + 1]; gcol = tok4[:, 2 + h:3 + h]
            P.op('dve', lambda e: e.tensor_tensor_scan(
                out=f['R1'][:], data0=onescol[:, 0:1].to_broadcast([128, 128]), data1=Grep[h][:, ss], initial=0.0,
                op0=ALU.mult, op1=ALU.add), R=[('Grep', h), 'onescol'], W=FK('R1'))
            P.mm(pA[:, 0:1], U[:], gcol, R=['U', 'tok4'], W=[kA])
            P.copy('act', sm['gcc'][:], pA[:, 0:1], R=[kA], W=MK('gcc'))
            P.copy('pool', sm['glast'][:], f['R1'][:, 127:128], R=FK('R1'), W=MK('glast'))
            yield
            P.ts('dve', f['DT'][:], f['R1'][:], sm['gcc'][:], ALU.subtract, 0.0, ALU.min, R=FK('R1') + MK('gcc'), W=FK('DT'))
            P.act(f['Gam'][:], f['DT'][:], AF.Exp, R=FK('DT'), W=FK('Gam'))
            P.tt('pool', f['GamU'][:], f['Gam'][:], U[:], ALU.mult, R=FK('Gam') + ['U'], W=FK('GamU'))
            P.tt('pool', f['BU'][:], Brep[h][:, ss], Us[:], ALU.mult, R=[('Brep', h), 'Us'], W=FK('BU'))
            P.act(f['egr'][:], f['R1'][:], AF.Exp, R=FK('R1'), W=FK('egr'))
            P.act(sm['egc'][:], sm['gcc'][:], AF.Exp, R=MK('gcc'), W=MK('egc'))
            P.tt('pool', sm['bege'][:], sm['egc'][:], bcol, ALU.mult, R=MK('egc') + ['tok4'], W=MK('bege'))
            P.act(sm['ekl'][:], sm['gcc'][:], AF.Exp, scale=-1.0, bias=sm['glast'][:], R=MK('gcc', 'glast'), W=MK('ekl'))
            P.act(sm['dec'][:], sm['glast'][:], AF.Exp, R=MK('glast'), W=MK('dec'))
            yield
            P.mm(pA[:, 0:128], knb[h][:, ss], knb[h][:, ss], R=[('knb', h)], W=[kA])
            P.mm(pA[:, 128:256], knb[h][:, ss], qnb[h][:, ss], R=[('knb', h), ('qnb', h)], W=[kA])
            P.transpose(pA[:, 256:384], knf[h][:, ss], ident[:], R=[('knf', h), 'ident'], W=[kA])
            P.transpose(pA[:, 384:512], pc[4 + h][:, ss], ident[:], R=[('pc', 4 + h), 'ident'], W=[kA])
            yield
            P.tt('dve', f['P0'][:], pA[:, 0:128], f['Gam'][:], ALU.mult, R=[kA] + FK('Gam'), W=FK('P0'))
            P.tt('dve', f['Pa'][:], f['P0'][:], f['BU'][:], ALU.mult, R=FK('P0', 'BU'), W=FK('Pa'))
            P.tt('dve', bq['Aqk'][:], pA[:, 128:256], f['GamU'][:], ALU.mult, R=[kA] + FK('GamU'), W=BK('Aqk'))
            P.copy('act', f['ktok'][:], pA[:, 256:384], R=[kA], W=FK('ktok'))
            P.copy('act', f['vtok'][:], pA[:, 384:512], R=[kA], W=FK('vtok'))
            P.ts('pool', bq['kbg'][:], f['ktok'][:], sm['bege'][:], ALU.mult, R=FK('ktok') + MK('bege'), W=BK('kbg'))
            P.ts('pool', bq['kst'][:], f['ktok'][:], sm['ekl'][:], ALU.mult, R=FK('ktok') + MK('ekl'), W=BK('kst'))
            P.ts('pool', bq['vb'][:], f['vtok'][:], bcol, ALU.mult, R=FK('vtok') + ['tok4'], W=BK('vb'))
            yield
            P.transpose(pB[:, 0:128], f['Pa'][:], ident[:], R=FK('Pa') + ['ident'], W=[kB])
            P.copy('act', f['Qa'][:], pB[:, 0:128], R=[kB], W=FK('Qa'))
            P.tt('pool', f['R'][:], ident[:], f['Pa'][:], ALU.subtract, R=['ident'] + FK('Pa'), W=FK('R'))
            yield
            Pc, Qc, Pn, Qn = 'Pa', 'Qa', 'Pb', 'Qb'
            for lvl in range(6):
                P.mm(pB[:, 128:256], f[Qc][:], f[Pc][:], R=FK(Qc, Pc), W=[kB])
                P.mm(pB[:, 256:384], f[Pc][:], f[Qc][:], R=FK(Qc, Pc), W=[kB])
                P.copy('act', f[Pn][:], pB[:, 128:256], R=[kB], W=FK(Pn))
                P.copy('act', f[Qn][:], pB[:, 256:384], R=[kB], W=FK(Qn))
                yield
                P.mm(pB[:, 384:512], f[Qn][:], f['R'][:], R=FK(Qn, 'R'), W=[kB])
                P.tt('dve', f['R'][:], f['R'][:], pB[:, 384:512], ALU.add, R=FK('R') + [kB], W=FK('R'))
                Pc, Qc, Pn, Qn = Pn, Qn, Pc, Qc
                yield
            P.copy('act', bq['Rb'][:], f['R'][:], R=FK('R'), W=BK('Rb'))
            P.mm(pC[:, 128:256], bq['kbg'][:], bq['Rb'][:], R=BK('kbg', 'Rb'), W=[kC])
            P.op('act', lambda e: e.activation(out=bq['nwc'][:], in_=pC[:, 128:256], func=AF.Copy, scale=-1.0),
                 R=[kC], W=BK('nwc'))
            yield
            P.mm(pC[:, 0:128], bq['Rb'][:], bq['vb'][:], start=True, stop=False, R=BK('Rb', 'vb'), W=[kC])
            P.mm(pC[:, 0:128], bq['nwc'][:], Sb[h][:], start=False, stop=True, R=BK('nwc') + [('Sb', h)], W=[kC])
            P.copy('act', bq['vnew'][:], pC[:, 0:128], R=[kC], W=BK('vnew'))
            P.tt('pool', bq['qdec'][:], qnb[h][:, ss], f['egr'][:], ALU.mult, R=[('qnb', h)] + FK('egr'), W=BK('qdec'))
            yield
            P.mm(pC[:, 256:384], bq['qdec'][:], Sb[h][:], start=True, stop=False, R=BK('qdec') + [('Sb', h)], W=[kC])
            P.mm(pC[:, 256:384], bq['Aqk'][:], bq['vnew'][:], start=False, stop=True, R=BK('Aqk', 'vnew'), W=[kC])
            P.mm(pC[:, 384:512], bq['kst'][:], bq['vnew'][:], R=BK('kst', 'vnew'), W=[kC])
            P.stt('dve', S[h][:], S[h][:], sm['dec'][:], pC[:, 384:512], ALU.mult, ALU.add,
                  R=[('S', h), kC] + MK('dec'), W=[('S', h)])
            P.copy('act', Sb[h][:], S[h][:], R=[('S', h)], W=[('Sb', h)])
            yield
            P.act(f['junk'][:], pC[:, 256:384], AF.Square, accum_out=sm['ss'][:], R=[kC], W=FK('junk') + MK('ss'))
            P.ts('dve', sm['rstd'][:], sm['ss'][:], 1.0 / 128, ALU.mult, 1e-6, ALU.add, R=MK('ss'), W=MK('rstd'))
            P.act(sm['rstd'][:], sm['rstd'][:], AF.Sqrt, R=MK('rstd'), W=MK('rstd'))
            P.op('dve', lambda e: e.reciprocal(out=sm['rstd'][:], in_=sm['rstd'][:]), R=MK('rstd'), W=MK('rstd'))
            P.transpose(pA[:, 0:128], pz[h][:, ss], ident[:], R=[('pz', h), 'ident'], W=[kA])
            P.act(f['zs'][:], pA[:, 0:128], AF.Silu, R=[kA], W=FK('zs'))
            P.tt('pool', f['zs'][:], f['zs'][:], ngrep[:], ALU.mult, R=FK('zs') + ['ngrep'], W=FK('zs'))
            yield
            P.stt('dve', f['osb'][:], pC[:, 256:384], sm['rstd'][:], f['zs'][:], ALU.mult, ALU.mult,
                  R=[kC] + MK('rstd') + FK('zs'), W=FK('osb'))
            P.transpose(pA[:, 128:256], f['osb'][:], ident[:], R=FK('osb') + ['ident'], W=[kA])
            P.copy('act', gobc[:, h, ss], pA[:, 128:256], R=[kA], W=['gobc'])

        for sb_ in range(4):
            if (c * CH + (sb_ + 1) * 128) <= PAD:
                continue
            ss = slice(sb_ * 128, (sb_ + 1) * 128)
            P.transpose(pb[1][:, 0:4], bg4[:, ss], ident[:4, :4], R=['bg4', 'ident'], W=[pk[1]])
            P.copy('act', tok4[:], pb[1][:, 0:4], R=[pk[1]], W=['tok4'])
            gens = [head_chain(0, sb_, ss), head_chain(1, sb_, ss)]
            while gens:
                for g in list(gens):
                    try:
                        next(g)
                    except StopIteration:
                        gens.remove(g)
        for h in range(2):
            P.dma(y_loc[c][h * 128:(h + 1) * 128, :], gobc[:, h, :], R=['gobc'], W=[('y_loc', c, h)], q='pool')
            ykeys.append(('y_loc', c, h))
        P.allgather(y_all[c], y_loc[c], groups, R=[('y_loc', c, k) for k in range(4)], W=[('y_all', c)])
    return ykeys


D = 2048
NE = 32
FF = 256
DN_ALPHA = (2.0 * 2) ** 0.25
LN_EPS = 1e-5
PAD_ = 496


def layer_norm_fm(P, v, N, gcol, bcol, pbA, pbB, kA, kB, ones_f, tmp, hb=None, vkey='v', hbkey='hb'):
    sq = tmp['sq']; mean = tmp['mean']; rstd = tmp['rstd']; m2 = tmp['m2']
    for dt in range(16):
        P.mm(pbA[:, :N], ones_f[:], v[:, dt, :N], start=(dt == 0), stop=(dt == 15), R=[(vkey, dt)], W=[kA])
    for dt in range(16):
        s = sq[dt % 2]
        P.act(s[:, :N], v[:, dt, :N], AF.Square, R=[(vkey, dt)], W=[('sq', dt % 2)])
        P.mm(pbB[:, :N], ones_f[:], s[:, :N], start=(dt == 0), stop=(dt == 15), R=[('sq', dt % 2)], W=[kB])
    P.act(mean[:, :N], pbA[:, :N], AF.Copy, scale=1.0 / D, R=[kA], W=['mean'])
    P.tt('dve', m2[:, :N], mean[:, :N], mean[:, :N], ALU.mult, R=['mean'], W=['m2'])
    P.ts('dve', rstd[:, :N], pbB[:, :N], 1.0 / D, ALU.mult, LN_EPS, ALU.add, R=[kB], W=['rstd'])
    P.tt('dve', rstd[:, :N], rstd[:, :N], m2[:, :N], ALU.subtract, R=['rstd', 'm2'], W=['rstd'])
    P.act(rstd[:, :N], rstd[:, :N], AF.Sqrt, R=['rstd'], W=['rstd'])
    P.op('dve', lambda e: e.reciprocal(out=rstd[:, :N], in_=rstd[:, :N]), R=['rstd'], W=['rstd'])
    for dt in range(16):
        eng = 'dve' if dt % 2 == 0 else 'pool'
        P.tt(eng, v[:, dt, :N], v[:, dt, :N], mean[:, :N], ALU.subtract, R=[(vkey, dt), 'mean'], W=[(vkey, dt)])
        P.tt(eng, v[:, dt, :N], v[:, dt, :N], rstd[:, :N], ALU.mult, R=[(vkey, dt), 'rstd'], W=[(vkey, dt)])
        P.ts(eng, v[:, dt, :N], v[:, dt, :N], gcol[:, dt:dt + 1], ALU.mult, bcol[:, dt:dt + 1], ALU.add,
             R=[(vkey, dt)], W=[(vkey, dt)])
        if hb is not None:
            P.copy('act', hb[:, dt, :N], v[:, dt, :N], R=[(vkey, dt)], W=[(hbkey, dt)])


def phase_post(P, pb, pk, glu, pre, y_all, hres, out32, outb, outb_all, groups, esel, NCH=6, N=342):
    T2 = NCH * N
    hT = hres
    outT = out32
    w_out = P.dram(pre + "w_out", [D, D])
    if glu:
        w_glu = P.dram(pre + "w_glu", [1024, 1024]); b_glu = P.dram(pre + "b_glu", [128, 8])
    lnp = P.dram(pre + "lnp", [128, 64])
    w_r = P.dram(pre + "w_r", [D, 36]); b_r = P.dram(pre + "b_r", [1, 36])
    w_gate = P.dram(pre + "w_gate", [NE, D, FF]); w_up = P.dram(pre + "w_up", [NE, D, FF]); w_down = P.dram(pre + "w_down", [NE, FF, D])
    c_ident = P.dram(pre + "c_ident", [128, 128]); c_sel = P.dram(pre + "c_sel", [32, NE * 128])

    ones_f = P.sb("ones_f", [128, 128]); ident = P.sb("ident", [128, 128]); selb = [P.sb("selb%d" % i, [32, 128]) for i in range(2)]
    lnp_sb = P.sb("lnp_sb", [128, 64]); wr_sb = P.sb("wr_sb", [128, 16, 36]); br_sb = P.sb("br_sb", [1, 36])
    if glu:
        bglu_sb = P.sb("bglu_sb", [128, 8])
    mst = P.sb("mst", [128, 8, N])
    yb = P.sb("yb", [128, 16, N], BF16)
    v = P.sb("v", [128, 16, N])
    hb = P.sb("hb", [128, 16, N], BF16)
    hh = P.sb("hh", [128, 2 * NE, N], BF16)
    hres = [P.sb("hres%d" % i, [128, N]) for i in range(2)]
    wst = [P.sb("wst%d" % i, [128, 8, 128]) for i in range(3)]
    wsm = [P.sb("wsm%d" % i, [128, 16, 128]) for i in range(1)]
    wsmb = [P.sb("wsmb%d" % i, [128, 16, 128], BF16) for i in range(2)]
    NWB = 3
    wb = [P.sb("wb%d" % i, [128, 16, 256], BF16) for i in range(NWB)]
    wdst = [P.sb("wdst%d" % i, [128, 1024]) for i in range(2)]
    NWD = 4
    wd = [P.sb("wd%d" % i, [128, 1, 1024], BF16) for i in range(NWD)]
    tmp = {'sq': [P.sb("sq%d" % i, [128, N]) for i in range(2)], 'mean': P.sb("mean", [128, N]),
           'rstd': P.sb("rstd", [128, N]), 'm2': P.sb("m2", [128, N])}
    sg = [P.sb("sg%d" % i, [128, N]) for i in range(2)]
    tq = [P.sb("tq%d" % i, [128, N]) for i in range(2)]
    combT = P.sb("combT", [32, N])
    rs = {k: P.sb("rs_" + k, [128, w]) for k, w in
          [('L', 36), ('gmax', 1), ('ngmax', 1), ('ohg', 4), ('eg', 4), ('se', 1), ('ptop', 1), ('lesel', 8),
           ('m1', 1), ('oh1', 8), ('msk', 8), ('m2', 1), ('oh2', 8), ('d', 1), ('ed', 1), ('w1', 1), ('w2', 1),
           ('ce', 8), ('comb', 32)]}
    idram = lambda n, sh: P.nc.dram_tensor(pre + n, sh, BF16, kind="Internal").ap()
    wc_gu = idram("wc_gu", [NE * 2, 128, 16 * 256]); wc_d = idram("wc_d", [2 * NE * 2, 128, 1024])
    wc_o = idram("wc_o", [16, 128, 16 * 128]); wc_g = idram("wc_g", [8, 128, 8 * 128])
    es_sb = P.sb("esel", [128, 4]); P.dma(es_sb[:], esel, W=['esel'])
    cand = [P.sb("cand%d" % i, [128, N], BF16) for i in range(4)]

    def select_into_mst(c, part):
        for j in range(8):
            row0 = (j // 2) * 512 + part + (j % 2) * 128
            for r in range(4):
                col0 = PAD_ + r * T2 + c * N
                pos = col0
                while pos < col0 + N:
                    yc = pos // 512
                    n = min(col0 + N - pos, (yc + 1) * 512 - pos)
                    P.dma(cand[r][:, pos - col0:pos - col0 + n], y_all[yc][row0:row0 + 128, pos - yc * 512:pos - yc * 512 + n],
                          W=[('cand', r)], q='sp')
                    pos += n
            P.ts('dve', mst[:, j, :], cand[0][:], es_sb[:, 0:1], ALU.mult, R=[('cand', 0), 'esel'], W=['mst'])
            for r in range(1, 4):
                P.stt('dve', mst[:, j, :], cand[r][:], es_sb[:, r:r + 1], mst[:, j, :], ALU.mult, ALU.add,
                      R=[('cand', r), 'esel', 'mst'], W=['mst'])

    P.memset('dve', ones_f[:], 1.0, W=['ones_f'])
    P.dma(ident[:], c_ident, W=['ident'])
    P.dma(lnp_sb[:], lnp, W=['lnp']); P.dma(br_sb[:], b_r, W=['br'])
    P.dma(wr_sb[:], w_r.rearrange("(t p) n -> p t n", p=128), W=['wr'])
    if glu:
        P.dma(bglu_sb[:], b_glu, W=['bglu'])

    cast_rr = [0]

    def cast(out, in_, R, W):
        eng = ('act', 'pool')[cast_rr[0] % 2]
        cast_rr[0] += 1
        P.copy(eng, out, in_, R=R, W=W)

    wsm_i = [0]

    def load_coltile(wdram, ktiles, col0, c, cache):
        i = wsm_i[0] % 2
        wsm_i[0] += 1
        cv = cache.rearrange("p (t n) -> p t n", t=ktiles)
        ck = ('wcache', cache.tensor.name, col0)
        if c == 0:
            P.dma(wsm[0][:, :ktiles, :], wdram[:, col0:col0 + 128].rearrange("(t p) n -> p t n", p=128),
                  W=[('wsm', 0)], q='sp')
            cast(wsmb[i][:, :ktiles, :], wsm[0][:, :ktiles, :], R=[('wsm', 0)], W=[('wsmb', i)])
            P.dma(cv, wsmb[i][:, :ktiles, :], R=[('wsmb', i)], W=[ck], q='pool')
        else:
            P.dma(wsmb[i][:, :ktiles, :], cv, R=[ck], W=[('wsmb', i)], q='sp')
        return wsmb[i], ('wsmb', i)

    for c in range(NCH):
        cs = slice(c * N, (c + 1) * N)
        select_into_mst(c, 0)
        if glu:
            for j in range(8):
                P.copy('act', yb[:, 8 + j, :], mst[:, j, :], R=['mst'], W=[('yb', 8 + j)])
            for j in range(8):
                wt, wk = load_coltile(w_glu, 8, j * 128, c, wc_g[j])
                for i in range(8):
                    P.mm(pb[6][:, :N], wt[:, i, :], yb[:, 8 + i, :], start=(i == 0), stop=(i == 7),
                         R=[wk, ('yb', 8 + i)], W=[pk[6]])
                P.act(sg[j % 2][:, :], pb[6][:, :N], AF.Sigmoid, bias=bglu_sb[:, j:j + 1], R=[pk[6], 'bglu'], W=[('sg', j % 2)])
                P.tt('dve', yb[:, j, :], mst[:, j, :], sg[j % 2][:, :], ALU.mult, R=['mst', ('sg', j % 2)], W=[('yb', j)])
        else:
            for j in range(8):
                P.copy('act', yb[:, j, :], mst[:, j, :], R=['mst'], W=[('yb', j)])
        select_into_mst(c, 256)
        for j in range(8):
            P.copy('act', yb[:, 8 + j, :], mst[:, j, :], R=['mst'], W=[('yb', 8 + j)])
        for dt in range(16):
            wt, wk = load_coltile(w_out, 16, dt * 128, c, wc_o[dt])
            hr = hres[dt % 2]
            P.dma(hr[:], hT[dt * 128:(dt + 1) * 128, cs], W=[('hres', dt % 2)], q='pool')
            bank = 6 + dt % 2
            for i in range(16):
                P.mm(pb[bank][:, :N], wt[:, i, :], yb[:, i, :], start=(i == 0), stop=(i == 15),
                     R=[wk, ('yb', i)], W=[pk[bank]])
            P.stt('dve', v[:, dt, :], hr[:], DN_ALPHA, pb[bank][:, :N], ALU.mult, ALU.add,
                  R=[('hres', dt % 2), pk[bank]], W=[('v', dt)])
        layer_norm_fm(P, v, N, lnp_sb[:, 0:16], lnp_sb[:, 16:32], pb[6], pb[7], pk[6], pk[7], ones_f, tmp, hb=hb)
        t0 = 0
        while t0 < N:
            M = min(128, N - t0)
            for dt in range(16):
                P.mm(pb[6][:M, :36], v[:, dt, t0:t0 + M], wr_sb[:, dt, :], start=(dt == 0), stop=False,
                     R=[('v', dt), 'wr'], W=[pk[6]])
            P.mm(pb[6][:M, :36], ones_f[0:1, :M], br_sb[:, :], start=False, stop=True, R=['ones_f', 'br'], W=[pk[6]])
            r = {k: t[:M, :] for k, t in rs.items()}
            K = lambda *names: ['rs_' + n for n in names]
            P.copy('dve', r['L'], pb[6][:M, :36], R=[pk[6]], W=K('L'))
            P.op('dve', lambda e, r=r: e.reduce_max(out=r['gmax'], in_=r['L'][:, 0:4], axis=AX.X), R=K('L'), W=K('gmax'))
            P.ts('dve', r['ohg'], r['L'][:, 0:4], r['gmax'], ALU.is_equal, R=K('L', 'gmax'), W=K('ohg'))
            P.ts('dve', r['ngmax'], r['gmax'], -1.0, ALU.mult, R=K('gmax'), W=K('ngmax'))
            P.act(r['eg'], r['L'][:, 0:4], AF.Exp, bias=r['ngmax'], R=K('L', 'ngmax'), W=K('eg'))
            P.op('dve', lambda e, r=r: e.reduce_sum(out=r['se'], in_=r['eg'], axis=AX.X), R=K('eg'), W=K('se'))
            P.op('dve', lambda e, r=r: e.reciprocal(out=r['ptop'], in_=r['se']), R=K('se'), W=K('ptop'))
            P.ts('dve', r['lesel'], r['L'][:, 4:12], r['ohg'][:, 0:1], ALU.mult, R=K('L', 'ohg'), W=K('lesel'))
            for g in range(1, 4):
                P.stt('dve', r['lesel'], r['L'][:, 4 + 8 * g:12 + 8 * g], r['ohg'][:, g:g + 1], r['lesel'],
                      ALU.mult, ALU.add, R=K('L', 'ohg', 'lesel'), W=K('lesel'))
            P.op('dve', lambda e, r=r: e.reduce_max(out=r['m1'], in_=r['lesel'], axis=AX.X), R=K('lesel'), W=K('m1'))
            P.ts('dve', r['oh1'], r['lesel'], r['m1'], ALU.is_equal, R=K('lesel', 'm1'), W=K('oh1'))
            P.stt('dve', r['msk'], r['oh1'], -1e30, r['lesel'], ALU.mult, ALU.add, R=K('oh1', 'lesel'), W=K('msk'))
            P.op('dve', lambda e, r=r: e.reduce_max(out=r['m2'], in_=r['msk'], axis=AX.X), R=K('msk'), W=K('m2'))
            P.ts('dve', r['oh2'], r['msk'], r['m2'], ALU.is_equal, R=K('msk', 'm2'), W=K('oh2'))
            P.tt('dve', r['d'], r['m2'], r['m1'], ALU.subtract, R=K('m1', 'm2'), W=K('d'))
            P.act(r['ed'], r['d'], AF.Exp, R=K('d'), W=K('ed'))
            P.ts('dve', r['w1'], r['ed'], 1.0, ALU.add, R=K('ed'), W=K('w1'))
            P.op('dve', lambda e, r=r: e.reciprocal(out=r['w1'], in_=r['w1']), R=K('w1'), W=K('w1'))
            P.tt('dve', r['w2'], r['ed'], r['w1'], ALU.mult, R=K('ed', 'w1'), W=K('w2'))
            P.tt('dve', r['w1'], r['w1'], r['ptop'], ALU.mult, R=K('w1', 'ptop'), W=K('w1'))
            P.tt('dve', r['w2'], r['w2'], r['ptop'], ALU.mult, R=K('w2', 'ptop'), W=K('w2'))
            P.ts('dve', r['ce'], r['oh1'], r['w1'], ALU.mult, R=K('oh1', 'w1'), W=K('ce'))
            P.stt('dve', r['ce'], r['oh2'], r['w2'], r['ce'], ALU.mult, ALU.add, R=K('oh2', 'w2', 'ce'), W=K('ce'))
            for g in range(4):
                P.ts('dve', r['comb'][:, 8 * g:8 * g + 8], r['ce'], r['ohg'][:, g:g + 1], ALU.mult,
                     R=K('ce', 'ohg', 'comb'), W=K('comb'))
            P.transpose(pb[7][:32, :M], r['comb'], ident[:M, :M], R=K('comb') + ['ident'], W=[pk[7]])
            P.copy('dve', combT[:, t0:t0 + M], pb[7][:32, :M], R=[pk[7], 'combT'], W=['combT'])
            t0 += M
        wst_i = 0
        for e in range(NE):
            cbk = pk[4 + e % 2]; cbp = pb[4 + e % 2]
            P.dma(selb[e % 2][:], c_sel[:, e * 128:(e + 1) * 128], W=[('selb', e % 2)], q='pool')
            P.mm(cbp[:, :N], selb[e % 2][:], combT[:, :], R=[('selb', e % 2), 'combT'], W=[cbk])
            for f in range(2):
                bi = (2 * e + f) % 2
                wi = (2 * e + f) % NWB
                wbt = wb[wi]; wbk = ('wb', wi)
                cgu = wc_gu[2 * e + f].rearrange("p (t n) -> p t n", t=16)
                if c == 0:
                    for gi, wsrc in enumerate((w_gate, w_up)):
                        for q2 in range(2):
                            st = wst[wst_i % 3]; sk = ('wst', wst_i % 3); wst_i += 1
                            P.dma(st[:], wsrc[e, q2 * 1024:(q2 + 1) * 1024, f * 128:(f + 1) * 128].rearrange(
                                "(t p) n -> p t n", p=128), W=[sk], q='sp')
                            cast(wbt[:, q2 * 8:(q2 + 1) * 8, gi * 128:(gi + 1) * 128], st[:], R=[sk, wbk], W=[wbk])
                    P.dma(cgu, wbt[:], R=[wbk], W=[('wc_gu', 2 * e + f)], q='pool')
                else:
                    P.dma(wbt[:], cgu, R=[('wc_gu', 2 * e + f)], W=[wbk], q='sp')
                pg = pb[bi]; pu = pb[2 + bi]
                for dt in range(16):
                    P.mm(pg[:, :N], wbt[:, dt, 0:128], hb[:, dt, :], start=(dt == 0), stop=(dt == 15),
                         R=[wbk, ('hb', dt)], W=[pk[bi]])
                for dt in range(16):
                    P.mm(pu[:, :N], wbt[:, dt, 128:256], hb[:, dt, :], start=(dt == 0),
                         stop=(dt == 15), R=[wbk, ('hb', dt)], W=[pk[2 + bi]])
                P.act(sg[bi][:, :], pg[:, :N], AF.Silu, R=[pk[bi]], W=[('sg', bi)])
                P.tt('dve', tq[bi][:, :], pu[:, :N], sg[bi][:, :], ALU.mult, R=[pk[2 + bi], ('sg', bi)], W=[('tq', bi)])
                P.tt('dve', hh[:, 2 * e + f, :], tq[bi][:, :], cbp[:, :N], ALU.mult, R=[('tq', bi), cbk],
                     W=[('hh', 2 * e + f)])
        for half in range(2):
            for e in range(NE):
                for ft in range(2):
                    i = (2 * e + ft) % 2
                    ci = (half * NE + e) * 2 + ft
                    wi = (2 * e + ft) % NWD
                    if c == 0:
                        P.dma(wdst[i][:], w_down[e, ft * 128:(ft + 1) * 128, half * 1024:(half + 1) * 1024],
                              W=[('wdst', i)], q='sp')
                        cast(wd[wi][:, 0, :], wdst[i][:], R=[('wdst', i)], W=[('wd', wi)])
                        P.dma(wc_d[ci], wd[wi][:, 0, :], R=[('wd', wi)], W=[('wc_d', ci)], q='pool')
                    else:
                        P.dma(wd[wi][:, 0, :], wc_d[ci], R=[('wc_d', ci)], W=[('wd', wi)], q='sp')
                    for j in range(8):
                        P.mm(pb[j][:, :N], wd[wi][:, 0, j * 128:(j + 1) * 128], hh[:, 2 * e + ft, :],
                             start=(e == 0 and ft == 0), stop=(e == NE - 1 and ft == 1),
                             R=[('wd', wi), ('hh', 2 * e + ft)], W=[pk[j]])
            for j in range(8):
                dt = half * 8 + j
                P.stt('dve', v[:, dt, :], v[:, dt, :], DN_ALPHA, pb[j][:, :N], ALU.mult, ALU.add,
                      R=[('v', dt), pk[j]], W=[('v', dt)])
        layer_norm_fm(P, v, N, lnp_sb[:, 32:48], lnp_sb[:, 48:64], pb[6], pb[7], pk[6], pk[7], ones_f, tmp, hb=None)
        P.dma(outT[:, cs].rearrange("(t p) n -> p t n", p=128), v[:], R=[('v', dt) for dt in range(16)], W=[('out32', c)], q='pool')
        if outb is not None:
            for dt in range(16):
                P.copy('act', hb[:, dt, :], v[:, dt, :], R=[('v', dt)], W=[('hb', dt)])
            for half in range(2):
                P.dma(outb[c][half].rearrange("(t p) n -> p t n", p=128), hb[:, 8 * half:8 * half + 8, :],
                      R=[('hb', dt) for dt in range(16)], W=[('outb', c, half)], q='pool')
                P.allgather(outb_all[c][half], outb[c][half], groups, R=[('outb', c, half)], W=[('outb_all', c, half)])

import numpy as np
PAD = 496

def c_consts():
    U = np.triu(np.ones((128, 128), np.float32))
    return {"c_U": U, "c_ident": np.eye(128, dtype=np.float32)}

def prep_ab(inp, j):
    w = inp['ab_w_in'][0]
    cols = np.concatenate([np.arange(256 * j, 256 * j + 256), 1024 + np.arange(128 * j, 128 * j + 128),
                           1536 + np.arange(128 * j, 128 * j + 128), 2048 + np.arange(256 * j, 256 * j + 256),
                           3088 + np.arange(256 * j, 256 * j + 256), np.arange(3072, 3088)])
    d = {"w_in": np.ascontiguousarray(w[:, cols])}
    a_re = inp['ab_s5_a_re'][0]; a_im = inp['ab_s5_a_im'][0]; ldt = inp['ab_s5_log_dt'][0]
    B = [inp['ab_s5_b_re'][0], inp['ab_s5_b_im'][0]]; C = [inp['ab_s5_c_re'][0], inp['ab_s5_c_im'][0]]
    par = np.zeros((128, 8, 3), np.float32)
    BT = np.zeros((128, 2, 8, 128), np.float32); CT = np.zeros((128, 2, 8, 128), np.float32)
    for st in range(8):
        for g2 in range(2):
            g = 16 * j + 2 * st + g2
            gl = (2 * st + g2) % 8
            ps = slice(g2 * 64, g2 * 64 + 64)
            par[ps, st, 0] = a_re[g]; par[ps, st, 1] = a_im[g]; par[ps, st, 2] = ldt[g]
            for ri in range(2):
                BT[gl * 16:(gl + 1) * 16, ri, st, ps] = B[ri][g].T
                CT[ps, ri, st, gl * 16:(gl + 1) * 16] = C[ri][g].T
    d["s5par"] = par; d["s5BT"] = BT; d["s5CT"] = CT
    d["s5d"] = np.ascontiguousarray(inp['ab_s5_d'][0][256 * j:256 * j + 256].reshape(2, 128).T)
    d["gla_wg"] = np.ascontiguousarray(inp['ab_gla_w_gate'][0][:, 128 * j:128 * j + 128])
    d["gla_bg"] = np.ascontiguousarray(inp['ab_gla_b_gate'][0][128 * j:128 * j + 128, None])
    d["gla_ng"] = np.ascontiguousarray(np.broadcast_to(inp['ab_gla_norm'][0][None, :], (128, 256)))
    d.update(c_consts())
    return d

def prep_hT(h, Tp):
    L = h.shape[0]
    out = np.zeros((h.shape[1], Tp), np.float32)
    out[:, Tp - L:] = h.T
    return out

def prep_cd(inp, j):
    w = inp['cd_w_in'][0]
    hs = [2 * j, 2 * j + 1]
    blk = lambda base, i: base + np.arange(i * 128, i * 128 + 128)
    tiles = [blk(0, hs[0]), blk(0, hs[1]), blk(1024, hs[0]), blk(1024, hs[1]), blk(2048, hs[0]), blk(2048, hs[1]),
             blk(4112, hs[0]), blk(4112, hs[1]), blk(3072, hs[0]), blk(3072, hs[1]), blk(5136, hs[0]), blk(5136, hs[1]),
             np.array([4096 + hs[0], 4096 + hs[1], 4104 + hs[0], 4104 + hs[1]])]
    d = {"w_in": np.ascontiguousarray(w[:, np.concatenate(tiles)])}
    cw = np.zeros((128, 8, 4), np.float32); cb = np.zeros((128, 8), np.float32)
    for i in range(6):
        cw[:, i, :] = inp['cd_conv_w'][0][:, tiles[i]].T
    for b in range(2):
        cw[:, 6 + b, :] = inp['cd_lru_conv_w'][0][:, blk(0, hs[b])].T
        cb[:, 6 + b] = inp['cd_lru_conv_b'][0][blk(0, hs[b])]
    d["convw"] = cw; d["convb"] = cb
    gp = np.zeros((4, 4), np.float32)
    for h in range(2):
        gp[2 + h, 0] = inp['cd_gdn_dt_bias'][0][hs[h]]; gp[2 + h, 1] = inp['cd_gdn_a_log'][0][hs[h]]
    gp[0:2, 2] = 1.0; gp[2:4, 3] = 1.0
    d["gpar"] = gp
    d["gdn_ng"] = np.ascontiguousarray(np.broadcast_to(inp['cd_gdn_norm'][0][None, :], (128, 128)))
    d["lwa"] = np.ascontiguousarray(np.stack([inp['cd_lru_w_a'][0][hs[b]] for b in range(2)], 1))
    d["lwx"] = np.ascontiguousarray(np.stack([inp['cd_lru_w_x'][0][hs[b]] for b in range(2)], 1))
    lp = np.zeros((128, 2, 3), np.float32)
    for b in range(2):
        lp[:, b, 0] = inp['cd_lru_b_a'][0][blk(0, hs[b])]; lp[:, b, 1] = inp['cd_lru_b_x'][0][blk(0, hs[b])]
        lp[:, b, 2] = inp['cd_lru_lambda'][0][blk(0, hs[b])]
    d["lpar"] = lp
    d.update(c_consts())
    d["c_Us"] = np.triu(np.ones((128, 128), np.float32), 1)
    sel4 = np.zeros((4, 512), np.float32)
    for r in range(4):
        sel4[r, r * 128:(r + 1) * 128] = 1.0
    d["c_sel4"] = sel4
    return d


N_META = 16
SEQ = 8192
NCHUNK = 17
TP = NCHUNK * CH
POST_NCH, POST_N = 6, 342
T2 = POST_NCH * POST_N
G4 = [[0, 1, 2, 3], [4, 5, 6, 7]]


def build_fused():
    P = Prog()
    nc = P.nc
    pb = [P.ps("pb%d" % i, [128, 512]) for i in range(8)]
    pk = ['pb%d' % i for i in range(8)]
    hT = P.dram("hT", [D, TP]); hq = P.dram("hq", [D, T2]); esel = P.dram("esel", [128, 4])
    outT = P.dram("outT", [D, T2], kind="ExternalOutput")
    idram = lambda n, sh, dt: nc.dram_tensor(n, sh, dt, kind="Internal").ap()
    y0_loc = [idram("y0_loc%d" % c, [512, CH], BF16) for c in range(NCHUNK)]
    y0_all = [idram("y0_all%d" % c, [4 * 512, CH], BF16) for c in range(NCHUNK)]
    y1_loc = [idram("y1_loc%d" % c, [512, CH], BF16) for c in range(NCHUNK)]
    y1_all = [idram("y1_all%d" % c, [4 * 512, CH], BF16) for c in range(NCHUNK)]
    h1_32 = idram("h1_32", [D, T2], F32)
    h1_b = [[idram("h1_b%d_%d" % (c, h), [1024, POST_N], BF16) for h in range(2)] for c in range(POST_NCH)]
    h1_all = [[idram("h1_all%d_%d" % (c, h), [4 * 1024, POST_N], BF16) for h in range(2)] for c in range(POST_NCH)]

    with P.scope():
        phase_ab(P, pb, pk, NCHUNK, hT, y0_loc, y0_all, G4)
    P.new_phase()
    with P.scope():
        phase_post(P, pb, pk, True, "p0_", y0_all, hq, h1_32, h1_b, h1_all, G4, esel, POST_NCH, POST_N)
    P.new_phase()
    with P.scope():
        phase_cd(P, pb, pk, NCHUNK, h1_all, y1_loc, y1_all, G4)
    P.new_phase()
    with P.scope():
        phase_post(P, pb, pk, False, "p1_", y1_all, h1_32, outT, None, None, G4, esel, POST_NCH, POST_N)
    return P.finalize(), P


def _post_weights(inp, layer, glu, pre):
    pt = lambda a: np.ascontiguousarray(a.reshape(-1, 128).T)
    d = {"w_out": (inp['ab_w_out'][0] if glu else inp['cd_w_out'][0]),
         "lnp": np.concatenate([pt(inp['ln_mix_g'][layer]), pt(inp['ln_mix_b'][layer]),
                                pt(inp['ln_ffn_g'][layer]), pt(inp['ln_ffn_b'][layer])], 1),
         "w_r": np.ascontiguousarray(np.concatenate([inp['moe_w_router_g'][layer],
                                                     inp['moe_w_router_e'][layer].reshape(D, 32)], 1)),
         "b_r": np.concatenate([inp['moe_b_router_g'][layer], inp['moe_b_router_e'][layer].reshape(32)])[None],
         "w_gate": inp['moe_w_gate'][layer].reshape(32, D, FF), "w_up": inp['moe_w_up'][layer].reshape(32, D, FF),
         "w_down": inp['moe_w_down'][layer].reshape(32, FF, D),
         "c_ident": np.eye(128, dtype=np.float32),
         "c_sel": np.ascontiguousarray(np.repeat(np.eye(32, dtype=np.float32), 128, axis=1))}
    if glu:
        d["w_glu"] = inp['ab_s5_w_glu'][0]
        d["b_glu"] = pt(inp['ab_s5_b_glu'][0])
    return {pre + k: v for k, v in d.items()}


def kernel(**inputs):
    inp = {k: np.asarray(v, dtype=np.float32) for k, v in inputs.items()}
    x = inp['x']
    B = x.shape[0]
    L = N_META + SEQ
    h0 = np.concatenate([np.broadcast_to(inp['meta_tokens'][None], (B, N_META, D)), x], axis=1)
    hTs = [prep_hT(h0[b], TP) for b in range(B)]
    pw0 = _post_weights(inp, 0, True, "p0_"); pw1 = _post_weights(inp, 1, False, "p1_")
    ab = [{"a_" + k: v for k, v in prep_ab(inp, j).items()} for j in range(4)]
    cd = [{"c_" + k: v for k, v in prep_cd(inp, j).items()} for j in range(4)]
    maps = []
    for i in range(8):
        b, j = i // 4, i % 4
        d = {"hT": hTs[b], "hq": np.ascontiguousarray(h0[b, j * T2:(j + 1) * T2].T)}
        es = np.zeros((128, 4), np.float32); es[:, j] = 1.0
        d["esel"] = es
        d.update(ab[j]); d.update(cd[j]); d.update(pw0); d.update(pw1)
        maps.append(d)
    nc, _ = build_fused()
    res = run_bass_kernel_spmd(nc, maps, core_ids=list(range(8)))
    out = np.zeros((B, L, D), np.float32)
    for i in range(8):
        b, j = i // 4, i % 4
        out[b, j * T2:(j + 1) * T2] = np.asarray(res.results[i]["outT"]).T
    return np.ascontiguousarray(out[:, N_META:])
```

```python
import numpy as np
import os
from contextlib import ExitStack
import concourse.bass as bass
import concourse.mybir as mybir
from concourse.bass_utils import run_bass_kernel_spmd

F32 = mybir.dt.float32
BF16 = mybir.dt.bfloat16
ALU = mybir.AluOpType
AF = mybir.ActivationFunctionType
AX = mybir.AxisListType

ENGS = ['pe', 'act', 'dve', 'pool', 'sp']
N_DMA_SEMS = 12
SAME_ENGINE_SYNC = True


class Prog:
    def __init__(self):
        self.nc = bass.Bass("TRN2", target_bir_lowering=False)
        self.es = ExitStack()
        self.ops = {e: [] for e in ENGS}
        self.cnt = {e: 0 for e in ENGS}
        self.seen = {e: {} for e in ENGS}
        self.last_w = {}
        self.readers = {}
        self.dma_cnt = [0] * N_DMA_SEMS
        self.dma_rr = 0
        self.sems = {}
        self.n_ops = 0
        self.phase = 0
        self.barrier_toks = {e: [] for e in ENGS}
        self.scopes = []
        self.cc_cnt = 0
        self._alloc_sems()

    def _alloc_sems(self):
        p = self.phase
        for e in ENGS:
            self.sems[(e, p)] = self.es.enter_context(self.nc.semaphore("s_%s_%d" % (e, p)))
        for j in range(N_DMA_SEMS):
            self.sems[(('dma', j), p)] = self.es.enter_context(self.nc.semaphore("s_dma%d_%d" % (j, p)))

    def new_phase(self):
        p = self.phase
        toks = [((e, p), self.cnt[e]) for e in ENGS if self.cnt[e] > 0]
        toks += [((('dma', j), p), self.dma_cnt[j]) for j in range(N_DMA_SEMS) if self.dma_cnt[j] > 0]
        if self.cc_cnt > 0:
            toks.append((('cc', 0), self.cc_cnt))
        for e in ENGS:
            self.barrier_toks[e] = self.barrier_toks[e] + toks
        self.phase += 1
        self._alloc_sems()
        self.cnt = {e: 0 for e in ENGS}
        self.dma_cnt = [0] * N_DMA_SEMS
        self.last_w = {}
        self.readers = {}

    def scope(self):
        prog = self

        class _S:
            def __enter__(self_):
                prog.scopes.append(ExitStack())

            def __exit__(self_, *a):
                prog.scopes.pop().close()
                return False
        return _S()

    def sb(self, name, shape, dt=F32):
        es = self.scopes[-1] if self.scopes else self.es
        return es.enter_context(self.nc.sbuf_tensor("p%d_%s" % (self.phase, name), list(shape), dt))

    def ps(self, name, shape, dt=F32):
        return self.es.enter_context(self.nc.psum_tensor(name, list(shape), dt))

    def dram(self, name, shape, dt=F32, kind="ExternalInput"):
        return self.nc.dram_tensor(name, list(shape), dt, kind=kind).ap()

    def tok(self, name, val):
        return ((name, self.phase), val)

    def _deps(self, R, W):
        toks = []
        for k in R:
            if k in self.last_w:
                toks.append(self.last_w[k])
        for k in W:
            if k in self.last_w:
                toks.append(self.last_w[k])
            for t in self.readers.get(k, {}).items():
                toks.append(t)
        return toks

    def _mark(self, tok, R, W):
        for k in R:
            self.readers.setdefault(k, {})[tok[0]] = tok[1]
        for k in W:
            self.last_w[k] = tok
            self.readers[k] = {}

    def _waits(self, eng, toks):
        need = {}
        for s, v in toks:
            if s[0] == eng and (eng in ('pe', 'sp') or not SAME_ENGINE_SYNC):
                continue
            if self.seen[eng].get(s, 0) >= v:
                continue
            need[s] = max(need.get(s, 0), v)
        for s, v in need.items():
            self.seen[eng][s] = v
        return list(need.items())

    @staticmethod
    def _excl(R, W):
        ps = [k for k in R if isinstance(k, str) and k.startswith('pb')]
        if ps:
            R = [k for k in R if k not in ps]
            W = list(W) + ps
        return R, W

    def _bar(self, eng):
        b = self.barrier_toks[eng]
        self.barrier_toks[eng] = []
        return b

    def op(self, eng, fn, R=(), W=()):
        R, W = self._excl(R, W)
        waits = self._waits(eng, self._deps(R, W) + self._bar(eng))
        self.cnt[eng] += 1
        tok = self.tok(eng, self.cnt[eng])
        self.ops[eng].append((waits, fn, ((eng, self.phase), 1)))
        self._mark(tok, R, W)
        self.n_ops += 1

    def dma(self, out, in_, R=(), W=(), q='sp', **kw):
        if q == 'pool' and os.environ.get('NO_SWDGE'):
            q = 'sp'
        j = self.dma_rr
        self.dma_rr = (self.dma_rr + 1) % N_DMA_SEMS
        toks = self._deps(R, W) + self._bar(q)
        if self.dma_cnt[j] > 0:
            toks.append(self.tok(('dma', j), self.dma_cnt[j]))
        waits = self._waits(q, toks)
        self.dma_cnt[j] += 16
        tok = self.tok(('dma', j), self.dma_cnt[j])
        self.ops[q].append((waits, lambda e: e.dma_start(out=out, in_=in_, **kw), ((('dma', j), self.phase), 16)))
        self._mark(tok, R, W)
        self.n_ops += 1

    def allgather(self, out, in_, groups, R=(), W=()):
        if ('cc', 0) not in self.sems:
            self.sems[('cc', 0)] = self.es.enter_context(self.nc.semaphore("s_cc"))
        toks = self._deps(R, W) + self._bar('pool')
        waits = self._waits('pool', toks)
        self.cc_cnt += 1
        tok = (('cc', 0), self.cc_cnt)
        self.ops['pool'].append((waits, lambda e: e.collective_compute(
            "AllGather", ALU.bypass, replica_groups=groups, ins=[in_], outs=[out]), (('cc', 0), None)))
        self._mark(tok, R, W)
        self.n_ops += 1

    def mm(self, out, lhsT, rhs, start=True, stop=True, R=(), W=()):
        self.op('pe', lambda e: e.matmul(out, lhsT, rhs, start=start, stop=stop), R, W)

    def transpose(self, out, in_, ident, R=(), W=()):
        self.op('pe', lambda e: e.transpose(out, in_, ident), R, W)

    def act(self, out, in_, func, bias=None, scale=1.0, R=(), W=(), accum_out=None):
        kw = {}
        if bias is not None:
            kw['bias'] = bias
        if accum_out is not None:
            kw['accum_out'] = accum_out
        self.op('act', lambda e: e.activation(out=out, in_=in_, func=func, scale=scale, **kw), R, W)

    def copy(self, eng, out, in_, R=(), W=()):
        if eng == 'act':
            self.op('act', lambda e: e.copy(out=out, in_=in_), R, W)
        else:
            self.op(eng, lambda e: e.tensor_copy(out=out, in_=in_), R, W)

    def tt(self, eng, out, a, b, op, R=(), W=()):
        self.op(eng, lambda e: e.tensor_tensor(out=out, in0=a, in1=b, op=op), R, W)

    def ts(self, eng, out, a, s1, op0, s2=None, op1=None, R=(), W=()):
        if op1 is None:
            self.op(eng, lambda e: e.tensor_scalar(out=out, in0=a, scalar1=s1, scalar2=None, op0=op0), R, W)
        else:
            self.op(eng, lambda e: e.tensor_scalar(out=out, in0=a, scalar1=s1, scalar2=s2, op0=op0, op1=op1), R, W)

    def stt(self, eng, out, a, s, b, op0, op1, R=(), W=()):
        eng = 'dve'
        self.op(eng, lambda e: e.scalar_tensor_tensor(out=out, in0=a, scalar=s, in1=b, op0=op0, op1=op1), R, W)

    def memset(self, eng, ap, val, W=()):
        self.op(eng, lambda e: e.memset(ap, val), (), W)

    def finalize(self):
        nc = self.nc
        fin = list(self.barrier_toks['sp'])
        for j in range(N_DMA_SEMS):
            if self.dma_cnt[j] > 0:
                fin.append(self.tok(('dma', j), self.dma_cnt[j]))
        for e in ENGS:
            if e != 'sp' and self.cnt[e] > 0:
                fin.append(self.tok(e, self.cnt[e]))
        if self.cc_cnt > 0:
            fin.append((('cc', 0), self.cc_cnt))
        ops = self.ops
        sems = self.sems

        def run(engobj, name):
            for waits, fn, (s, inc) in ops[name]:
                for ws, wv in waits:
                    engobj.wait_ge(sems[ws], wv)
                ins = fn(engobj)
                if inc is None:
                    ins.then_inc(sems[s])
                else:
                    ins.then_inc(sems[s], inc)
            if name == 'sp':
                for ws, wv in fin:
                    engobj.wait_ge(sems[ws], wv)

        with nc.Block() as block:
            @block.sync
            def _(e):
                run(e, 'sp')

            if ops['pe']:
                @block.tensor
                def _(e):
                    run(e, 'pe')
            if ops['act']:
                @block.scalar
                def _(e):
                    run(e, 'act')
            if ops['dve']:
                @block.vector
                def _(e):
                    run(e, 'dve')
            if ops['pool']:
                @block.gpsimd
                def _(e):
                    run(e, 'pool')
        self.es.close()
        return nc

import math

D = 2048
CH = 512
PAD = 496


def gelu_tanh(P, out, x, tA, tB, N, kx, kout, ktmp):
    P.tt('pool', tA[:, :N], x, x, ALU.mult, R=[kx], W=[ktmp[0]])
    P.ts('pool', tA[:, :N], tA[:, :N], 0.044715, ALU.mult, 1.0, ALU.add, R=[ktmp[0]], W=[ktmp[0]])
    P.tt('pool', tA[:, :N], tA[:, :N], x, ALU.mult, R=[ktmp[0], kx], W=[ktmp[0]])
    P.act(tB[:, :N], tA[:, :N], AF.Sigmoid, scale=1.5957691216057308, R=[ktmp[0]], W=[ktmp[1]])
    P.tt('pool', out, x, tB[:, :N], ALU.mult, R=[kx, ktmp[1]], W=[kout])


def inproj_setup(P, w_in, ncols, cast_engs=('act', 'pool')):
    wb = P.sb("win_b", [128, 16, ncols], BF16)
    stg = [P.sb("win_st%d" % i, [128, ncols]) for i in range(2)]
    for dt in range(16):
        i = dt % 2
        P.dma(stg[i][:], w_in[dt * 128:(dt + 1) * 128, :], W=[('win_st', i)])
        P.copy(cast_engs[dt % 2], wb[:, dt, :], stg[i][:], R=[('win_st', i)], W=['win_b'])
    return wb


def load_x_chunk(P, hT, c, xst, xb):
    for dt in range(16):
        i = dt % 4
        P.dma(xst[i][:], hT[dt * 128:(dt + 1) * 128, c * CH:(c + 1) * CH], W=[('xst', i)], q='sp')
        P.copy(('act', 'pool')[dt % 2], xb[:, dt, :], xst[i][:], R=[('xst', i)], W=[('xb', dt)])


def proj_tile(P, wb, col0, M, xb, ps, pskey):
    for dt in range(16):
        P.mm(ps[:M, :CH], wb[:, dt, col0:col0 + M], xb[:, dt, :], start=(dt == 0), stop=(dt == 15),
             R=['win_b', ('xb', dt)], W=[pskey])


def phase_ab(P, pb, pk, NCHUNK, hT, y_loc, y_all, groups, pre="a_"):
    Tp = NCHUNK * CH
    NCOL = 1040
    w_in = P.dram(pre + "w_in", [D, NCOL])
    s5par = P.dram(pre + "s5par", [128, 8, 3])
    s5BT = P.dram(pre + "s5BT", [128, 2, 8, 128])
    s5CT = P.dram(pre + "s5CT", [128, 2, 8, 128])
    s5d = P.dram(pre + "s5d", [128, 2])
    gla_wg = P.dram(pre + "gla_wg", [16, 128]); gla_bg = P.dram(pre + "gla_bg", [128, 1]); gla_ng = P.dram(pre + "gla_ng", [128, 256])
    c_U = P.dram(pre + "c_U", [128, 128]); c_ident = P.dram(pre + "c_ident", [128, 128])
    ykeys = []
    wb = inproj_setup(P, w_in, NCOL)
    xst = [P.sb("xst%d" % i, [128, CH]) for i in range(4)]
    xb = P.sb("xb", [128, 16, CH], BF16)
    names = ['u0', 'u1', 'q', 'k', 'v0', 'v1', 'r0', 'r1']
    pt = {n: P.sb("pt_" + n, [128, CH]) for n in names}
    glT = P.sb("glT", [16, CH])
    U = P.sb("U", [128, 128]); ident = P.sb("ident", [128, 128]); identb = None
    onescol = P.sb("onescol", [128, 1])
    P.dma(U[:], c_U, W=['U']); P.dma(ident[:], c_ident, W=['ident'])
    P.memset('dve', onescol[:], 1.0, W=['onescol'])

    par = P.sb("s5par_sb", [128, 8, 3]); P.dma(par[:], s5par, W=['par'])
    BTb = P.sb("BTb", [128, 2, 8, 128], BF16); CTb = P.sb("CTb", [128, 2, 8, 128], BF16)
    stg128 = [P.sb("stg128_%d" % i, [128, 128]) for i in range(4)]
    dcol = P.sb("dcol", [128, 2]); P.dma(dcol[:], s5d, W=['dcol'])
    Er = P.sb("Er", [128, 8, CH]); Ei = P.sb("Ei", [128, 8, CH])
    sc = {k: P.sb("sc_" + k, [128, 8]) for k in
          ['dt', 'th', 'r', 'c', 's', 'c2', 's2', 't', 'cr', 'ci', 'nr', 'ni', 'den', 'zr', 'zi', 'x', 'y']}
    SK = lambda *n: ['sc_' + a for a in n]
    halfpi = P.sb("halfpi", [128, 1]); P.memset('dve', halfpi[:], math.pi / 2, W=['halfpi'])
    P.act(sc['dt'][:], par[:, :, 2], AF.Exp, R=['par'], W=SK('dt'))
    P.tt('dve', sc['th'][:], par[:, :, 1], sc['dt'][:], ALU.mult, R=['par'] + SK('dt'), W=SK('th'))
    P.tt('dve', sc['r'][:], par[:, :, 0], sc['dt'][:], ALU.mult, R=['par'] + SK('dt'), W=SK('r'))
    P.act(sc['r'][:], sc['r'][:], AF.Exp, R=SK('r'), W=SK('r'))
    P.act(sc['s'][:], sc['th'][:], AF.Sin, scale=1.0 / 16, R=SK('th'), W=SK('s'))
    P.act(sc['c'][:], sc['th'][:], AF.Sin, scale=1.0 / 16, bias=halfpi[:], R=SK('th') + ['halfpi'], W=SK('c'))
    for _ in range(4):
        P.tt('dve', sc['c2'][:], sc['c'][:], sc['c'][:], ALU.mult, R=SK('c'), W=SK('c2'))
        P.tt('dve', sc['s2'][:], sc['s'][:], sc['s'][:], ALU.mult, R=SK('s'), W=SK('s2'))
        P.tt('dve', sc['t'][:], sc['c'][:], sc['s'][:], ALU.mult, R=SK('c', 's'), W=SK('t'))
        P.tt('dve', sc['c'][:], sc['c2'][:], sc['s2'][:], ALU.subtract, R=SK('c2', 's2'), W=SK('c'))
        P.ts('dve', sc['s'][:], sc['t'][:], 2.0, ALU.mult, R=SK('t'), W=SK('s'))
    P.tt('dve', sc['nr'][:], sc['r'][:], sc['c'][:], ALU.mult, R=SK('r', 'c'), W=SK('nr'))
    P.ts('dve', sc['nr'][:], sc['nr'][:], -1.0, ALU.add, R=SK('nr'), W=SK('nr'))
    P.tt('dve', sc['ni'][:], sc['r'][:], sc['s'][:], ALU.mult, R=SK('r', 's'), W=SK('ni'))
    P.tt('dve', sc['den'][:], par[:, :, 0], par[:, :, 0], ALU.mult, R=['par'], W=SK('den'))
    P.tt('dve', sc['x'][:], par[:, :, 1], par[:, :, 1], ALU.mult, R=['par'], W=SK('x'))
    P.tt('dve', sc['den'][:], sc['den'][:], sc['x'][:], ALU.add, R=SK('den', 'x'), W=SK('den'))
    P.op('dve', lambda e: e.reciprocal(out=sc['den'][:], in_=sc['den'][:]), R=SK('den'), W=SK('den'))
    P.tt('dve', sc['x'][:], sc['nr'][:], par[:, :, 0], ALU.mult, R=SK('nr') + ['par'], W=SK('x'))
    P.tt('dve', sc['y'][:], sc['ni'][:], par[:, :, 1], ALU.mult, R=SK('ni') + ['par'], W=SK('y'))
    P.tt('dve', sc['zr'][:], sc['x'][:], sc['y'][:], ALU.add, R=SK('x', 'y'), W=SK('zr'))
    P.tt('dve', sc['zr'][:], sc['zr'][:], sc['den'][:], ALU.mult, R=SK('zr', 'den'), W=SK('zr'))
    P.tt('dve', sc['x'][:], sc['ni'][:], par[:, :, 0], ALU.mult, R=SK('ni') + ['par'], W=SK('x'))
    P.tt('dve', sc['y'][:], sc['nr'][:], par[:, :, 1], ALU.mult, R=SK('nr') + ['par'], W=SK('y'))
    P.tt('dve', sc['zi'][:], sc['x'][:], sc['y'][:], ALU.subtract, R=SK('x', 'y'), W=SK('zi'))
    P.tt('dve', sc['zi'][:], sc['zi'][:], sc['den'][:], ALU.mult, R=SK('zi', 'den'), W=SK('zi'))
    tmpc = P.sb("tmpc", [128, 256])
    for st in range(8):
        c1 = sc['c'][:, st:st + 1]; s1 = sc['s'][:, st:st + 1]
        eng = 'dve' if st % 2 == 0 else 'pool'
        P.copy(eng, Er[:, st, 0:1], c1, R=SK('c'), W=['Er']); P.copy(eng, Ei[:, st, 0:1], s1, R=SK('s'), W=['Ei'])
        m = 1
        while m < CH:
            ar = Er[:, st, m - 1:m]; ai = Ei[:, st, m - 1:m]
            P.ts(eng, tmpc[:, :m], Ei[:, st, 0:m], ai, ALU.mult, R=['Ei'], W=['tmpc'])
            P.stt(eng, Er[:, st, m:2 * m], Er[:, st, 0:m], ar, tmpc[:, :m], ALU.mult, ALU.subtract, R=['Er', 'tmpc'], W=['Er'])
            P.ts(eng, tmpc[:, :m], Ei[:, st, 0:m], ar, ALU.mult, R=['Ei', 'Er'], W=['tmpc'])
            P.stt(eng, Ei[:, st, m:2 * m], Er[:, st, 0:m], ai, tmpc[:, :m], ALU.mult, ALU.add, R=['Er', 'tmpc', 'Ei'], W=['Ei'])
            m *= 2
        zr = sc['zr'][:, st:st + 1]; zi = sc['zi'][:, st:st + 1]
        for ri in range(2):
            P.dma(stg128[ri][:], s5BT[:, ri, st, :], W=[('stg128', ri)], q='sp')
            P.copy('act', BTb[:, ri, st, :], stg128[ri][:], R=[('stg128', ri)], W=['BTb'])
        for ri in range(2):
            P.dma(stg128[2 + ri][:], s5CT[:, ri, st, :], W=[('stg128', 2 + ri)], q='sp')
        P.ts(eng, tmpc[:, :128], stg128[3][:], zi, ALU.mult, R=[('stg128', 3)] + SK('zi'), W=['tmpc'])
        P.stt(eng, CTb[:, 0, st, :], stg128[2][:], zr, tmpc[:, :128], ALU.mult, ALU.subtract,
              R=[('stg128', 2), 'tmpc'] + SK('zr'), W=['CTb'])
        P.ts(eng, tmpc[:, :128], stg128[3][:], zr, ALU.mult, R=[('stg128', 3), 'CTb'] + SK('zr'), W=['tmpc'])
        P.stt(eng, tmpc[:, 128:256], stg128[2][:], zi, tmpc[:, :128], ALU.mult, ALU.add,
              R=[('stg128', 2), 'tmpc'] + SK('zi'), W=['tmpc'])
        P.ts(eng, CTb[:, 1, st, :], tmpc[:, 128:256], -1.0, ALU.mult, R=['tmpc'], W=['CTb'])
    carry = P.sb("carry", [128, 8, 2]); P.memset('dve', carry[:], 0.0, W=['carry'])
    s5t = {k: [P.sb("s5_%s%d" % (k, i), [128, CH]) for i in range(2)] for k in ['a', 'b', 'c', 'd']}
    sre = [P.sb("sre%d" % i, [128, CH]) for i in range(2)]; sim = [P.sb("sim%d" % i, [128, CH]) for i in range(2)]
    sreb = [P.sb("sreb%d" % i, [128, CH], BF16) for i in range(2)]; simb = [P.sb("simb%d" % i, [128, CH], BF16) for i in range(2)]
    ub = [P.sb("ub%d" % i, [128, CH], BF16) for i in range(2)]
    zt = [P.sb("zt%d" % i, [128, CH]) for i in range(2)]
    ztb = [P.sb("ztb%d" % i, [128, CH], BF16) for i in range(2)]
    obc = P.sb("obc", [128, 2, CH], BF16)
    gA = P.sb("gA", [128, CH]); gB = P.sb("gB", [128, CH])

    wg = P.sb("wg", [16, 128]); nbg = P.sb("nbg", [128, 1]); ngrep = P.sb("ngrep", [128, 256])
    P.dma(wg[:], gla_wg, W=['wg']); P.dma(nbg[:], gla_bg, W=['nbg']); P.dma(ngrep[:], gla_ng, W=['ngrep'])
    P.ts('dve', nbg[:], nbg[:], -1.0, ALU.mult, R=['nbg'], W=['nbg'])
    S = P.sb("S", [128, 256]); Sb = P.sb("Sb", [128, 256], BF16)
    P.memset('dve', S[:], 0.0, W=['S']); P.memset('dve', Sb[:], 0.0, W=['Sb'])
    gk = P.sb("gk", [128, CH]); bcum = P.sb("bcum", [128, CH])
    gt = {k: P.sb("g_" + k, [128, 128]) for k in ['eb', 'enb', 'ekst', 'kstT']}
    gtb = {k: P.sb("gb_" + k, [128, 128], BF16) for k in ['qin', 'kin', 'kst', 'att']}
    vtok = P.sb("vtok", [128, 256], BF16); rtok = P.sb("rtok", [128, 256]); osb = P.sb("osb", [128, 256])
    rsil = [P.sb("rsil%d" % i, [128, CH]) for i in range(2)]
    gsm = {k: P.sb("gsm_" + k, [128, 1]) for k in ['dec', 'ss', 'rstd', 'junk']}
    junk = P.sb("junk", [128, 256])
    GK = lambda *n: ['g_' + a for a in n]

    for c in range(NCHUNK):
        load_x_chunk(P, hT, c, xst, xb)
        for i, n in enumerate(names):
            bank = i % 2
            proj_tile(P, wb, i * 128, 128, xb, pb[bank], pk[bank])
            P.copy('act' if i % 2 == 0 else 'dve', pt[n][:], pb[bank][:, :CH], R=[pk[bank]], W=['pt_' + n])
        proj_tile(P, wb, 1024, 16, xb, pb[0], pk[0])
        P.copy('dve', glT[:], pb[0][:16, :CH], R=[pk[0]], W=['glT'])

        def s5_chain():
            for ut in range(2):
                un = 'u%d' % ut
                P.copy('act', ub[ut][:], pt[un][:], R=['pt_' + un], W=[('ub', ut)])
                for s4 in range(4):
                    st = ut * 4 + s4
                    i2 = st % 2
                    xr = pb[2]; xi = pb[3]
                    P.mm(xr[:, :CH], BTb[:, 0, st, :], ub[ut][:], R=['BTb', ('ub', ut)], W=[pk[2]])
                    P.mm(xi[:, :CH], BTb[:, 1, st, :], ub[ut][:], R=['BTb', ('ub', ut)], W=[pk[3]])
                    a = s5t['a'][i2]; b = s5t['b'][i2]; cc = s5t['c'][i2]; d = s5t['d'][i2]
                    ka, kb_, kc, kd = ('s5a', i2), ('s5b', i2), ('s5c', i2), ('s5d', i2)
                    P.tt('dve', a[:], xr[:, :CH], Er[:, st, :], ALU.mult, R=[pk[2], 'Er'], W=[ka])
                    P.tt('dve', b[:], xi[:, :CH], Ei[:, st, :], ALU.mult, R=[pk[3], 'Ei'], W=[kb_])
                    P.tt('pool', a[:], a[:], b[:], ALU.add, R=[ka, kb_], W=[ka])
                    P.tt('dve', cc[:], xi[:, :CH], Er[:, st, :], ALU.mult, R=[pk[3], 'Er'], W=[kc])
                    P.tt('dve', d[:], xr[:, :CH], Ei[:, st, :], ALU.mult, R=[pk[2], 'Ei'], W=[kd])
                    P.tt('pool', cc[:], cc[:], d[:], ALU.subtract, R=[kc, kd], W=[kc])
                    yield
                    P.op('dve', lambda e, a=a, b=b, st=st: e.tensor_tensor_scan(
                        out=b[:], data0=sc['r'][:, st:st + 1].to_broadcast([128, CH]), data1=a[:], initial=carry[:, st, 0:1], op0=ALU.mult, op1=ALU.add),
                        R=[ka, 'carry'] + SK('r'), W=[kb_])
                    P.op('dve', lambda e, cc=cc, d=d, st=st: e.tensor_tensor_scan(
                        out=d[:], data0=sc['r'][:, st:st + 1].to_broadcast([128, CH]), data1=cc[:], initial=carry[:, st, 1:2], op0=ALU.mult, op1=ALU.add),
                        R=[kc, 'carry'] + SK('r'), W=[kd])
                    sr = sre[i2]; si = sim[i2]
                    P.tt('pool', a[:], b[:], Er[:, st, :], ALU.mult, R=[kb_, 'Er'], W=[ka])
                    P.tt('pool', cc[:], d[:], Ei[:, st, :], ALU.mult, R=[kd, 'Ei'], W=[kc])
                    P.tt('dve', sr[:], a[:], cc[:], ALU.subtract, R=[ka, kc], W=[('sre', i2)])
                    P.tt('pool', a[:], b[:], Ei[:, st, :], ALU.mult, R=[kb_, 'Ei'], W=[ka])
                    P.tt('pool', cc[:], d[:], Er[:, st, :], ALU.mult, R=[kd, 'Er'], W=[kc])
                    P.tt('dve', si[:], a[:], cc[:], ALU.add, R=[ka, kc], W=[('sim', i2)])
                    P.copy('dve', carry[:, st, 0:1], sr[:, CH - 1:CH], R=[('sre', i2), 'carry'], W=['carry'])
                    P.copy('dve', carry[:, st, 1:2], si[:, CH - 1:CH], R=[('sim', i2), 'carry'], W=['carry'])
                    P.copy('act', sreb[i2][:], sr[:], R=[('sre', i2)], W=[('sreb', i2)])
                    P.copy('act', simb[i2][:], si[:], R=[('sim', i2)], W=[('simb', i2)])
                    P.mm(pb[4 + ut][:, :CH], CTb[:, 0, st, :], sreb[i2][:], start=(s4 == 0), stop=False,
                         R=['CTb', ('sreb', i2)], W=[pk[4 + ut]])
                    P.mm(pb[4 + ut][:, :CH], CTb[:, 1, st, :], simb[i2][:], start=False, stop=(s4 == 3),
                         R=['CTb', ('simb', i2)], W=[pk[4 + ut]])
                    yield
                P.stt('dve', zt[ut][:], pt[un][:], dcol[:, ut:ut + 1], pb[4 + ut][:, :CH], ALU.mult, ALU.add,
                      R=['pt_' + un, 'dcol', pk[4 + ut]], W=[('zt', ut)])
                gelu_tanh(P, zt[ut][:], zt[ut][:], gA, gB, CH, ('zt', ut), ('zt', ut), ['gA', 'gB'])
                P.copy('act', ztb[ut][:], zt[ut][:], R=[('zt', ut)], W=[('ztb', ut)])
                P.dma(y_loc[c][ut * 128:(ut + 1) * 128, :], ztb[ut][:], R=[('ztb', ut)], W=[('y_loc', c, ut)], q='pool')
                ykeys.append(('y_loc', c, ut))
                yield


        def gla_chain():
            P.mm(pb[7][:, :CH], wg[:], glT[:], R=['wg', 'glT'], W=[pk[7]])
            P.act(gk[:], pb[7][:, :CH], AF.Exp, scale=-1.0, bias=nbg[:], R=[pk[7], 'nbg'], W=['gk'])
            P.ts('dve', gk[:], gk[:], 1.0, ALU.add, R=['gk'], W=['gk'])
            P.act(gk[:], gk[:], AF.Ln, R=['gk'], W=['gk'])
            P.ts('dve', gk[:], gk[:], -1.0 / 16, ALU.mult, R=['gk'], W=['gk'])
            for i in range(2):
                P.act(rsil[i][:], pt['r%d' % i][:], AF.Silu, R=['pt_r%d' % i], W=[('rsil', i)])
            for sb_ in range(4):
                if (c * CH + (sb_ + 1) * 128) <= PAD:
                    continue
                ss = slice(sb_ * 128, (sb_ + 1) * 128)
                P.op('dve', lambda e, ss=ss: e.tensor_tensor_scan(out=bcum[:, ss], data0=onescol[:, 0:1].to_broadcast([128, 128]), data1=gk[:, ss],
                                                                 initial=0.0, op0=ALU.mult, op1=ALU.add),
                     R=['gk', 'onescol'], W=['bcum'])
                P.act(gt['eb'][:], bcum[:, ss], AF.Exp, R=['bcum'], W=GK('eb'))
                P.act(gt['enb'][:], bcum[:, ss], AF.Exp, scale=-1.0, R=['bcum'], W=GK('enb'))
                P.act(gt['ekst'][:], bcum[:, ss], AF.Exp, scale=-1.0, bias=bcum[:, sb_ * 128 + 127:sb_ * 128 + 128],
                      R=['bcum'], W=GK('ekst'))
                P.act(gsm['dec'][:], bcum[:, sb_ * 128 + 127:sb_ * 128 + 128], AF.Exp, R=['bcum'], W=['gsm_dec'])
                P.stt('dve', gtb['qin'][:], pt['q'][:, ss], 128.0 ** -0.5, gt['eb'][:], ALU.mult, ALU.mult,
                      R=['pt_q'] + GK('eb'), W=['gb_qin'])
                P.tt('dve', gtb['kin'][:], pt['k'][:, ss], gt['enb'][:], ALU.mult, R=['pt_k'] + GK('enb'), W=['gb_kin'])
                P.tt('dve', gt['kstT'][:], pt['k'][:, ss], gt['ekst'][:], ALU.mult, R=['pt_k'] + GK('ekst'), W=GK('kstT'))
                yield
                P.transpose(pb[6][:, 0:128], gt['kstT'][:], ident[:], R=GK('kstT') + ['ident'], W=[pk[6]])
                for i in range(2):
                    P.transpose(pb[6][:, 128 + i * 128:256 + i * 128], pt['v%d' % i][:, ss], ident[:],
                                R=['pt_v%d' % i, 'ident'], W=[pk[6]])
                P.copy('act', gtb['kst'][:], pb[6][:, 0:128], R=[pk[6]], W=['gb_kst'])
                P.copy('act', vtok[:], pb[6][:, 128:384], R=[pk[6]], W=['vtok'])
                yield
                for i in range(2):
                    P.transpose(pb[7][:, i * 128:(i + 1) * 128], rsil[i][:, ss], ident[:], R=[('rsil', i), 'ident'], W=[pk[7]])
                P.copy('act', rtok[:], pb[7][:, :256], R=[pk[7]], W=['rtok'])
                P.mm(pb[6][:, 384:512], gtb['kin'][:], gtb['qin'][:], R=['gb_kin', 'gb_qin'], W=[pk[6]])
                P.tt('dve', gtb['att'][:], pb[6][:, 384:512], U[:], ALU.mult, R=[pk[6], 'U'], W=['gb_att'])
                yield
                P.mm(pb[7][:, 256:512], gtb['att'][:], vtok[:], start=True, stop=False, R=['gb_att', 'vtok'], W=[pk[7]])
                P.mm(pb[7][:, 256:512], gtb['qin'][:], Sb[:], start=False, stop=True, R=['gb_qin', 'Sb'], W=[pk[7]])
                P.mm(pb[6][:, 0:256], gtb['kst'][:], vtok[:], R=['gb_kst', 'vtok'], W=[pk[6]])
                P.stt('dve', S[:], S[:], gsm['dec'][:], pb[6][:, 0:256], ALU.mult, ALU.add, R=['S', 'gsm_dec', pk[6]], W=['S'])
                P.copy('act', Sb[:], S[:], R=['S'], W=['Sb'])
                yield
                P.act(junk[:], pb[7][:, 256:512], AF.Square, accum_out=gsm['ss'][:], R=[pk[7]], W=['junk', 'gsm_ss'])
                P.ts('dve', gsm['rstd'][:], gsm['ss'][:], 1.0 / 256, ALU.mult, 1e-6, ALU.add, R=['gsm_ss'], W=['gsm_rstd'])
                P.act(gsm['rstd'][:], gsm['rstd'][:], AF.Sqrt, R=['gsm_rstd'], W=['gsm_rstd'])
                P.op('dve', lambda e: e.reciprocal(out=gsm['rstd'][:], in_=gsm['rstd'][:]), R=['gsm_rstd'], W=['gsm_rstd'])
                P.tt('pool', rtok[:], rtok[:], ngrep[:], ALU.mult, R=['rtok', 'ngrep'], W=['rtok'])
                P.stt('dve', osb[:], pb[7][:, 256:512], gsm['rstd'][:], rtok[:], ALU.mult, ALU.mult,
                      R=[pk[7], 'gsm_rstd', 'rtok'], W=['osb'])
                yield
                for i in range(2):
                    P.transpose(pb[7][:, i * 128:(i + 1) * 128], osb[:, i * 128:(i + 1) * 128], ident[:], R=['osb', 'ident'], W=[pk[7]])
                for i in range(2):
                    P.copy('act', obc[:, i, ss], pb[7][:, i * 128:(i + 1) * 128], R=[pk[7]], W=['obc'])
                yield
        gens = [s5_chain(), gla_chain()]
        while gens:
            for g in list(gens):
                try:
                    next(g)
                except StopIteration:
                    gens.remove(g)
        for i in range(2):
            P.dma(y_loc[c][256 + i * 128:256 + (i + 1) * 128, :], obc[:, i, :], R=['obc'], W=[('y_loc', c, 2 + i)], q='pool')
            ykeys.append(('y_loc', c, 2 + i))
        P.allgather(y_all[c], y_loc[c], groups, R=[('y_loc', c, k) for k in range(4)], W=[('y_all', c)])
    return ykeys

import math, os


def phase_cd(P, pb, pk, NCHUNK, h_all, y_loc, y_all, groups, pre="c_", stage=99, PN=342):
    Tp = NCHUNK * CH
    NCOL = 12 * 128 + 4
    T2 = 2052
    w_in = P.dram(pre + "w_in", [D, NCOL])
    convw = P.dram(pre + "convw", [128, 8, 4]); convb = P.dram(pre + "convb", [128, 8])
    gpar = P.dram(pre + "gpar", [4, 4])
    gdn_ng = P.dram(pre + "gdn_ng", [128, 128])
    lwa = P.dram(pre + "lwa", [128, 2, 128]); lwx = P.dram(pre + "lwx", [128, 2, 128]); lpar = P.dram(pre + "lpar", [128, 2, 3])
    c_U = P.dram(pre + "c_U", [128, 128]); c_Us = P.dram(pre + "c_Us", [128, 128]); c_ident = P.dram(pre + "c_ident", [128, 128])
    c_sel4 = P.dram(pre + "c_sel4", [4, 512])
    ykeys = []
    R_ = lambda b, i: pk[6] if (b, i) == (3, 3) else pk[b]
    wb = inproj_setup(P, w_in, NCOL)
    xb = P.sb("xb", [128, 16, CH], BF16)
    gobc = P.sb("gobc", [128, 2, CH], BF16); ylb = P.sb("ylb", [128, 2, CH], BF16)
    praw = [P.sb("praw%d" % i, [128, CH + 3]) for i in range(8)]
    pc = [P.sb("pc%d" % i, [128, CH]) for i in range(8)]
    pz = [P.sb("pz%d" % i, [128, CH]) for i in range(4)]
    baT = P.sb("baT", [4, CH])
    U = P.sb("U", [128, 128]); Us = P.sb("Us", [128, 128]); ident = P.sb("ident", [128, 128])
    ones_f = P.sb("ones_f", [128, 128]); onescol = P.sb("onescol", [128, 1]); sel4 = P.sb("sel4", [4, 512])
    P.dma(U[:], c_U, W=['U']); P.dma(Us[:], c_Us, W=['Us']); P.dma(ident[:], c_ident, W=['ident'])
    P.dma(sel4[:], c_sel4, W=['sel4'])
    P.memset('dve', ones_f[:], 1.0, W=['ones_f']); P.memset('dve', onescol[:], 1.0, W=['onescol'])
    cw = P.sb("cw", [128, 8, 4]); cb = P.sb("cb", [128, 8])
    P.dma(cw[:], convw, W=['cw']); P.dma(cb[:], convb, W=['cb'])
    for i in range(8):
        P.memset('pool', praw[i][:, 0:3], 0.0, W=[('praw', i)])
    gp = P.sb("gp", [4, 4]); P.dma(gp[:], gpar, W=['gp'])
    negA = P.sb("negA", [4, 1])
    P.act(negA[:], gp[:, 1:2], AF.Exp, R=['gp'], W=['negA'])
    P.ts('dve', negA[:], negA[:], -1.0, ALU.mult, R=['negA'], W=['negA'])
    ngrep = P.sb("ngrep", [128, 128]); P.dma(ngrep[:], gdn_ng, W=['ngrep'])
    S = [P.sb("S%d" % h, [128, 128]) for h in range(2)]; Sb = [P.sb("Sb%d" % h, [128, 128], BF16) for h in range(2)]
    for h in range(2):
        P.memset('dve', S[h][:], 0.0, W=[('S', h)]); P.memset('dve', Sb[h][:], 0.0, W=[('Sb', h)])
    sig4 = P.sb("sig4", [4, CH]); g4 = P.sb("g4", [4, CH]); bg4 = P.sb("bg4", [4, CH])
    Brep = [P.sb("Brep%d" % h, [128, CH]) for h in range(2)]; Grep = [P.sb("Grep%d" % h, [128, CH]) for h in range(2)]
    qnb = [P.sb("qnb%d" % h, [128, CH], BF16) for h in range(2)]; knb = [P.sb("knb%d" % h, [128, CH], BF16) for h in range(2)]
    knf = [P.sb("knf%d" % h, [128, CH]) for h in range(2)]
    t512 = [P.sb("t512_%d" % i, [128, CH]) for i in range(3)]
    f2 = [{k: P.sb("f%d_%s" % (h, k), [128, 128]) for k in
           ['R1', 'DT', 'Gam', 'GamU', 'P0', 'BU', 'Pa', 'Pb', 'Qa', 'Qb', 'R', 'egr', 'zs', 'osb', 'junk', 'ktok', 'vtok']}
          for h in range(2)]
    bq2 = [{k: P.sb("b%d_%s" % (h, k), [128, 128], BF16) for k in ['Rb', 'Aqk', 'kbg', 'kst', 'vb', 'nwc', 'vnew', 'qdec']}
           for h in range(2)]
    sm2 = [{k: P.sb("sm%d_%s" % (h, k), [128, 1]) for k in ['gcc', 'glast', 'egc', 'bege', 'ekl', 'dec', 'ss', 'rstd']}
           for h in range(2)]
    tok4 = P.sb("tok4", [128, 4])
    FK = lambda *n: ['f_' + a for a in n]
    BK = lambda *n: ['b_' + a for a in n]
    MK = lambda *n: ['sm_' + a for a in n]
    wa_f = P.sb("wa_f", [128, 2, 128]); wx_f = P.sb("wx_f", [128, 2, 128]); lp = P.sb("lp", [128, 2, 3])
    wa_b = P.sb("wa_b", [128, 2, 128], BF16); wx_b = P.sb("wx_b", [128, 2, 128], BF16)
    P.dma(wa_f[:], lwa, W=['wa_f']); P.dma(wx_f[:], lwx, W=['wx_f']); P.dma(lp[:], lpar, W=['lp'])
    P.copy('dve', wa_b[:], wa_f[:], R=['wa_f'], W=['wa_b']); P.copy('dve', wx_b[:], wx_f[:], R=['wx_f'], W=['wx_b'])
    ccol = P.sb("ccol", [128, 2])
    P.act(ccol[:], lp[:, :, 2], AF.Exp, scale=-1.0, R=['lp'], W=['ccol'])
    P.ts('dve', ccol[:], ccol[:], 1.0, ALU.add, R=['ccol'], W=['ccol'])
    P.act(ccol[:], ccol[:], AF.Ln, R=['ccol'], W=['ccol'])
    P.ts('dve', ccol[:], ccol[:], -8.0, ALU.mult, R=['ccol'], W=['ccol'])
    hprev = P.sb("hprev", [128, 2]); P.memset('dve', hprev[:], 0.0, W=['hprev'])
    xcb = P.sb("xcb", [128, CH], BF16)
    L = {k: P.sb("l_" + k, [128, CH]) for k in ['r', 'i', 'a', 'bx', 'h', 'gA', 'gB', 'y']}
    LK = lambda *n: ['l_' + a for a in n]

    for c in range(NCHUNK):
        xk = [('xb', dt) for dt in range(16)]
        p0 = c * CH
        if p0 < PAD:
            P.memset('pool', xb[:, :, 0:PAD - p0], 0.0, W=xk)
        pos = max(p0, PAD)
        while pos < p0 + CH:
            t = pos - PAD
            r = t // T2
            col = t - r * T2
            pc_ = col // PN
            off = col - pc_ * PN
            n = min(p0 + CH - pos, PN - off)
            for half in range(2):
                P.dma(xb[:, 8 * half:8 * half + 8, pos - p0:pos - p0 + n],
                      h_all[pc_][half][r * 1024:(r + 1) * 1024, off:off + n].rearrange("(t p) n -> p t n", p=128),
                      W=xk, q='sp')
            pos += n
        for i in range(12):
            bank = i % 2
            proj_tile(P, wb, i * 128, 128, xb, pb[bank], pk[bank])
            if i < 8:
                P.copy('act', praw[i][:, 3:CH + 3], pb[bank][:, :CH], R=[pk[bank]], W=[('praw', i)])
                P.ts('dve', pc[i][:], praw[i][:, 0:CH], cw[:, i, 0:1], ALU.mult, cb[:, i:i + 1], ALU.add,
                     R=[('praw', i), 'cw', 'cb'], W=[('pc', i)])
                for jj in range(1, 4):
                    P.stt('dve', pc[i][:], praw[i][:, jj:CH + jj], cw[:, i, jj:jj + 1], pc[i][:], ALU.mult, ALU.add,
                          R=[('praw', i), 'cw', ('pc', i)], W=[('pc', i)])
                P.copy('pool', praw[i][:, 0:3], praw[i][:, CH:CH + 3], R=[('praw', i)], W=[('praw', i)])
                if i < 6:
                    P.act(pc[i][:], pc[i][:], AF.Silu, R=[('pc', i)], W=[('pc', i)])
            else:
                P.copy('act', pz[i - 8][:], pb[bank][:, :CH], R=[pk[bank]], W=[('pz', i - 8)])
        proj_tile(P, wb, 12 * 128, 4, xb, pb[0], pk[0])
        P.copy('act', baT[:], pb[0][:4, :CH], R=[pk[0]], W=['baT'])

        for b in range(2):
            xc = pc[6 + b]
            P.copy('act', xcb[:], xc[:], R=[('pc', 6 + b)], W=['xcb'])
            P.mm(pb[6][:, :CH], wa_b[:, b, :], xcb[:], R=['wa_b', 'xcb'], W=[pk[6]])
            P.mm(pb[7][:, :CH], wx_b[:, b, :], xcb[:], R=['wx_b', 'xcb'], W=[pk[7]])
            P.act(L['r'][:], pb[6][:, :CH], AF.Sigmoid, bias=lp[:, b, 0:1], R=[pk[6], 'lp'], W=LK('r'))
            P.act(L['i'][:], pb[7][:, :CH], AF.Sigmoid, bias=lp[:, b, 1:2], R=[pk[7], 'lp'], W=LK('i'))
            P.act(L['a'][:], L['r'][:], AF.Exp, scale=ccol[:, b:b + 1], R=LK('r') + ['ccol'], W=LK('a'))
            P.tt('pool', L['bx'][:], L['a'][:], L['a'][:], ALU.mult, R=LK('a'), W=LK('bx'))
            P.ts('pool', L['bx'][:], L['bx'][:], -1.0, ALU.mult, 1.0, ALU.add, R=LK('bx'), W=LK('bx'))
            P.act(L['bx'][:], L['bx'][:], AF.Sqrt, R=LK('bx'), W=LK('bx'))
            P.tt('pool', L['bx'][:], L['bx'][:], L['i'][:], ALU.mult, R=LK('bx', 'i'), W=LK('bx'))
            P.tt('pool', L['bx'][:], L['bx'][:], xc[:], ALU.mult, R=LK('bx') + [('pc', 6 + b)], W=LK('bx'))
            c0 = PAD if c == 0 else 0
            if c == 0:
                P.memset('dve', L['h'][:, :PAD], 0.0, W=LK('h'))
            P.op('dve', lambda e, b=b, c0=c0: e.tensor_tensor_scan(
                out=L['h'][:, c0:], data0=L['a'][:, c0:], data1=L['bx'][:, c0:], initial=hprev[:, b:b + 1],
                op0=ALU.mult, op1=ALU.add), R=LK('a', 'bx') + ['hprev'], W=LK('h'))
            P.copy('dve', hprev[:, b:b + 1], L['h'][:, CH - 1:CH], R=LK('h') + ['hprev'], W=['hprev'])
            gelu_tanh(P, L['y'][:], pz[2 + b][:], L['gA'], L['gB'], CH, ('pz', 2 + b), 'l_y', LK('gA', 'gB'))
            P.tt('pool', L['y'][:], L['y'][:], L['h'][:], ALU.mult, R=LK('y', 'h'), W=LK('y'))
            P.copy('act', ylb[:, b, :], L['y'][:], R=LK('y'), W=['ylb'])
            P.dma(y_loc[c][256 + b * 128:256 + (b + 1) * 128, :], ylb[:, b, :], R=['ylb'], W=[('y_loc', c, 2 + b)], q='pool')
            ykeys.append(('y_loc', c, 2 + b))

        if stage < 1:
            continue
        P.act(sig4[:], baT[:], AF.Sigmoid, R=['baT'], W=['sig4'])
        P.act(g4[:], baT[:], AF.Exp, bias=gp[:, 0:1], R=['baT', 'gp'], W=['g4'])
        P.ts('dve', g4[:], g4[:], 1.0, ALU.add, R=['g4'], W=['g4'])
        P.act(g4[:], g4[:], AF.Ln, R=['g4'], W=['g4'])
        P.ts('dve', g4[:], g4[:], negA[:, 0:1], ALU.mult, R=['g4', 'negA'], W=['g4'])
        P.ts('dve', bg4[:], sig4[:], gp[:, 2:3], ALU.mult, R=['sig4', 'gp'], W=['bg4'])
        P.stt('dve', bg4[:], g4[:], gp[:, 3:4], bg4[:], ALU.mult, ALU.add, R=['g4', 'gp', 'bg4'], W=['bg4'])
        for h in range(2):
            P.mm(pb[6][:, :CH], sel4[:, h * 128:(h + 1) * 128], sig4[:], R=['sel4', 'sig4'], W=[pk[6]])
            P.copy('act', Brep[h][:], pb[6][:, :CH], R=[pk[6]], W=[('Brep', h)])
            P.mm(pb[7][:, :CH], sel4[:, (2 + h) * 128:(3 + h) * 128], g4[:], R=['sel4', 'g4'], W=[pk[7]])
            P.copy('act', Grep[h][:], pb[7][:, :CH], R=[pk[7]], W=[('Grep', h)])
            for which, src, scale_ in (('q', pc[h], 128.0 ** -0.5), ('k', pc[2 + h], 1.0)):
                P.act(t512[0][:], src[:], AF.Square, R=[('pc', h if which == 'q' else 2 + h)], W=[('t512', 0)])
                P.mm(pb[6][:, :CH], ones_f[:], t512[0][:], R=['ones_f', ('t512', 0)], W=[pk[6]])
                P.ts('dve', t512[1][:], pb[6][:, :CH], 1e-6, ALU.add, R=[pk[6]], W=[('t512', 1)])
                P.act(t512[1][:], t512[1][:], AF.Sqrt, R=[('t512', 1)], W=[('t512', 1)])
                P.op('dve', lambda e: e.reciprocal(out=t512[1][:], in_=t512[1][:]), R=[('t512', 1)], W=[('t512', 1)])
                if which == 'q':
                    P.stt('dve', qnb[h][:], src[:], scale_, t512[1][:], ALU.mult, ALU.mult, R=[('pc', h), ('t512', 1)], W=[('qnb', h)])
                else:
                    P.tt('dve', knf[h][:], src[:], t512[1][:], ALU.mult, R=[('pc', 2 + h), ('t512', 1)], W=[('knf', h)])
                    P.copy('act', knb[h][:], knf[h][:], R=[('knf', h)], W=[('knb', h)])

        if stage < 2:
            continue
        def head_chain(h, sb_, ss):
            f = f2[h]; bq = bq2[h]; sm = sm2[h]
            FK = lambda *n: ['f%d_%s' % (h, a) for a in n]
            BK = lambda *n: ['b%d_%s' % (h, a) for a in n]
            MK = lambda *n: ['sm%d_%s' % (h, a) for a in n]
            bA, bB, bC = (2, 3, 4) if h == 0 else (5, 6, 7)
            pA, pB, pC = pb[bA], pb[bB], pb[bC]
            kA, kB, kC = pk[bA], pk[bB], pk[bC]
            bcol = tok4[:, h:h + 1]; gcol = tok4[:, 2 + h:3 + h]
            P.op('dve', lambda e: e.tensor_tensor_scan(
                out=f['R1'][:], data0=onescol[:, 0:1].to_broadcast([128, 128]), data1=Grep[h][:, ss], initial=0.0,
                op0=ALU.mult, op1=ALU.add), R=[('Grep', h), 'onescol'], W=FK('R1'))
            P.mm(pA[:, 0:1], U[:], gcol, R=['U', 'tok4'], W=[kA])
            P.copy('act', sm['gcc'][:], pA[:, 0:1], R=[kA], W=MK('gcc'))
            P.copy('pool', sm['glast'][:], f['R1'][:, 127:128], R=FK('R1'), W=MK('glast'))
            yield
            P.ts('dve', f['DT'][:], f['R1'][:], sm['gcc'][:], ALU.subtract, 0.0, ALU.min, R=FK('R1') + MK('gcc'), W=FK('DT'))
            P.act(f['Gam'][:], f['DT'][:], AF.Exp, R=FK('DT'), W=FK('Gam'))
            P.tt('pool', f['GamU'][:], f['Gam'][:], U[:], ALU.mult, R=FK('Gam') + ['U'], W=FK('GamU'))
            P.tt('pool', f['BU'][:], Brep[h][:, ss], Us[:], ALU.mult, R=[('Brep', h), 'Us'], W=FK('BU'))
            P.act(f['egr'][:], f['R1'][:], AF.Exp, R=FK('R1'), W=FK('egr'))
            P.act(sm['egc'][:], sm['gcc'][:], AF.Exp, R=MK('gcc'), W=MK('egc'))
            P.tt('pool', sm['bege'][:], sm['egc'][:], bcol, ALU.mult, R=MK('egc') + ['tok4'], W=MK('bege'))
            P.act(sm['ekl'][:], sm['gcc'][:], AF.Exp, scale=-1.0, bias=sm['glast'][:], R=MK('gcc', 'glast'), W=MK('ekl'))
            P.act(sm['dec'][:], sm['glast'][:], AF.Exp, R=MK('glast'), W=MK('dec'))
            yield
            P.mm(pA[:, 0:128], knb[h][:, ss], knb[h][:, ss], R=[('knb', h)], W=[kA])
            P.mm(pA[:, 128:256], knb[h][:, ss], qnb[h][:, ss], R=[('knb', h), ('qnb', h)], W=[kA])
            P.transpose(pA[:, 256:384], knf[h][:, ss], ident[:], R=[('knf', h), 'ident'], W=[kA])
            P.transpose(pA[:, 384:512], pc[4 + h][:, ss], ident[:], R=[('pc', 4 + h), 'ident'], W=[kA])
            yield
            P.tt('dve', f['P0'][:], pA[:, 0:128], f['Gam'][:], ALU.mult, R=[kA] + FK('Gam'), W=FK('P0'))
            P.tt('dve', f['Pa'][:], f['P0'][:], f['BU'][:], ALU.mult, R=FK('P0', 'BU'), W=FK('Pa'))
            P.tt('dve', bq['Aqk'][:], pA[:, 128:256], f['GamU'][:], ALU.mult, R=[kA] + FK('GamU'), W=BK('Aqk'))
            P.copy('act', f['ktok'][:], pA[:, 256:384], R=[kA], W=FK('ktok'))
            P.copy('act', f['vtok'][:], pA[:, 384:512], R=[kA], W=FK('vtok'))
            P.ts('pool', bq['kbg'][:], f['ktok'][:], sm['bege'][:], ALU.mult, R=FK('ktok') + MK('bege'), W=BK('kbg'))
            P.ts('pool', bq['kst'][:], f['ktok'][:], sm['ekl'][:], ALU.mult, R=FK('ktok') + MK('ekl'), W=BK('kst'))
            P.ts('pool', bq['vb'][:], f['vtok'][:], bcol, ALU.mult, R=FK('vtok') + ['tok4'], W=BK('vb'))
            yield
            P.transpose(pB[:, 0:128], f['Pa'][:], ident[:], R=FK('Pa') + ['ident'], W=[kB])
            P.copy('act', f['Qa'][:], pB[:, 0:128], R=[kB], W=FK('Qa'))
            P.tt('pool', f['R'][:], ident[:], f['Pa'][:], ALU.subtract, R=['ident'] + FK('Pa'), W=FK('R'))
            yield
            Pc, Qc, Pn, Qn = 'Pa', 'Qa', 'Pb', 'Qb'
            for lvl in range(6):
                P.mm(pB[:, 128:256], f[Qc][:], f[Pc][:], R=FK(Qc, Pc), W=[kB])
                P.mm(pB[:, 256:384], f[Pc][:], f[Qc][:], R=FK(Qc, Pc), W=[kB])
                P.copy('act', f[Pn][:], pB[:, 128:256], R=[kB], W=FK(Pn))
                P.copy('act', f[Qn][:], pB[:, 256:384], R=[kB], W=FK(Qn))
                yield
                P.mm(pB[:, 384:512], f[Qn][:], f['R'][:], R=FK(Qn, 'R'), W=[kB])
                P.tt('dve', f['R'][:], f['R'][:], pB[:, 384:512], ALU.add, R=FK('R') + [kB], W=FK('R'))
                Pc, Qc, Pn, Qn = Pn, Qn, Pc, Qc
                yield
            P.copy('act', bq['Rb'][:], f['R'][:], R=FK('R'), W=BK('Rb'))
            P.mm(pC[:, 128:256], bq['kbg'][:], bq['Rb'][:], R=BK('kbg', 'Rb'), W=[kC])
            P.op('act', lambda e: e.activation(out=bq['nwc'][:], in_=pC[:, 128:256], func=AF.Copy, scale=-1.0),
                 R=[kC], W=BK('nwc'))
            yield
            P.mm(pC[:, 0:128], bq['Rb'][:], bq['vb'][:], start=True, stop=False, R=BK('Rb', 'vb'), W=[kC])
            P.mm(pC[:, 0:128], bq['nwc'][:], Sb[h][:], start=False, stop=True, R=BK('nwc') + [('Sb', h)], W=[kC])
            P.copy('act', bq['vnew'][:], pC[:, 0:128], R=[kC], W=BK('vnew'))
            P.tt('pool', bq['qdec'][:], qnb[h][:, ss], f['egr'][:], ALU.mult, R=[('qnb', h)] + FK('egr'), W=BK('qdec'))
            yield
            P.mm(pC[:, 256:384], bq['qdec'][:], Sb[h][:], start=True, stop=False, R=BK('qdec') + [('Sb', h)], W=[kC])
            P.mm(pC[:, 256:384], bq['Aqk'][:], bq['vnew'][:], start=False, stop=True, R=BK('Aqk', 'vnew'), W=[kC])
            P.mm(pC[:, 384:512], bq['kst'][:], bq['vnew'][:], R=BK('kst', 'vnew'), W=[kC])
            P.stt('dve', S[h][:], S[h][:], sm['dec'][:], pC[:, 384:512], ALU.mult, ALU.add,
                  R=[('S', h), kC] + MK('dec'), W=[('S', h)])
            P.copy('act', Sb[h][:], S[h][:], R=[('S', h)], W=[('Sb', h)])
            yield
            P.act(f['junk'][:], pC[:, 256:384], AF.Square, accum_out=sm['ss'][:], R=[kC], W=FK('junk') + MK('ss'))
            P.ts('dve', sm['rstd'][:], sm['ss'][:], 1.0 / 128, ALU.mult, 1e-6, ALU.add, R=MK('ss'), W=MK('rstd'))
            P.act(sm['rstd'][:], sm['rstd'][:], AF.Sqrt, R=MK('rstd'), W=MK('rstd'))
            P.op('dve', lambda e: e.reciprocal(out=sm['rstd'][:], in_=sm['rstd'][:]), R=MK('rstd'), W=MK('rstd'))
            P.transpose(pA[:, 0:128], pz[h][:, ss], ident[:], R=[('pz', h), 'ident'], W=[kA])
            P.act(f['zs'][:], pA[:, 0:128], AF.Silu, R=[kA], W=FK('zs'))
            P.tt('pool', f['zs'][:], f['zs'][:], ngrep[:], ALU.mult, R=FK('zs') + ['ngrep'], W=FK('zs'))
            yield
            P.stt('dve', f['osb'][:], pC[:, 256:384], sm['rstd'][:], f['zs'][:], ALU.mult, ALU.mult,
                  R=[kC] + MK('rstd') + FK('zs'), W=FK('osb'))
            P.transpose(pA[:, 128:256], f['osb'][:], ident[:], R=FK('osb') + ['ident'], W=[kA])
            P.copy('act', gobc[:, h, ss], pA[:, 128:256], R=[kA], W=['gobc'])

        for sb_ in range(4):
            if (c * CH + (sb_ + 1) * 128) <= PAD:
                continue
            ss = slice(sb_ * 128, (sb_ + 1) * 128)
            P.transpose(pb[1][:, 0:4], bg4[:, ss], ident[:4, :4], R=['bg4', 'ident'], W=[pk[1]])
            P.copy('act', tok4[:], pb[1][:, 0:4], R=[pk[1]], W=['tok4'])
            gens = [head_chain(0, sb_, ss), head_chain(1, sb_, ss)]
            while gens:
                for g in list(gens):
                    try:
                        next(g)
                    except StopIteration:
                        gens.remove(g)
        for h in range(2):
            P.dma(y_loc[c][h * 128:(h + 1) * 128, :], gobc[:, h, :], R=['gobc'], W=[('y_loc', c, h)], q='pool')
            ykeys.append(('y_loc', c, h))
        P.allgather(y_all[c], y_loc[c], groups, R=[('y_loc', c, k) for k in range(4)], W=[('y_all', c)])
    return ykeys


D = 2048
NE = 32
FF = 256
DN_ALPHA = (2.0 * 2) ** 0.25
LN_EPS = 1e-5
PAD_ = 496


def layer_norm_fm(P, v, N, gcol, bcol, pbA, pbB, kA, kB, ones_f, tmp, hb=None, vkey='v', hbkey='hb'):
    sq = tmp['sq']; mean = tmp['mean']; rstd = tmp['rstd']; m2 = tmp['m2']
    for dt in range(16):
        P.mm(pbA[:, :N], ones_f[:], v[:, dt, :N], start=(dt == 0), stop=(dt == 15), R=[(vkey, dt)], W=[kA])
    for dt in range(16):
        s = sq[dt % 2]
        P.act(s[:, :N], v[:, dt, :N], AF.Square, R=[(vkey, dt)], W=[('sq', dt % 2)])
        P.mm(pbB[:, :N], ones_f[:], s[:, :N], start=(dt == 0), stop=(dt == 15), R=[('sq', dt % 2)], W=[kB])
    P.act(mean[:, :N], pbA[:, :N], AF.Copy, scale=1.0 / D, R=[kA], W=['mean'])
    P.tt('dve', m2[:, :N], mean[:, :N], mean[:, :N], ALU.mult, R=['mean'], W=['m2'])
    P.ts('dve', rstd[:, :N], pbB[:, :N], 1.0 / D, ALU.mult, LN_EPS, ALU.add, R=[kB], W=['rstd'])
    P.tt('dve', rstd[:, :N], rstd[:, :N], m2[:, :N], ALU.subtract, R=['rstd', 'm2'], W=['rstd'])
    P.act(rstd[:, :N], rstd[:, :N], AF.Sqrt, R=['rstd'], W=['rstd'])
    P.op('dve', lambda e: e.reciprocal(out=rstd[:, :N], in_=rstd[:, :N]), R=['rstd'], W=['rstd'])
    for dt in range(16):
        eng = 'dve' if dt % 2 == 0 else 'pool'
        P.tt(eng, v[:, dt, :N], v[:, dt, :N], mean[:, :N], ALU.subtract, R=[(vkey, dt), 'mean'], W=[(vkey, dt)])
        P.tt(eng, v[:, dt, :N], v[:, dt, :N], rstd[:, :N], ALU.mult, R=[(vkey, dt), 'rstd'], W=[(vkey, dt)])
        P.ts(eng, v[:, dt, :N], v[:, dt, :N], gcol[:, dt:dt + 1], ALU.mult, bcol[:, dt:dt + 1], ALU.add,
             R=[(vkey, dt)], W=[(vkey, dt)])
        if hb is not None:
            P.copy('act', hb[:, dt, :N], v[:, dt, :N], R=[(vkey, dt)], W=[(hbkey, dt)])


def phase_post(P, pb, pk, glu, pre, y_all, hres, out32, outb, outb_all, groups, esel, NCH=6, N=342):
    T2 = NCH * N
    hT = hres
    outT = out32
    w_out = P.dram(pre + "w_out", [D, D])
    if glu:
        w_glu = P.dram(pre + "w_glu", [1024, 1024]); b_glu = P.dram(pre + "b_glu", [128, 8])
    lnp = P.dram(pre + "lnp", [128, 64])
    w_r = P.dram(pre + "w_r", [D, 36]); b_r = P.dram(pre + "b_r", [1, 36])
    w_gate = P.dram(pre + "w_gate", [NE, D, FF]); w_up = P.dram(pre + "w_up", [NE, D, FF]); w_down = P.dram(pre + "w_down", [NE, FF, D])
    c_ident = P.dram(pre + "c_ident", [128, 128]); c_sel = P.dram(pre + "c_sel", [32, NE * 128])

    ones_f = P.sb("ones_f", [128, 128]); ident = P.sb("ident", [128, 128]); selb = [P.sb("selb%d" % i, [32, 128]) for i in range(2)]
    lnp_sb = P.sb("lnp_sb", [128, 64]); wr_sb = P.sb("wr_sb", [128, 16, 36]); br_sb = P.sb("br_sb", [1, 36])
    if glu:
        bglu_sb = P.sb("bglu_sb", [128, 8])
    mst = P.sb("mst", [128, 8, N])
    yb = P.sb("yb", [128, 16, N], BF16)
    v = P.sb("v", [128, 16, N])
    hb = P.sb("hb", [128, 16, N], BF16)
    hh = P.sb("hh", [128, 2 * NE, N], BF16)
    hres = [P.sb("hres%d" % i, [128, N]) for i in range(2)]
    wst = [P.sb("wst%d" % i, [128, 8, 128]) for i in range(3)]
    wsm = [P.sb("wsm%d" % i, [128, 16, 128]) for i in range(1)]
    wsmb = [P.sb("wsmb%d" % i, [128, 16, 128], BF16) for i in range(2)]
    NWB = 4
    wb = [P.sb("wb%d" % i, [128, 16, 256], BF16) for i in range(NWB)]
    wdst = [P.sb("wdst%d" % i, [128, 1024]) for i in range(2)]
    NWD = 6
    wd = [P.sb("wd%d" % i, [128, 1, 1024], BF16) for i in range(NWD)]
    tmp = {'sq': [P.sb("sq%d" % i, [128, N]) for i in range(2)], 'mean': P.sb("mean", [128, N]),
           'rstd': P.sb("rstd", [128, N]), 'm2': P.sb("m2", [128, N])}
    sg = [P.sb("sg%d" % i, [128, N]) for i in range(2)]
    tq = [P.sb("tq%d" % i, [128, N]) for i in range(2)]
    combT = P.sb("combT", [32, N])
    rs = {k: P.sb("rs_" + k, [128, w]) for k, w in
          [('L', 36), ('gmax', 1), ('ngmax', 1), ('ohg', 4), ('eg', 4), ('se', 1), ('ptop', 1), ('lesel', 8),
           ('m1', 1), ('oh1', 8), ('msk', 8), ('m2', 1), ('oh2', 8), ('d', 1), ('ed', 1), ('w1', 1), ('w2', 1),
           ('ce', 8), ('comb', 32)]}
    idram = lambda n, sh: P.nc.dram_tensor(pre + n, sh, BF16, kind="Internal").ap()
    wc_gu = idram("wc_gu", [NE * 2, 128, 16 * 256]); wc_d = idram("wc_d", [2 * NE * 2, 128, 1024])
    wc_o = idram("wc_o", [16, 128, 16 * 128]); wc_g = idram("wc_g", [8, 128, 8 * 128])
    es_sb = P.sb("esel", [128, 4]); P.dma(es_sb[:], esel, W=['esel'])
    cand = [P.sb("cand%d" % i, [128, N], BF16) for i in range(4)]

    def select_into_mst(c, part):
        for j in range(8):
            row0 = (j // 2) * 512 + part + (j % 2) * 128
            for r in range(4):
                col0 = PAD_ + r * T2 + c * N
                pos = col0
                while pos < col0 + N:
                    yc = pos // 512
                    n = min(col0 + N - pos, (yc + 1) * 512 - pos)
                    P.dma(cand[r][:, pos - col0:pos - col0 + n], y_all[yc][row0:row0 + 128, pos - yc * 512:pos - yc * 512 + n],
                          W=[('cand', r)], q='sp')
                    pos += n
            P.ts('dve', mst[:, j, :], cand[0][:], es_sb[:, 0:1], ALU.mult, R=[('cand', 0), 'esel'], W=['mst'])
            for r in range(1, 4):
                P.stt('dve', mst[:, j, :], cand[r][:], es_sb[:, r:r + 1], mst[:, j, :], ALU.mult, ALU.add,
                      R=[('cand', r), 'esel', 'mst'], W=['mst'])

    P.memset('dve', ones_f[:], 1.0, W=['ones_f'])
    P.dma(ident[:], c_ident, W=['ident'])
    P.dma(lnp_sb[:], lnp, W=['lnp']); P.dma(br_sb[:], b_r, W=['br'])
    P.dma(wr_sb[:], w_r.rearrange("(t p) n -> p t n", p=128), W=['wr'])
    if glu:
        P.dma(bglu_sb[:], b_glu, W=['bglu'])

    cast_rr = [0]

    def cast(out, in_, R, W):
        eng = ('act', 'pool')[cast_rr[0] % 2]
        cast_rr[0] += 1
        P.copy(eng, out, in_, R=R, W=W)

    wsm_i = [0]

    def load_coltile(wdram, ktiles, col0, c, cache):
        i = wsm_i[0] % 2
        wsm_i[0] += 1
        cv = cache.rearrange("p (t n) -> p t n", t=ktiles)
        ck = ('wcache', cache.tensor.name, col0)
        if c == 0:
            P.dma(wsm[0][:, :ktiles, :], wdram[:, col0:col0 + 128].rearrange("(t p) n -> p t n", p=128),
                  W=[('wsm', 0)], q='sp')
            cast(wsmb[i][:, :ktiles, :], wsm[0][:, :ktiles, :], R=[('wsm', 0)], W=[('wsmb', i)])
            P.dma(cv, wsmb[i][:, :ktiles, :], R=[('wsmb', i)], W=[ck], q='pool')
        else:
            P.dma(wsmb[i][:, :ktiles, :], cv, R=[ck], W=[('wsmb', i)], q='sp')
        return wsmb[i], ('wsmb', i)

    for c in range(NCH):
        cs = slice(c * N, (c + 1) * N)
        select_into_mst(c, 0)
        if glu:
            for j in range(8):
                P.copy('act', yb[:, 8 + j, :], mst[:, j, :], R=['mst'], W=[('yb', 8 + j)])
            for j in range(8):
                wt, wk = load_coltile(w_glu, 8, j * 128, c, wc_g[j])
                for i in range(8):
                    P.mm(pb[6][:, :N], wt[:, i, :], yb[:, 8 + i, :], start=(i == 0), stop=(i == 7),
                         R=[wk, ('yb', 8 + i)], W=[pk[6]])
                P.act(sg[j % 2][:, :], pb[6][:, :N], AF.Sigmoid, bias=bglu_sb[:, j:j + 1], R=[pk[6], 'bglu'], W=[('sg', j % 2)])
                P.tt('dve', yb[:, j, :], mst[:, j, :], sg[j % 2][:, :], ALU.mult, R=['mst', ('sg', j % 2)], W=[('yb', j)])
        else:
            for j in range(8):
                P.copy('act', yb[:, j, :], mst[:, j, :], R=['mst'], W=[('yb', j)])
        select_into_mst(c, 256)
        for j in range(8):
            P.copy('act', yb[:, 8 + j, :], mst[:, j, :], R=['mst'], W=[('yb', 8 + j)])
        for dt in range(16):
            wt, wk = load_coltile(w_out, 16, dt * 128, c, wc_o[dt])
            hr = hres[dt % 2]
            P.dma(hr[:], hT[dt * 128:(dt + 1) * 128, cs], W=[('hres', dt % 2)], q='pool')
            bank = 6 + dt % 2
            for i in range(16):
                P.mm(pb[bank][:, :N], wt[:, i, :], yb[:, i, :], start=(i == 0), stop=(i == 15),
                     R=[wk, ('yb', i)], W=[pk[bank]])
            P.stt('dve', v[:, dt, :], hr[:], DN_ALPHA, pb[bank][:, :N], ALU.mult, ALU.add,
                  R=[('hres', dt % 2), pk[bank]], W=[('v', dt)])
        layer_norm_fm(P, v, N, lnp_sb[:, 0:16], lnp_sb[:, 16:32], pb[6], pb[7], pk[6], pk[7], ones_f, tmp, hb=hb)
        t0 = 0
        while t0 < N:
            M = min(128, N - t0)
            for dt in range(16):
                P.mm(pb[6][:M, :36], v[:, dt, t0:t0 + M], wr_sb[:, dt, :], start=(dt == 0), stop=False,
                     R=[('v', dt), 'wr'], W=[pk[6]])
            P.mm(pb[6][:M, :36], ones_f[0:1, :M], br_sb[:, :], start=False, stop=True, R=['ones_f', 'br'], W=[pk[6]])
            r = {k: t[:M, :] for k, t in rs.items()}
            K = lambda *names: ['rs_' + n for n in names]
            P.copy('dve', r['L'], pb[6][:M, :36], R=[pk[6]], W=K('L'))
            P.op('dve', lambda e, r=r: e.reduce_max(out=r['gmax'], in_=r['L'][:, 0:4], axis=AX.X), R=K('L'), W=K('gmax'))
            P.ts('dve', r['ohg'], r['L'][:, 0:4], r['gmax'], ALU.is_equal, R=K('L', 'gmax'), W=K('ohg'))
            P.ts('dve', r['ngmax'], r['gmax'], -1.0, ALU.mult, R=K('gmax'), W=K('ngmax'))
            P.act(r['eg'], r['L'][:, 0:4], AF.Exp, bias=r['ngmax'], R=K('L', 'ngmax'), W=K('eg'))
            P.op('dve', lambda e, r=r: e.reduce_sum(out=r['se'], in_=r['eg'], axis=AX.X), R=K('eg'), W=K('se'))
            P.op('dve', lambda e, r=r: e.reciprocal(out=r['ptop'], in_=r['se']), R=K('se'), W=K('ptop'))
            P.ts('dve', r['lesel'], r['L'][:, 4:12], r['ohg'][:, 0:1], ALU.mult, R=K('L', 'ohg'), W=K('lesel'))
            for g in range(1, 4):
                P.stt('dve', r['lesel'], r['L'][:, 4 + 8 * g:12 + 8 * g], r['ohg'][:, g:g + 1], r['lesel'],
                      ALU.mult, ALU.add, R=K('L', 'ohg', 'lesel'), W=K('lesel'))
            P.op('dve', lambda e, r=r: e.reduce_max(out=r['m1'], in_=r['lesel'], axis=AX.X), R=K('lesel'), W=K('m1'))
            P.ts('dve', r['oh1'], r['lesel'], r['m1'], ALU.is_equal, R=K('lesel', 'm1'), W=K('oh1'))
            P.stt('dve', r['msk'], r['oh1'], -1e30, r['lesel'], ALU.mult, ALU.add, R=K('oh1', 'lesel'), W=K('msk'))
            P.op('dve', lambda e, r=r: e.reduce_max(out=r['m2'], in_=r['msk'], axis=AX.X), R=K('msk'), W=K('m2'))
            P.ts('dve', r['oh2'], r['msk'], r['m2'], ALU.is_equal, R=K('msk', 'm2'), W=K('oh2'))
            P.tt('dve', r['d'], r['m2'], r['m1'], ALU.subtract, R=K('m1', 'm2'), W=K('d'))
            P.act(r['ed'], r['d'], AF.Exp, R=K('d'), W=K('ed'))
            P.ts('dve', r['w1'], r['ed'], 1.0, ALU.add, R=K('ed'), W=K('w1'))
            P.op('dve', lambda e, r=r: e.reciprocal(out=r['w1'], in_=r['w1']), R=K('w1'), W=K('w1'))
            P.tt('dve', r['w2'], r['ed'], r['w1'], ALU.mult, R=K('ed', 'w1'), W=K('w2'))
            P.tt('dve', r['w1'], r['w1'], r['ptop'], ALU.mult, R=K('w1', 'ptop'), W=K('w1'))
            P.tt('dve', r['w2'], r['w2'], r['ptop'], ALU.mult, R=K('w2', 'ptop'), W=K('w2'))
            P.ts('dve', r['ce'], r['oh1'], r['w1'], ALU.mult, R=K('oh1', 'w1'), W=K('ce'))
            P.stt('dve', r['ce'], r['oh2'], r['w2'], r['ce'], ALU.mult, ALU.add, R=K('oh2', 'w2', 'ce'), W=K('ce'))
            for g in range(4):
                P.ts('dve', r['comb'][:, 8 * g:8 * g + 8], r['ce'], r['ohg'][:, g:g + 1], ALU.mult,
                     R=K('ce', 'ohg', 'comb'), W=K('comb'))
            P.transpose(pb[7][:32, :M], r['comb'], ident[:M, :M], R=K('comb') + ['ident'], W=[pk[7]])
            P.copy('dve', combT[:, t0:t0 + M], pb[7][:32, :M], R=[pk[7], 'combT'], W=['combT'])
            t0 += M
        wst_i = 0
        for e in range(NE):
            cbk = pk[4 + e % 2]; cbp = pb[4 + e % 2]
            P.dma(selb[e % 2][:], c_sel[:, e * 128:(e + 1) * 128], W=[('selb', e % 2)], q='pool')
            P.mm(cbp[:, :N], selb[e % 2][:], combT[:, :], R=[('selb', e % 2), 'combT'], W=[cbk])
            for f in range(2):
                bi = (2 * e + f) % 2
                wi = (2 * e + f) % NWB
                wbt = wb[wi]; wbk = ('wb', wi)
                cgu = wc_gu[2 * e + f].rearrange("p (t n) -> p t n", t=16)
                if c == 0:
                    for gi, wsrc in enumerate((w_gate, w_up)):
                        for q2 in range(2):
                            st = wst[wst_i % 3]; sk = ('wst', wst_i % 3); wst_i += 1
                            P.dma(st[:], wsrc[e, q2 * 1024:(q2 + 1) * 1024, f * 128:(f + 1) * 128].rearrange(
                                "(t p) n -> p t n", p=128), W=[sk], q='sp')
                            cast(wbt[:, q2 * 8:(q2 + 1) * 8, gi * 128:(gi + 1) * 128], st[:], R=[sk, wbk], W=[wbk])
                    P.dma(cgu, wbt[:], R=[wbk], W=[('wc_gu', 2 * e + f)], q='pool')
                else:
                    P.dma(wbt[:], cgu, R=[('wc_gu', 2 * e + f)], W=[wbk], q='sp')
                pg = pb[bi]; pu = pb[2 + bi]
                for dt in range(16):
                    P.mm(pg[:, :N], wbt[:, dt, 0:128], hb[:, dt, :], start=(dt == 0), stop=(dt == 15),
                         R=[wbk, ('hb', dt)], W=[pk[bi]])
                for dt in range(16):
                    P.mm(pu[:, :N], wbt[:, dt, 128:256], hb[:, dt, :], start=(dt == 0),
                         stop=(dt == 15), R=[wbk, ('hb', dt)], W=[pk[2 + bi]])
                P.act(sg[bi][:, :], pg[:, :N], AF.Silu, R=[pk[bi]], W=[('sg', bi)])
                P.tt('dve', tq[bi][:, :], pu[:, :N], sg[bi][:, :], ALU.mult, R=[pk[2 + bi], ('sg', bi)], W=[('tq', bi)])
                P.tt('dve', hh[:, 2 * e + f, :], tq[bi][:, :], cbp[:, :N], ALU.mult, R=[('tq', bi), cbk],
                     W=[('hh', 2 * e + f)])
        for half in range(2):
            for e in range(NE):
                for ft in range(2):
                    i = (2 * e + ft) % 2
                    ci = (half * NE + e) * 2 + ft
                    wi = (2 * e + ft) % NWD
                    if c == 0:
                        P.dma(wdst[i][:], w_down[e, ft * 128:(ft + 1) * 128, half * 1024:(half + 1) * 1024],
                              W=[('wdst', i)], q='sp')
                        cast(wd[wi][:, 0, :], wdst[i][:], R=[('wdst', i)], W=[('wd', wi)])
                        P.dma(wc_d[ci], wd[wi][:, 0, :], R=[('wd', wi)], W=[('wc_d', ci)], q='pool')
                    else:
                        P.dma(wd[wi][:, 0, :], wc_d[ci], R=[('wc_d', ci)], W=[('wd', wi)], q='sp')
                    for j in range(8):
                        P.mm(pb[j][:, :N], wd[wi][:, 0, j * 128:(j + 1) * 128], hh[:, 2 * e + ft, :],
                             start=(e == 0 and ft == 0), stop=(e == NE - 1 and ft == 1),
                             R=[('wd', wi), ('hh', 2 * e + ft)], W=[pk[j]])
            for j in range(8):
                dt = half * 8 + j
                P.stt('dve', v[:, dt, :], v[:, dt, :], DN_ALPHA, pb[j][:, :N], ALU.mult, ALU.add,
                      R=[('v', dt), pk[j]], W=[('v', dt)])
        layer_norm_fm(P, v, N, lnp_sb[:, 32:48], lnp_sb[:, 48:64], pb[6], pb[7], pk[6], pk[7], ones_f, tmp, hb=None)
        P.dma(outT[:, cs].rearrange("(t p) n -> p t n", p=128), v[:], R=[('v', dt) for dt in range(16)], W=[('out32', c)], q='pool')
        if outb is not None:
            for dt in range(16):
                P.copy('act', hb[:, dt, :], v[:, dt, :], R=[('v', dt)], W=[('hb', dt)])
            for half in range(2):
                P.dma(outb[c][half].rearrange("(t p) n -> p t n", p=128), hb[:, 8 * half:8 * half + 8, :],
                      R=[('hb', dt) for dt in range(16)], W=[('outb', c, half)], q='pool')
                P.allgather(outb_all[c][half], outb[c][half], groups, R=[('outb', c, half)], W=[('outb_all', c, half)])

import numpy as np
PAD = 496

def c_consts():
    U = np.triu(np.ones((128, 128), np.float32))
    return {"c_U": U, "c_ident": np.eye(128, dtype=np.float32)}

def prep_ab(inp, j):
    w = inp['ab_w_in'][0]
    cols = np.concatenate([np.arange(256 * j, 256 * j + 256), 1024 + np.arange(128 * j, 128 * j + 128),
                           1536 + np.arange(128 * j, 128 * j + 128), 2048 + np.arange(256 * j, 256 * j + 256),
                           3088 + np.arange(256 * j, 256 * j + 256), np.arange(3072, 3088)])
    d = {"w_in": np.ascontiguousarray(w[:, cols])}
    a_re = inp['ab_s5_a_re'][0]; a_im = inp['ab_s5_a_im'][0]; ldt = inp['ab_s5_log_dt'][0]
    B = [inp['ab_s5_b_re'][0], inp['ab_s5_b_im'][0]]; C = [inp['ab_s5_c_re'][0], inp['ab_s5_c_im'][0]]
    par = np.zeros((128, 8, 3), np.float32)
    BT = np.zeros((128, 2, 8, 128), np.float32); CT = np.zeros((128, 2, 8, 128), np.float32)
    for st in range(8):
        for g2 in range(2):
            g = 16 * j + 2 * st + g2
            gl = (2 * st + g2) % 8
            ps = slice(g2 * 64, g2 * 64 + 64)
            par[ps, st, 0] = a_re[g]; par[ps, st, 1] = a_im[g]; par[ps, st, 2] = ldt[g]
            for ri in range(2):
                BT[gl * 16:(gl + 1) * 16, ri, st, ps] = B[ri][g].T
                CT[ps, ri, st, gl * 16:(gl + 1) * 16] = C[ri][g].T
    d["s5par"] = par; d["s5BT"] = BT; d["s5CT"] = CT
    d["s5d"] = np.ascontiguousarray(inp['ab_s5_d'][0][256 * j:256 * j + 256].reshape(2, 128).T)
    d["gla_wg"] = np.ascontiguousarray(inp['ab_gla_w_gate'][0][:, 128 * j:128 * j + 128])
    d["gla_bg"] = np.ascontiguousarray(inp['ab_gla_b_gate'][0][128 * j:128 * j + 128, None])
    d["gla_ng"] = np.ascontiguousarray(np.broadcast_to(inp['ab_gla_norm'][0][None, :], (128, 256)))
    d.update(c_consts())
    return d

def prep_hT(h, Tp):
    L = h.shape[0]
    out = np.zeros((h.shape[1], Tp), np.float32)
    out[:, Tp - L:] = h.T
    return out

def prep_cd(inp, j):
    w = inp['cd_w_in'][0]
    hs = [2 * j, 2 * j + 1]
    blk = lambda base, i: base + np.arange(i * 128, i * 128 + 128)
    tiles = [blk(0, hs[0]), blk(0, hs[1]), blk(1024, hs[0]), blk(1024, hs[1]), blk(2048, hs[0]), blk(2048, hs[1]),
             blk(4112, hs[0]), blk(4112, hs[1]), blk(3072, hs[0]), blk(3072, hs[1]), blk(5136, hs[0]), blk(5136, hs[1]),
             np.array([4096 + hs[0], 4096 + hs[1], 4104 + hs[0], 4104 + hs[1]])]
    d = {"w_in": np.ascontiguousarray(w[:, np.concatenate(tiles)])}
    cw = np.zeros((128, 8, 4), np.float32); cb = np.zeros((128, 8), np.float32)
    for i in range(6):
        cw[:, i, :] = inp['cd_conv_w'][0][:, tiles[i]].T
    for b in range(2):
        cw[:, 6 + b, :] = inp['cd_lru_conv_w'][0][:, blk(0, hs[b])].T
        cb[:, 6 + b] = inp['cd_lru_conv_b'][0][blk(0, hs[b])]
    d["convw"] = cw; d["convb"] = cb
    gp = np.zeros((4, 4), np.float32)
    for h in range(2):
        gp[2 + h, 0] = inp['cd_gdn_dt_bias'][0][hs[h]]; gp[2 + h, 1] = inp['cd_gdn_a_log'][0][hs[h]]
    gp[0:2, 2] = 1.0; gp[2:4, 3] = 1.0
    d["gpar"] = gp
    d["gdn_ng"] = np.ascontiguousarray(np.broadcast_to(inp['cd_gdn_norm'][0][None, :], (128, 128)))
    d["lwa"] = np.ascontiguousarray(np.stack([inp['cd_lru_w_a'][0][hs[b]] for b in range(2)], 1))
    d["lwx"] = np.ascontiguousarray(np.stack([inp['cd_lru_w_x'][0][hs[b]] for b in range(2)], 1))
    lp = np.zeros((128, 2, 3), np.float32)
    for b in range(2):
        lp[:, b, 0] = inp['cd_lru_b_a'][0][blk(0, hs[b])]; lp[:, b, 1] = inp['cd_lru_b_x'][0][blk(0, hs[b])]
        lp[:, b, 2] = inp['cd_lru_lambda'][0][blk(0, hs[b])]
    d["lpar"] = lp
    d.update(c_consts())
    d["c_Us"] = np.triu(np.ones((128, 128), np.float32), 1)
    sel4 = np.zeros((4, 512), np.float32)
    for r in range(4):
        sel4[r, r * 128:(r + 1) * 128] = 1.0
    d["c_sel4"] = sel4
    return d


N_META = 16
SEQ = 8192
NCHUNK = 17
TP = NCHUNK * CH
POST_NCH, POST_N = 6, 342
T2 = POST_NCH * POST_N
G4 = [[0, 1, 2, 3], [4, 5, 6, 7]]


def build_fused():
    P = Prog()
    nc = P.nc
    pb = [P.ps("pb%d" % i, [128, 512]) for i in range(8)]
    pk = ['pb%d' % i for i in range(8)]
    hT = P.dram("hT", [D, TP]); hq = P.dram("hq", [D, T2]); esel = P.dram("esel", [128, 4])
    outT = P.dram("outT", [D, T2], kind="ExternalOutput")
    idram = lambda n, sh, dt: nc.dram_tensor(n, sh, dt, kind="Internal").ap()
    y0_loc = [idram("y0_loc%d" % c, [512, CH], BF16) for c in range(NCHUNK)]
    y0_all = [idram("y0_all%d" % c, [4 * 512, CH], BF16) for c in range(NCHUNK)]
    y1_loc = [idram("y1_loc%d" % c, [512, CH], BF16) for c in range(NCHUNK)]
    y1_all = [idram("y1_all%d" % c, [4 * 512, CH], BF16) for c in range(NCHUNK)]
    h1_32 = idram("h1_32", [D, T2], F32)
    h1_b = [[idram("h1_b%d_%d" % (c, h), [1024, POST_N], BF16) for h in range(2)] for c in range(POST_NCH)]
    h1_all = [[idram("h1_all%d_%d" % (c, h), [4 * 1024, POST_N], BF16) for h in range(2)] for c in range(POST_NCH)]

    with P.scope():
        phase_ab(P, pb, pk, NCHUNK, hT, y0_loc, y0_all, G4)
    P.new_phase()
    with P.scope():
        phase_post(P, pb, pk, True, "p0_", y0_all, hq, h1_32, h1_b, h1_all, G4, esel, POST_NCH, POST_N)
    P.new_phase()
    with P.scope():
        phase_cd(P, pb, pk, NCHUNK, h1_all, y1_loc, y1_all, G4)
    P.new_phase()
    with P.scope():
        phase_post(P, pb, pk, False, "p1_", y1_all, h1_32, outT, None, None, G4, esel, POST_NCH, POST_N)
    return P.finalize(), P


def _post_weights(inp, layer, glu, pre):
    pt = lambda a: np.ascontiguousarray(a.reshape(-1, 128).T)
    d = {"w_out": (inp['ab_w_out'][0] if glu else inp['cd_w_out'][0]),
         "lnp": np.concatenate([pt(inp['ln_mix_g'][layer]), pt(inp['ln_mix_b'][layer]),
                                pt(inp['ln_ffn_g'][layer]), pt(inp['ln_ffn_b'][layer])], 1),
         "w_r": np.ascontiguousarray(np.concatenate([inp['moe_w_router_g'][layer],
                                                     inp['moe_w_router_e'][layer].reshape(D, 32)], 1)),
         "b_r": np.concatenate([inp['moe_b_router_g'][layer], inp['moe_b_router_e'][layer].reshape(32)])[None],
         "w_gate": inp['moe_w_gate'][layer].reshape(32, D, FF), "w_up": inp['moe_w_up'][layer].reshape(32, D, FF),
         "w_down": inp['moe_w_down'][layer].reshape(32, FF, D),
         "c_ident": np.eye(128, dtype=np.float32),
         "c_sel": np.ascontiguousarray(np.repeat(np.eye(32, dtype=np.float32), 128, axis=1))}
    if glu:
        d["w_glu"] = inp['ab_s5_w_glu'][0]
        d["b_glu"] = pt(inp['ab_s5_b_glu'][0])
    return {pre + k: v for k, v in d.items()}


def kernel(**inputs):
    inp = {k: np.asarray(v, dtype=np.float32) for k, v in inputs.items()}
    x = inp['x']
    B = x.shape[0]
    L = N_META + SEQ
    h0 = np.concatenate([np.broadcast_to(inp['meta_tokens'][None], (B, N_META, D)), x], axis=1)
    hTs = [prep_hT(h0[b], TP) for b in range(B)]
    pw0 = _post_weights(inp, 0, True, "p0_"); pw1 = _post_weights(inp, 1, False, "p1_")
    ab = [{"a_" + k: v for k, v in prep_ab(inp, j).items()} for j in range(4)]
    cd = [{"c_" + k: v for k, v in prep_cd(inp, j).items()} for j in range(4)]
    maps = []
    for i in range(8):
        b, j = i // 4, i % 4
        d = {"hT": hTs[b], "hq": np.ascontiguousarray(h0[b, j * T2:(j + 1) * T2].T)}
        es = np.zeros((128, 4), np.float32); es[:, j] = 1.0
        d["esel"] = es
        d.update(ab[j]); d.update(cd[j]); d.update(pw0); d.update(pw1)
        maps.append(d)
    nc, _ = build_fused()
    res = run_bass_kernel_spmd(nc, maps, core_ids=list(range(8)))
    out = np.zeros((B, L, D), np.float32)
    for i in range(8):
        b, j = i // 4, i % 4
        out[b, j * T2:(j + 1) * T2] = np.asarray(res.results[i]["outT"]).T
    return np.ascontiguousarray(out[:, N_META:])
```

```python
import numpy as np
import os
from contextlib import ExitStack
import concourse.bass as bass
import concourse.mybir as mybir
from concourse.bass_utils import run_bass_kernel_spmd

F32 = mybir.dt.float32
BF16 = mybir.dt.bfloat16
ALU = mybir.AluOpType
AF = mybir.ActivationFunctionType
AX = mybir.AxisListType

ENGS = ['pe', 'act', 'dve', 'pool', 'sp']
N_DMA_SEMS = 12
SAME_ENGINE_SYNC = True


class Prog:
    def __init__(self):
        self.nc = bass.Bass("TRN2", target_bir_lowering=False)
        self.es = ExitStack()
        self.ops = {e: [] for e in ENGS}
        self.cnt = {e: 0 for e in ENGS}
        self.seen = {e: {} for e in ENGS}
        self.last_w = {}
        self.readers = {}
        self.dma_cnt = [0] * N_DMA_SEMS
        self.dma_rr = 0
        self.sems = {}
        self.n_ops = 0
        self.phase = 0
        self.barrier_toks = {e: [] for e in ENGS}
        self.scopes = []
        self.cc_cnt = 0
        self._alloc_sems()

    def _alloc_sems(self):
        p = self.phase
        for e in ENGS:
            self.sems[(e, p)] = self.es.enter_context(self.nc.semaphore("s_%s_%d" % (e, p)))
        for j in range(N_DMA_SEMS):
            self.sems[(('dma', j), p)] = self.es.enter_context(self.nc.semaphore("s_dma%d_%d" % (j, p)))

    def new_phase(self):
        p = self.phase
        toks = [((e, p), self.cnt[e]) for e in ENGS if self.cnt[e] > 0]
        toks += [((('dma', j), p), self.dma_cnt[j]) for j in range(N_DMA_SEMS) if self.dma_cnt[j] > 0]
        if self.cc_cnt > 0:
            toks.append((('cc', 0), self.cc_cnt))
        for e in ENGS:
            self.barrier_toks[e] = self.barrier_toks[e] + toks
        self.phase += 1
        self._alloc_sems()
        self.cnt = {e: 0 for e in ENGS}
        self.dma_cnt = [0] * N_DMA_SEMS
        self.last_w = {}
        self.readers = {}

    def scope(self):
        prog = self

        class _S:
            def __enter__(self_):
                prog.scopes.append(ExitStack())

            def __exit__(self_, *a):
                prog.scopes.pop().close()
                return False
        return _S()

    def sb(self, name, shape, dt=F32):
        es = self.scopes[-1] if self.scopes else self.es
        return es.enter_context(self.nc.sbuf_tensor("p%d_%s" % (self.phase, name), list(shape), dt))

    def ps(self, name, shape, dt=F32):
        return self.es.enter_context(self.nc.psum_tensor(name, list(shape), dt))

    def dram(self, name, shape, dt=F32, kind="ExternalInput"):
        return self.nc.dram_tensor(name, list(shape), dt, kind=kind).ap()

    def tok(self, name, val):
        return ((name, self.phase), val)

    def _deps(self, R, W):
        toks = []
        for k in R:
            if k in self.last_w:
                toks.append(self.last_w[k] + (True,))
        for k in W:
            if k in self.last_w:
                toks.append(self.last_w[k] + (False,))
            for t in self.readers.get(k, {}).items():
                toks.append(t + (False,))
        return toks

    def _mark(self, tok, R, W):
        for k in R:
            self.readers.setdefault(k, {})[tok[0]] = tok[1]
        for k in W:
            self.last_w[k] = tok
            self.readers[k] = {}

    def _waits(self, eng, toks):
        need = {}
        for t in toks:
            s, v = t[0], t[1]
            raw = t[2] if len(t) > 2 else True
            if s[0] == eng and (eng in ('pe', 'sp') or not SAME_ENGINE_SYNC or not raw):
                continue
            if self.seen[eng].get(s, 0) >= v:
                continue
            need[s] = max(need.get(s, 0), v)
        for s, v in need.items():
            self.seen[eng][s] = v
        return list(need.items())

    @staticmethod
    def _excl(R, W):
        ps = [k for k in R if isinstance(k, str) and k.startswith('pb')]
        if ps:
            R = [k for k in R if k not in ps]
            W = list(W) + ps
        return R, W

    def _bar(self, eng):
        b = self.barrier_toks[eng]
        self.barrier_toks[eng] = []
        return b

    def op(self, eng, fn, R=(), W=()):
        R, W = self._excl(R, W)
        waits = self._waits(eng, self._deps(R, W) + self._bar(eng))
        self.cnt[eng] += 1
        tok = self.tok(eng, self.cnt[eng])
        self.ops[eng].append((waits, fn, ((eng, self.phase), 1)))
        self._mark(tok, R, W)
        self.n_ops += 1

    def dma(self, out, in_, R=(), W=(), q='sp', **kw):
        if q == 'pool' and os.environ.get('NO_SWDGE'):
            q = 'sp'
        j = self.dma_rr
        self.dma_rr = (self.dma_rr + 1) % N_DMA_SEMS
        toks = self._deps(R, W) + self._bar(q)
        if self.dma_cnt[j] > 0:
            toks.append(self.tok(('dma', j), self.dma_cnt[j]))
        waits = self._waits(q, toks)
        self.dma_cnt[j] += 16
        tok = self.tok(('dma', j), self.dma_cnt[j])
        self.ops[q].append((waits, lambda e: e.dma_start(out=out, in_=in_, **kw), ((('dma', j), self.phase), 16)))
        self._mark(tok, R, W)
        self.n_ops += 1

    def allgather(self, out, in_, groups, R=(), W=()):
        if ('cc', 0) not in self.sems:
            self.sems[('cc', 0)] = self.es.enter_context(self.nc.semaphore("s_cc"))
        toks = self._deps(R, W) + self._bar('pool')
        waits = self._waits('pool', toks)
        self.cc_cnt += 1
        tok = (('cc', 0), self.cc_cnt)
        self.ops['pool'].append((waits, lambda e: e.collective_compute(
            "AllGather", ALU.bypass, replica_groups=groups, ins=[in_], outs=[out]), (('cc', 0), None)))
        self._mark(tok, R, W)
        self.n_ops += 1

    def mm(self, out, lhsT, rhs, start=True, stop=True, R=(), W=()):
        self.op('pe', lambda e: e.matmul(out, lhsT, rhs, start=start, stop=stop), R, W)

    def transpose(self, out, in_, ident, R=(), W=()):
        self.op('pe', lambda e: e.transpose(out, in_, ident), R, W)

    def act(self, out, in_, func, bias=None, scale=1.0, R=(), W=(), accum_out=None):
        kw = {}
        if bias is not None:
            kw['bias'] = bias
        if accum_out is not None:
            kw['accum_out'] = accum_out
        self.op('act', lambda e: e.activation(out=out, in_=in_, func=func, scale=scale, **kw), R, W)

    def copy(self, eng, out, in_, R=(), W=()):
        if eng == 'act':
            self.op('act', lambda e: e.copy(out=out, in_=in_), R, W)
        else:
            self.op(eng, lambda e: e.tensor_copy(out=out, in_=in_), R, W)

    def tt(self, eng, out, a, b, op, R=(), W=()):
        self.op(eng, lambda e: e.tensor_tensor(out=out, in0=a, in1=b, op=op), R, W)

    def ts(self, eng, out, a, s1, op0, s2=None, op1=None, R=(), W=()):
        if op1 is None:
            self.op(eng, lambda e: e.tensor_scalar(out=out, in0=a, scalar1=s1, scalar2=None, op0=op0), R, W)
        else:
            self.op(eng, lambda e: e.tensor_scalar(out=out, in0=a, scalar1=s1, scalar2=s2, op0=op0, op1=op1), R, W)

    def stt(self, eng, out, a, s, b, op0, op1, R=(), W=()):
        eng = 'dve'
        self.op(eng, lambda e: e.scalar_tensor_tensor(out=out, in0=a, scalar=s, in1=b, op0=op0, op1=op1), R, W)

    def memset(self, eng, ap, val, W=()):
        self.op(eng, lambda e: e.memset(ap, val), (), W)

    def finalize(self):
        nc = self.nc
        fin = list(self.barrier_toks['sp'])
        for j in range(N_DMA_SEMS):
            if self.dma_cnt[j] > 0:
                fin.append(self.tok(('dma', j), self.dma_cnt[j]))
        for e in ENGS:
            if e != 'sp' and self.cnt[e] > 0:
                fin.append(self.tok(e, self.cnt[e]))
        if self.cc_cnt > 0:
            fin.append((('cc', 0), self.cc_cnt))
        ops = self.ops
        sems = self.sems

        def run(engobj, name):
            for waits, fn, (s, inc) in ops[name]:
                for ws, wv in waits:
                    engobj.wait_ge(sems[ws], wv)
                ins = fn(engobj)
                if inc is None:
                    ins.then_inc(sems[s])
                else:
                    ins.then_inc(sems[s], inc)
            if name == 'sp':
                for ws, wv in fin:
                    engobj.wait_ge(sems[ws], wv)

        with nc.Block() as block:
            @block.sync
            def _(e):
                run(e, 'sp')

            if ops['pe']:
                @block.tensor
                def _(e):
                    run(e, 'pe')
            if ops['act']:
                @block.scalar
                def _(e):
                    run(e, 'act')
            if ops['dve']:
                @block.vector
                def _(e):
                    run(e, 'dve')
            if ops['pool']:
                @block.gpsimd
                def _(e):
                    run(e, 'pool')
        self.es.close()
        return nc

import math

D = 2048
CH = 512
PAD = 496


def gelu_tanh(P, out, x, tA, tB, N, kx, kout, ktmp):
    P.tt('pool', tA[:, :N], x, x, ALU.mult, R=[kx], W=[ktmp[0]])
    P.ts('pool', tA[:, :N], tA[:, :N], 0.044715, ALU.mult, 1.0, ALU.add, R=[ktmp[0]], W=[ktmp[0]])
    P.tt('pool', tA[:, :N], tA[:, :N], x, ALU.mult, R=[ktmp[0], kx], W=[ktmp[0]])
    P.act(tB[:, :N], tA[:, :N], AF.Sigmoid, scale=1.5957691216057308, R=[ktmp[0]], W=[ktmp[1]])
    P.tt('pool', out, x, tB[:, :N], ALU.mult, R=[kx, ktmp[1]], W=[kout])


def inproj_setup(P, w_in, ncols, cast_engs=('act', 'pool')):
    wb = P.sb("win_b", [128, 16, ncols], BF16)
    stg = [P.sb("win_st%d" % i, [128, ncols]) for i in range(2)]
    for dt in range(16):
        i = dt % 2
        P.dma(stg[i][:], w_in[dt * 128:(dt + 1) * 128, :], W=[('win_st', i)])
        P.copy(cast_engs[dt % 2], wb[:, dt, :], stg[i][:], R=[('win_st', i)], W=['win_b'])
    return wb


def load_x_chunk(P, hT, c, xst, xb):
    for dt in range(16):
        i = dt % 4
        P.dma(xst[i][:], hT[dt * 128:(dt + 1) * 128, c * CH:(c + 1) * CH], W=[('xst', i)], q='sp')
        P.copy(('act', 'pool')[dt % 2], xb[:, dt, :], xst[i][:], R=[('xst', i)], W=[('xb', dt)])


def proj_tile(P, wb, col0, M, xb, ps, pskey):
    for dt in range(16):
        P.mm(ps[:M, :CH], wb[:, dt, col0:col0 + M], xb[:, dt, :], start=(dt == 0), stop=(dt == 15),
             R=['win_b', ('xb', dt)], W=[pskey])


def phase_ab(P, pb, pk, NCHUNK, hT, y_loc, y_all, groups, pre="a_"):
    Tp = NCHUNK * CH
    NCOL = 1040
    w_in = P.dram(pre + "w_in", [D, NCOL])
    s5par = P.dram(pre + "s5par", [128, 8, 3])
    s5BT = P.dram(pre + "s5BT", [128, 2, 8, 128])
    s5CT = P.dram(pre + "s5CT", [128, 2, 8, 128])
    s5d = P.dram(pre + "s5d", [128, 2])
    gla_wg = P.dram(pre + "gla_wg", [16, 128]); gla_bg = P.dram(pre + "gla_bg", [128, 1]); gla_ng = P.dram(pre + "gla_ng", [128, 256])
    c_U = P.dram(pre + "c_U", [128, 128]); c_ident = P.dram(pre + "c_ident", [128, 128])
    ykeys = []
    wb = inproj_setup(P, w_in, NCOL)
    xst = [P.sb("xst%d" % i, [128, CH]) for i in range(4)]
    xb = P.sb("xb", [128, 16, CH], BF16)
    names = ['u0', 'u1', 'q', 'k', 'v0', 'v1', 'r0', 'r1']
    pt = {n: P.sb("pt_" + n, [128, CH]) for n in names}
    glT = P.sb("glT", [16, CH])
    U = P.sb("U", [128, 128]); ident = P.sb("ident", [128, 128]); identb = None
    onescol = P.sb("onescol", [128, 1])
    P.dma(U[:], c_U, W=['U']); P.dma(ident[:], c_ident, W=['ident'])
    P.memset('dve', onescol[:], 1.0, W=['onescol'])

    par = P.sb("s5par_sb", [128, 8, 3]); P.dma(par[:], s5par, W=['par'])
    BTb = P.sb("BTb", [128, 2, 8, 128], BF16); CTb = P.sb("CTb", [128, 2, 8, 128], BF16)
    stg128 = [P.sb("stg128_%d" % i, [128, 128]) for i in range(4)]
    dcol = P.sb("dcol", [128, 2]); P.dma(dcol[:], s5d, W=['dcol'])
    Er = P.sb("Er", [128, 8, CH]); Ei = P.sb("Ei", [128, 8, CH])
    sc = {k: P.sb("sc_" + k, [128, 8]) for k in
          ['dt', 'th', 'r', 'c', 's', 'c2', 's2', 't', 'cr', 'ci', 'nr', 'ni', 'den', 'zr', 'zi', 'x', 'y']}
    SK = lambda *n: ['sc_' + a for a in n]
    halfpi = P.sb("halfpi", [128, 1]); P.memset('dve', halfpi[:], math.pi / 2, W=['halfpi'])
    P.act(sc['dt'][:], par[:, :, 2], AF.Exp, R=['par'], W=SK('dt'))
    P.tt('dve', sc['th'][:], par[:, :, 1], sc['dt'][:], ALU.mult, R=['par'] + SK('dt'), W=SK('th'))
    P.tt('dve', sc['r'][:], par[:, :, 0], sc['dt'][:], ALU.mult, R=['par'] + SK('dt'), W=SK('r'))
    P.act(sc['r'][:], sc['r'][:], AF.Exp, R=SK('r'), W=SK('r'))
    P.act(sc['s'][:], sc['th'][:], AF.Sin, scale=1.0 / 16, R=SK('th'), W=SK('s'))
    P.act(sc['c'][:], sc['th'][:], AF.Sin, scale=1.0 / 16, bias=halfpi[:], R=SK('th') + ['halfpi'], W=SK('c'))
    for _ in range(4):
        P.tt('dve', sc['c2'][:], sc['c'][:], sc['c'][:], ALU.mult, R=SK('c'), W=SK('c2'))
        P.tt('dve', sc['s2'][:], sc['s'][:], sc['s'][:], ALU.mult, R=SK('s'), W=SK('s2'))
        P.tt('dve', sc['t'][:], sc['c'][:], sc['s'][:], ALU.mult, R=SK('c', 's'), W=SK('t'))
        P.tt('dve', sc['c'][:], sc['c2'][:], sc['s2'][:], ALU.subtract, R=SK('c2', 's2'), W=SK('c'))
        P.ts('dve', sc['s'][:], sc['t'][:], 2.0, ALU.mult, R=SK('t'), W=SK('s'))
    P.tt('dve', sc['nr'][:], sc['r'][:], sc['c'][:], ALU.mult, R=SK('r', 'c'), W=SK('nr'))
    P.ts('dve', sc['nr'][:], sc['nr'][:], -1.0, ALU.add, R=SK('nr'), W=SK('nr'))
    P.tt('dve', sc['ni'][:], sc['r'][:], sc['s'][:], ALU.mult, R=SK('r', 's'), W=SK('ni'))
    P.tt('dve', sc['den'][:], par[:, :, 0], par[:, :, 0], ALU.mult, R=['par'], W=SK('den'))
    P.tt('dve', sc['x'][:], par[:, :, 1], par[:, :, 1], ALU.mult, R=['par'], W=SK('x'))
    P.tt('dve', sc['den'][:], sc['den'][:], sc['x'][:], ALU.add, R=SK('den', 'x'), W=SK('den'))
    P.op('dve', lambda e: e.reciprocal(out=sc['den'][:], in_=sc['den'][:]), R=SK('den'), W=SK('den'))
    P.tt('dve', sc['x'][:], sc['nr'][:], par[:, :, 0], ALU.mult, R=SK('nr') + ['par'], W=SK('x'))
    P.tt('dve', sc['y'][:], sc['ni'][:], par[:, :, 1], ALU.mult, R=SK('ni') + ['par'], W=SK('y'))
    P.tt('dve', sc['zr'][:], sc['x'][:], sc['y'][:], ALU.add, R=SK('x', 'y'), W=SK('zr'))
    P.tt('dve', sc['zr'][:], sc['zr'][:], sc['den'][:], ALU.mult, R=SK('zr', 'den'), W=SK('zr'))
    P.tt('dve', sc['x'][:], sc['ni'][:], par[:, :, 0], ALU.mult, R=SK('ni') + ['par'], W=SK('x'))
    P.tt('dve', sc['y'][:], sc['nr'][:], par[:, :, 1], ALU.mult, R=SK('nr') + ['par'], W=SK('y'))
    P.tt('dve', sc['zi'][:], sc['x'][:], sc['y'][:], ALU.subtract, R=SK('x', 'y'), W=SK('zi'))
    P.tt('dve', sc['zi'][:], sc['zi'][:], sc['den'][:], ALU.mult, R=SK('zi', 'den'), W=SK('zi'))
    tmpc = P.sb("tmpc", [128, 256])
    for st in range(8):
        c1 = sc['c'][:, st:st + 1]; s1 = sc['s'][:, st:st + 1]
        eng = 'dve' if st % 2 == 0 else 'pool'
        P.copy(eng, Er[:, st, 0:1], c1, R=SK('c'), W=['Er']); P.copy(eng, Ei[:, st, 0:1], s1, R=SK('s'), W=['Ei'])
        m = 1
        while m < CH:
            ar = Er[:, st, m - 1:m]; ai = Ei[:, st, m - 1:m]
            P.ts(eng, tmpc[:, :m], Ei[:, st, 0:m], ai, ALU.mult, R=['Ei'], W=['tmpc'])
            P.stt(eng, Er[:, st, m:2 * m], Er[:, st, 0:m], ar, tmpc[:, :m], ALU.mult, ALU.subtract, R=['Er', 'tmpc'], W=['Er'])
            P.ts(eng, tmpc[:, :m], Ei[:, st, 0:m], ar, ALU.mult, R=['Ei', 'Er'], W=['tmpc'])
            P.stt(eng, Ei[:, st, m:2 * m], Er[:, st, 0:m], ai, tmpc[:, :m], ALU.mult, ALU.add, R=['Er', 'tmpc', 'Ei'], W=['Ei'])
            m *= 2
        zr = sc['zr'][:, st:st + 1]; zi = sc['zi'][:, st:st + 1]
        for ri in range(2):
            P.dma(stg128[ri][:], s5BT[:, ri, st, :], W=[('stg128', ri)], q='sp')
            P.copy('act', BTb[:, ri, st, :], stg128[ri][:], R=[('stg128', ri)], W=['BTb'])
        for ri in range(2):
            P.dma(stg128[2 + ri][:], s5CT[:, ri, st, :], W=[('stg128', 2 + ri)], q='sp')
        P.ts(eng, tmpc[:, :128], stg128[3][:], zi, ALU.mult, R=[('stg128', 3)] + SK('zi'), W=['tmpc'])
        P.stt(eng, CTb[:, 0, st, :], stg128[2][:], zr, tmpc[:, :128], ALU.mult, ALU.subtract,
              R=[('stg128', 2), 'tmpc'] + SK('zr'), W=['CTb'])
        P.ts(eng, tmpc[:, :128], stg128[3][:], zr, ALU.mult, R=[('stg128', 3), 'CTb'] + SK('zr'), W=['tmpc'])
        P.stt(eng, tmpc[:, 128:256], stg128[2][:], zi, tmpc[:, :128], ALU.mult, ALU.add,
              R=[('stg128', 2), 'tmpc'] + SK('zi'), W=['tmpc'])
        P.ts(eng, CTb[:, 1, st, :], tmpc[:, 128:256], -1.0, ALU.mult, R=['tmpc'], W=['CTb'])
    carry = P.sb("carry", [128, 8, 2]); P.memset('dve', carry[:], 0.0, W=['carry'])
    s5t = {k: [P.sb("s5_%s%d" % (k, i), [128, CH]) for i in range(2)] for k in ['a', 'b', 'c', 'd']}
    sre = [P.sb("sre%d" % i, [128, CH]) for i in range(2)]; sim = [P.sb("sim%d" % i, [128, CH]) for i in range(2)]
    sreb = [P.sb("sreb%d" % i, [128, CH], BF16) for i in range(2)]; simb = [P.sb("simb%d" % i, [128, CH], BF16) for i in range(2)]
    ub = [P.sb("ub%d" % i, [128, CH], BF16) for i in range(2)]
    zt = [P.sb("zt%d" % i, [128, CH]) for i in range(2)]
    ztb = [P.sb("ztb%d" % i, [128, CH], BF16) for i in range(2)]
    obc = P.sb("obc", [128, 2, CH], BF16)
    gA = P.sb("gA", [128, CH]); gB = P.sb("gB", [128, CH])

    wg = P.sb("wg", [16, 128]); nbg = P.sb("nbg", [128, 1]); ngrep = P.sb("ngrep", [128, 256])
    P.dma(wg[:], gla_wg, W=['wg']); P.dma(nbg[:], gla_bg, W=['nbg']); P.dma(ngrep[:], gla_ng, W=['ngrep'])
    P.ts('dve', nbg[:], nbg[:], -1.0, ALU.mult, R=['nbg'], W=['nbg'])
    S = P.sb("S", [128, 256]); Sb = P.sb("Sb", [128, 256], BF16)
    P.memset('dve', S[:], 0.0, W=['S']); P.memset('dve', Sb[:], 0.0, W=['Sb'])
    gk = P.sb("gk", [128, CH]); bcum = P.sb("bcum", [128, CH])
    gt = {k: P.sb("g_" + k, [128, 128]) for k in ['eb', 'enb', 'ekst', 'kstT']}
    gtb = {k: P.sb("gb_" + k, [128, 128], BF16) for k in ['qin', 'kin', 'kst', 'att']}
    vtok = P.sb("vtok", [128, 256], BF16); rtok = P.sb("rtok", [128, 256]); osb = P.sb("osb", [128, 256])
    rsil = [P.sb("rsil%d" % i, [128, CH]) for i in range(2)]
    gsm = {k: P.sb("gsm_" + k, [128, 1]) for k in ['dec', 'ss', 'rstd', 'junk']}
    junk = P.sb("junk", [128, 256])
    GK = lambda *n: ['g_' + a for a in n]

    for c in range(NCHUNK):
        load_x_chunk(P, hT, c, xst, xb)
        for i, n in enumerate(names):
            bank = i % 2
            proj_tile(P, wb, i * 128, 128, xb, pb[bank], pk[bank])
            P.copy('act' if i % 2 == 0 else 'dve', pt[n][:], pb[bank][:, :CH], R=[pk[bank]], W=['pt_' + n])
        proj_tile(P, wb, 1024, 16, xb, pb[0], pk[0])
        P.copy('dve', glT[:], pb[0][:16, :CH], R=[pk[0]], W=['glT'])

        def s5_chain():
            for ut in range(2):
                un = 'u%d' % ut
                P.copy('act', ub[ut][:], pt[un][:], R=['pt_' + un], W=[('ub', ut)])
                for s4 in range(4):
                    st = ut * 4 + s4
                    i2 = st % 2
                    xr = pb[2]; xi = pb[3]
                    P.mm(xr[:, :CH], BTb[:, 0, st, :], ub[ut][:], R=['BTb', ('ub', ut)], W=[pk[2]])
                    P.mm(xi[:, :CH], BTb[:, 1, st, :], ub[ut][:], R=['BTb', ('ub', ut)], W=[pk[3]])
                    a = s5t['a'][i2]; b = s5t['b'][i2]; cc = s5t['c'][i2]; d = s5t['d'][i2]
                    ka, kb_, kc, kd = ('s5a', i2), ('s5b', i2), ('s5c', i2), ('s5d', i2)
                    P.tt('dve', a[:], xr[:, :CH], Er[:, st, :], ALU.mult, R=[pk[2], 'Er'], W=[ka])
                    P.tt('dve', b[:], xi[:, :CH], Ei[:, st, :], ALU.mult, R=[pk[3], 'Ei'], W=[kb_])
                    P.tt('pool', a[:], a[:], b[:], ALU.add, R=[ka, kb_], W=[ka])
                    P.tt('dve', cc[:], xi[:, :CH], Er[:, st, :], ALU.mult, R=[pk[3], 'Er'], W=[kc])
                    P.tt('dve', d[:], xr[:, :CH], Ei[:, st, :], ALU.mult, R=[pk[2], 'Ei'], W=[kd])
                    P.tt('pool', cc[:], cc[:], d[:], ALU.subtract, R=[kc, kd], W=[kc])
                    yield
                    P.op('dve', lambda e, a=a, b=b, st=st: e.tensor_tensor_scan(
                        out=b[:], data0=sc['r'][:, st:st + 1].to_broadcast([128, CH]), data1=a[:], initial=carry[:, st, 0:1], op0=ALU.mult, op1=ALU.add),
                        R=[ka, 'carry'] + SK('r'), W=[kb_])
                    P.op('dve', lambda e, cc=cc, d=d, st=st: e.tensor_tensor_scan(
                        out=d[:], data0=sc['r'][:, st:st + 1].to_broadcast([128, CH]), data1=cc[:], initial=carry[:, st, 1:2], op0=ALU.mult, op1=ALU.add),
                        R=[kc, 'carry'] + SK('r'), W=[kd])
                    sr = sre[i2]; si = sim[i2]
                    P.tt('pool', a[:], b[:], Er[:, st, :], ALU.mult, R=[kb_, 'Er'], W=[ka])
                    P.tt('pool', cc[:], d[:], Ei[:, st, :], ALU.mult, R=[kd, 'Ei'], W=[kc])
                    P.tt('dve', sr[:], a[:], cc[:], ALU.subtract, R=[ka, kc], W=[('sre', i2)])
                    P.tt('pool', a[:], b[:], Ei[:, st, :], ALU.mult, R=[kb_, 'Ei'], W=[ka])
                    P.tt('pool', cc[:], d[:], Er[:, st, :], ALU.mult, R=[kd, 'Er'], W=[kc])
                    P.tt('dve', si[:], a[:], cc[:], ALU.add, R=[ka, kc], W=[('sim', i2)])
                    P.copy('dve', carry[:, st, 0:1], sr[:, CH - 1:CH], R=[('sre', i2), 'carry'], W=['carry'])
                    P.copy('dve', carry[:, st, 1:2], si[:, CH - 1:CH], R=[('sim', i2), 'carry'], W=['carry'])
                    P.copy('act', sreb[i2][:], sr[:], R=[('sre', i2)], W=[('sreb', i2)])
                    P.copy('act', simb[i2][:], si[:], R=[('sim', i2)], W=[('simb', i2)])
                    P.mm(pb[4 + ut][:, :CH], CTb[:, 0, st, :], sreb[i2][:], start=(s4 == 0), stop=False,
                         R=['CTb', ('sreb', i2)], W=[pk[4 + ut]])
                    P.mm(pb[4 + ut][:, :CH], CTb[:, 1, st, :], simb[i2][:], start=False, stop=(s4 == 3),
                         R=['CTb', ('simb', i2)], W=[pk[4 + ut]])
                    yield
                P.stt('dve', zt[ut][:], pt[un][:], dcol[:, ut:ut + 1], pb[4 + ut][:, :CH], ALU.mult, ALU.add,
                      R=['pt_' + un, 'dcol', pk[4 + ut]], W=[('zt', ut)])
                gelu_tanh(P, zt[ut][:], zt[ut][:], gA, gB, CH, ('zt', ut), ('zt', ut), ['gA', 'gB'])
                P.copy('act', ztb[ut][:], zt[ut][:], R=[('zt', ut)], W=[('ztb', ut)])
                P.dma(y_loc[c][ut * 128:(ut + 1) * 128, :], ztb[ut][:], R=[('ztb', ut)], W=[('y_loc', c, ut)], q='pool')
                ykeys.append(('y_loc', c, ut))
                yield


        def gla_chain():
            P.mm(pb[7][:, :CH], wg[:], glT[:], R=['wg', 'glT'], W=[pk[7]])
            P.act(gk[:], pb[7][:, :CH], AF.Exp, scale=-1.0, bias=nbg[:], R=[pk[7], 'nbg'], W=['gk'])
            P.ts('dve', gk[:], gk[:], 1.0, ALU.add, R=['gk'], W=['gk'])
            P.act(gk[:], gk[:], AF.Ln, R=['gk'], W=['gk'])
            P.ts('dve', gk[:], gk[:], -1.0 / 16, ALU.mult, R=['gk'], W=['gk'])
            for i in range(2):
                P.act(rsil[i][:], pt['r%d' % i][:], AF.Silu, R=['pt_r%d' % i], W=[('rsil', i)])
            for sb_ in range(4):
                if (c * CH + (sb_ + 1) * 128) <= PAD:
                    continue
                ss = slice(sb_ * 128, (sb_ + 1) * 128)
                P.op('dve', lambda e, ss=ss: e.tensor_tensor_scan(out=bcum[:, ss], data0=onescol[:, 0:1].to_broadcast([128, 128]), data1=gk[:, ss],
                                                                 initial=0.0, op0=ALU.mult, op1=ALU.add),
                     R=['gk', 'onescol'], W=['bcum'])
                P.act(gt['eb'][:], bcum[:, ss], AF.Exp, R=['bcum'], W=GK('eb'))
                P.act(gt['enb'][:], bcum[:, ss], AF.Exp, scale=-1.0, R=['bcum'], W=GK('enb'))
                P.act(gt['ekst'][:], bcum[:, ss], AF.Exp, scale=-1.0, bias=bcum[:, sb_ * 128 + 127:sb_ * 128 + 128],
                      R=['bcum'], W=GK('ekst'))
                P.act(gsm['dec'][:], bcum[:, sb_ * 128 + 127:sb_ * 128 + 128], AF.Exp, R=['bcum'], W=['gsm_dec'])
                P.stt('dve', gtb['qin'][:], pt['q'][:, ss], 128.0 ** -0.5, gt['eb'][:], ALU.mult, ALU.mult,
                      R=['pt_q'] + GK('eb'), W=['gb_qin'])
                P.tt('dve', gtb['kin'][:], pt['k'][:, ss], gt['enb'][:], ALU.mult, R=['pt_k'] + GK('enb'), W=['gb_kin'])
                P.tt('dve', gt['kstT'][:], pt['k'][:, ss], gt['ekst'][:], ALU.mult, R=['pt_k'] + GK('ekst'), W=GK('kstT'))
                yield
                P.transpose(pb[6][:, 0:128], gt['kstT'][:], ident[:], R=GK('kstT') + ['ident'], W=[pk[6]])
                for i in range(2):
                    P.transpose(pb[6][:, 128 + i * 128:256 + i * 128], pt['v%d' % i][:, ss], ident[:],
                                R=['pt_v%d' % i, 'ident'], W=[pk[6]])
                P.copy('act', gtb['kst'][:], pb[6][:, 0:128], R=[pk[6]], W=['gb_kst'])
                P.copy('act', vtok[:], pb[6][:, 128:384], R=[pk[6]], W=['vtok'])
                yield
                for i in range(2):
                    P.transpose(pb[7][:, i * 128:(i + 1) * 128], rsil[i][:, ss], ident[:], R=[('rsil', i), 'ident'], W=[pk[7]])
                P.copy('act', rtok[:], pb[7][:, :256], R=[pk[7]], W=['rtok'])
                P.mm(pb[6][:, 384:512], gtb['kin'][:], gtb['qin'][:], R=['gb_kin', 'gb_qin'], W=[pk[6]])
                P.tt('dve', gtb['att'][:], pb[6][:, 384:512], U[:], ALU.mult, R=[pk[6], 'U'], W=['gb_att'])
                yield
                P.mm(pb[7][:, 256:512], gtb['att'][:], vtok[:], start=True, stop=False, R=['gb_att', 'vtok'], W=[pk[7]])
                P.mm(pb[7][:, 256:512], gtb['qin'][:], Sb[:], start=False, stop=True, R=['gb_qin', 'Sb'], W=[pk[7]])
                P.mm(pb[6][:, 0:256], gtb['kst'][:], vtok[:], R=['gb_kst', 'vtok'], W=[pk[6]])
                P.stt('dve', S[:], S[:], gsm['dec'][:], pb[6][:, 0:256], ALU.mult, ALU.add, R=['S', 'gsm_dec', pk[6]], W=['S'])
                P.copy('act', Sb[:], S[:], R=['S'], W=['Sb'])
                yield
                P.act(junk[:], pb[7][:, 256:512], AF.Square, accum_out=gsm['ss'][:], R=[pk[7]], W=['junk', 'gsm_ss'])
                P.ts('dve', gsm['rstd'][:], gsm['ss'][:], 1.0 / 256, ALU.mult, 1e-6, ALU.add, R=['gsm_ss'], W=['gsm_rstd'])
                P.act(gsm['rstd'][:], gsm['rstd'][:], AF.Sqrt, R=['gsm_rstd'], W=['gsm_rstd'])
                P.op('dve', lambda e: e.reciprocal(out=gsm['rstd'][:], in_=gsm['rstd'][:]), R=['gsm_rstd'], W=['gsm_rstd'])
                P.tt('pool', rtok[:], rtok[:], ngrep[:], ALU.mult, R=['rtok', 'ngrep'], W=['rtok'])
                P.stt('dve', osb[:], pb[7][:, 256:512], gsm['rstd'][:], rtok[:], ALU.mult, ALU.mult,
                      R=[pk[7], 'gsm_rstd', 'rtok'], W=['osb'])
                yield
                for i in range(2):
                    P.transpose(pb[7][:, i * 128:(i + 1) * 128], osb[:, i * 128:(i + 1) * 128], ident[:], R=['osb', 'ident'], W=[pk[7]])
                for i in range(2):
                    P.copy('act', obc[:, i, ss], pb[7][:, i * 128:(i + 1) * 128], R=[pk[7]], W=['obc'])
                yield
        gens = [s5_chain(), gla_chain()]
        while gens:
            for g in list(gens):
                try:
                    next(g)
                except StopIteration:
                    gens.remove(g)
        for i in range(2):
            P.dma(y_loc[c][256 + i * 128:256 + (i + 1) * 128, :], obc[:, i, :], R=['obc'], W=[('y_loc', c, 2 + i)], q='pool')
            ykeys.append(('y_loc', c, 2 + i))
        P.allgather(y_all[c], y_loc[c], groups, R=[('y_loc', c, k) for k in range(4)], W=[('y_all', c)])
    return ykeys

import math, os


def phase_cd(P, pb, pk, NCHUNK, h_all, y_loc, y_all, groups, pre="c_", stage=99, PN=342):
    Tp = NCHUNK * CH
    NCOL = 12 * 128 + 4
    T2 = 2052
    w_in = P.dram(pre + "w_in", [D, NCOL])
    convw = P.dram(pre + "convw", [128, 8, 4]); convb = P.dram(pre + "convb", [128, 8])
    gpar = P.dram(pre + "gpar", [4, 4])
    gdn_ng = P.dram(pre + "gdn_ng", [128, 128])
    lwa = P.dram(pre + "lwa", [128, 2, 128]); lwx = P.dram(pre + "lwx", [128, 2, 128]); lpar = P.dram(pre + "lpar", [128, 2, 3])
    c_U = P.dram(pre + "c_U", [128, 128]); c_Us = P.dram(pre + "c_Us", [128, 128]); c_ident = P.dram(pre + "c_ident", [128, 128])
    c_sel4 = P.dram(pre + "c_sel4", [4, 512])
    ykeys = []
    R_ = lambda b, i: pk[6] if (b, i) == (3, 3) else pk[b]
    wb = inproj_setup(P, w_in, NCOL)
    xb = P.sb("xb", [128, 16, CH], BF16)
    gobc = P.sb("gobc", [128, 2, CH], BF16); ylb = P.sb("ylb", [128, 2, CH], BF16)
    praw = [P.sb("praw%d" % i, [128, CH + 3]) for i in range(8)]
    pc = [P.sb("pc%d" % i, [128, CH]) for i in range(8)]
    pz = [P.sb("pz%d" % i, [128, CH]) for i in range(4)]
    baT = P.sb("baT", [4, CH])
    U = P.sb("U", [128, 128]); Us = P.sb("Us", [128, 128]); ident = P.sb("ident", [128, 128])
    ones_f = P.sb("ones_f", [128, 128]); onescol = P.sb("onescol", [128, 1]); sel4 = P.sb("sel4", [4, 512])
    P.dma(U[:], c_U, W=['U']); P.dma(Us[:], c_Us, W=['Us']); P.dma(ident[:], c_ident, W=['ident'])
    P.dma(sel4[:], c_sel4, W=['sel4'])
    P.memset('dve', ones_f[:], 1.0, W=['ones_f']); P.memset('dve', onescol[:], 1.0, W=['onescol'])
    cw = P.sb("cw", [128, 8, 4]); cb = P.sb("cb", [128, 8])
    P.dma(cw[:], convw, W=['cw']); P.dma(cb[:], convb, W=['cb'])
    for i in range(8):
        P.memset('pool', praw[i][:, 0:3], 0.0, W=[('praw', i)])
    gp = P.sb("gp", [4, 4]); P.dma(gp[:], gpar, W=['gp'])
    negA = P.sb("negA", [4, 1])
    P.act(negA[:], gp[:, 1:2], AF.Exp, R=['gp'], W=['negA'])
    P.ts('dve', negA[:], negA[:], -1.0, ALU.mult, R=['negA'], W=['negA'])
    ngrep = P.sb("ngrep", [128, 128]); P.dma(ngrep[:], gdn_ng, W=['ngrep'])
    S = [P.sb("S%d" % h, [128, 128]) for h in range(2)]; Sb = [P.sb("Sb%d" % h, [128, 128], BF16) for h in range(2)]
    for h in range(2):
        P.memset('dve', S[h][:], 0.0, W=[('S', h)]); P.memset('dve', Sb[h][:], 0.0, W=[('Sb', h)])
    sig4 = P.sb("sig4", [4, CH]); g4 = P.sb("g4", [4, CH]); bg4 = P.sb("bg4", [4, CH])
    Brep = [P.sb("Brep%d" % h, [128, CH]) for h in range(2)]; Grep = [P.sb("Grep%d" % h, [128, CH]) for h in range(2)]
    qnb = [P.sb("qnb%d" % h, [128, CH], BF16) for h in range(2)]; knb = [P.sb("knb%d" % h, [128, CH], BF16) for h in range(2)]
    knf = [P.sb("knf%d" % h, [128, CH]) for h in range(2)]
    t512 = [P.sb("t512_%d" % i, [128, CH]) for i in range(3)]
    f2 = [{k: P.sb("f%d_%s" % (h, k), [128, 128]) for k in
           ['R1', 'DT', 'Gam', 'GamU', 'P0', 'BU', 'Pa', 'Pb', 'Qa', 'Qb', 'R', 'egr', 'zs', 'osb', 'junk', 'ktok', 'vtok']}
          for h in range(2)]
    bq2 = [{k: P.sb("b%d_%s" % (h, k), [128, 128], BF16) for k in ['Rb', 'Aqk', 'kbg', 'kst', 'vb', 'nwc', 'vnew', 'qdec']}
           for h in range(2)]
    sm2 = [{k: P.sb("sm%d_%s" % (h, k), [128, 1]) for k in ['gcc', 'glast', 'egc', 'bege', 'ekl', 'dec', 'ss', 'rstd']}
           for h in range(2)]
    tok4 = P.sb("tok4", [128, 4])
    FK = lambda *n: ['f_' + a for a in n]
    BK = lambda *n: ['b_' + a for a in n]
    MK = lambda *n: ['sm_' + a for a in n]
    wa_f = P.sb("wa_f", [128, 2, 128]); wx_f = P.sb("wx_f", [128, 2, 128]); lp = P.sb("lp", [128, 2, 3])
    wa_b = P.sb("wa_b", [128, 2, 128], BF16); wx_b = P.sb("wx_b", [128, 2, 128], BF16)
    P.dma(wa_f[:], lwa, W=['wa_f']); P.dma(wx_f[:], lwx, W=['wx_f']); P.dma(lp[:], lpar, W=['lp'])
    P.copy('dve', wa_b[:], wa_f[:], R=['wa_f'], W=['wa_b']); P.copy('dve', wx_b[:], wx_f[:], R=['wx_f'], W=['wx_b'])
    ccol = P.sb("ccol", [128, 2])
    P.act(ccol[:], lp[:, :, 2], AF.Exp, scale=-1.0, R=['lp'], W=['ccol'])
    P.ts('dve', ccol[:], ccol[:], 1.0, ALU.add, R=['ccol'], W=['ccol'])
    P.act(ccol[:], ccol[:], AF.Ln, R=['ccol'], W=['ccol'])
    P.ts('dve', ccol[:], ccol[:], -8.0, ALU.mult, R=['ccol'], W=['ccol'])
    hprev = P.sb("hprev", [128, 2]); P.memset('dve', hprev[:], 0.0, W=['hprev'])
    xcb = P.sb("xcb", [128, CH], BF16)
    L = {k: P.sb("l_" + k, [128, CH]) for k in ['r', 'i', 'a', 'bx', 'h', 'gA', 'gB', 'y']}
    LK = lambda *n: ['l_' + a for a in n]

    for c in range(NCHUNK):
        xk = [('xb', dt) for dt in range(16)]
        p0 = c * CH
        if p0 < PAD:
            P.memset('pool', xb[:, :, 0:PAD - p0], 0.0, W=xk)
        pos = max(p0, PAD)
        while pos < p0 + CH:
            t = pos - PAD
            r = t // T2
            col = t - r * T2
            pc_ = col // PN
            off = col - pc_ * PN
            n = min(p0 + CH - pos, PN - off)
            for half in range(2):
                P.dma(xb[:, 8 * half:8 * half + 8, pos - p0:pos - p0 + n],
                      h_all[pc_][half][r * 1024:(r + 1) * 1024, off:off + n].rearrange("(t p) n -> p t n", p=128),
                      W=xk, q='sp')
            pos += n
        for i in range(12):
            bank = i % 2
            proj_tile(P, wb, i * 128, 128, xb, pb[bank], pk[bank])
            if i < 8:
                P.copy('act', praw[i][:, 3:CH + 3], pb[bank][:, :CH], R=[pk[bank]], W=[('praw', i)])
                P.ts('dve', pc[i][:], praw[i][:, 0:CH], cw[:, i, 0:1], ALU.mult, cb[:, i:i + 1], ALU.add,
                     R=[('praw', i), 'cw', 'cb'], W=[('pc', i)])
                for jj in range(1, 4):
                    P.stt('dve', pc[i][:], praw[i][:, jj:CH + jj], cw[:, i, jj:jj + 1], pc[i][:], ALU.mult, ALU.add,
                          R=[('praw', i), 'cw', ('pc', i)], W=[('pc', i)])
                P.copy('pool', praw[i][:, 0:3], praw[i][:, CH:CH + 3], R=[('praw', i)], W=[('praw', i)])
                if i < 6:
                    P.act(pc[i][:], pc[i][:], AF.Silu, R=[('pc', i)], W=[('pc', i)])
            else:
                P.copy('act', pz[i - 8][:], pb[bank][:, :CH], R=[pk[bank]], W=[('pz', i - 8)])
        proj_tile(P, wb, 12 * 128, 4, xb, pb[0], pk[0])
        P.copy('act', baT[:], pb[0][:4, :CH], R=[pk[0]], W=['baT'])

        for b in range(2):
            xc = pc[6 + b]
            P.copy('act', xcb[:], xc[:], R=[('pc', 6 + b)], W=['xcb'])
            P.mm(pb[6][:, :CH], wa_b[:, b, :], xcb[:], R=['wa_b', 'xcb'], W=[pk[6]])
            P.mm(pb[7][:, :CH], wx_b[:, b, :], xcb[:], R=['wx_b', 'xcb'], W=[pk[7]])
            P.act(L['r'][:], pb[6][:, :CH], AF.Sigmoid, bias=lp[:, b, 0:1], R=[pk[6], 'lp'], W=LK('r'))
            P.act(L['i'][:], pb[7][:, :CH], AF.Sigmoid, bias=lp[:, b, 1:2], R=[pk[7], 'lp'], W=LK('i'))
            P.act(L['a'][:], L['r'][:], AF.Exp, scale=ccol[:, b:b + 1], R=LK('r') + ['ccol'], W=LK('a'))
            P.tt('pool', L['bx'][:], L['a'][:], L['a'][:], ALU.mult, R=LK('a'), W=LK('bx'))
            P.ts('pool', L['bx'][:], L['bx'][:], -1.0, ALU.mult, 1.0, ALU.add, R=LK('bx'), W=LK('bx'))
            P.act(L['bx'][:], L['bx'][:], AF.Sqrt, R=LK('bx'), W=LK('bx'))
            P.tt('pool', L['bx'][:], L['bx'][:], L['i'][:], ALU.mult, R=LK('bx', 'i'), W=LK('bx'))
            P.tt('pool', L['bx'][:], L['bx'][:], xc[:], ALU.mult, R=LK('bx') + [('pc', 6 + b)], W=LK('bx'))
            c0 = PAD if c == 0 else 0
            if c == 0:
                P.memset('dve', L['h'][:, :PAD], 0.0, W=LK('h'))
            P.op('dve', lambda e, b=b, c0=c0: e.tensor_tensor_scan(
                out=L['h'][:, c0:], data0=L['a'][:, c0:], data1=L['bx'][:, c0:], initial=hprev[:, b:b + 1],
                op0=ALU.mult, op1=ALU.add), R=LK('a', 'bx') + ['hprev'], W=LK('h'))
            P.copy('dve', hprev[:, b:b + 1], L['h'][:, CH - 1:CH], R=LK('h') + ['hprev'], W=['hprev'])
            gelu_tanh(P, L['y'][:], pz[2 + b][:], L['gA'], L['gB'], CH, ('pz', 2 + b), 'l_y', LK('gA', 'gB'))
            P.tt('pool', L['y'][:], L['y'][:], L['h'][:], ALU.mult, R=LK('y', 'h'), W=LK('y'))
            P.copy('act', ylb[:, b, :], L['y'][:], R=LK('y'), W=['ylb'])
            P.dma(y_loc[c][256 + b * 128:256 + (b + 1) * 128, :], ylb[:, b, :], R=['ylb'], W=[('y_loc', c, 2 + b)], q='pool')
            ykeys.append(('y_loc', c, 2 + b))

        if stage < 1:
            continue
        P.act(sig4[:], baT[:], AF.Sigmoid, R=['baT'], W=['sig4'])
        P.act(g4[:], baT[:], AF.Exp, bias=gp[:, 0:1], R=['baT', 'gp'], W=['g4'])
        P.ts('dve', g4[:], g4[:], 1.0, ALU.add, R=['g4'], W=['g4'])
        P.act(g4[:], g4[:], AF.Ln, R=['g4'], W=['g4'])
        P.ts('dve', g4[:], g4[:], negA[:, 0:1], ALU.mult, R=['g4', 'negA'], W=['g4'])
        P.ts('dve', bg4[:], sig4[:], gp[:, 2:3], ALU.mult, R=['sig4', 'gp'], W=['bg4'])
        P.stt('dve', bg4[:], g4[:], gp[:, 3:4], bg4[:], ALU.mult, ALU.add, R=['g4', 'gp', 'bg4'], W=['bg4'])
        for h in range(2):
            P.mm(pb[6][:, :CH], sel4[:, h * 128:(h + 1) * 128], sig4[:], R=['sel4', 'sig4'], W=[pk[6]])
            P.copy('act', Brep[h][:], pb[6][:, :CH], R=[pk[6]], W=[('Brep', h)])
            P.mm(pb[7][:, :CH], sel4[:, (2 + h) * 128:(3 + h) * 128], g4[:], R=['sel4', 'g4'], W=[pk[7]])
            P.copy('act', Grep[h][:], pb[7][:, :CH], R=[pk[7]], W=[('Grep', h)])
            for which, src, scale_ in (('q', pc[h], 128.0 ** -0.5), ('k', pc[2 + h], 1.0)):
                P.act(t512[0][:], src[:], AF.Square, R=[('pc', h if which == 'q' else 2 + h)], W=[('t512', 0)])
                P.mm(pb[6][:, :CH], ones_f[:], t512[0][:], R=['ones_f', ('t512', 0)], W=[pk[6]])
                P.ts('dve', t512[1][:], pb[6][:, :CH], 1e-6, ALU.add, R=[pk[6]], W=[('t512', 1)])
                P.act(t512[1][:], t512[1][:], AF.Sqrt, R=[('t512', 1)], W=[('t512', 1)])
                P.op('dve', lambda e: e.reciprocal(out=t512[1][:], in_=t512[1][:]), R=[('t512', 1)], W=[('t512', 1)])
                if which == 'q':
                    P.stt('dve', qnb[h][:], src[:], scale_, t512[1][:], ALU.mult, ALU.mult, R=[('pc', h), ('t512', 1)], W=[('qnb', h)])
                else:
                    P.tt('dve', knf[h][:], src[:], t512[1][:], ALU.mult, R=[('pc', 2 + h), ('t512', 1)], W=[('knf', h)])
                    P.copy('act', knb[h][:], knf[h][:], R=[('knf', h)], W=[('knb', h)])

        if stage < 2:
            continue
        def head_chain(h, sb_, ss):
            f = f2[h]; bq = bq2[h]; sm = sm2[h]
            FK = lambda *n: ['f%d_%s' % (h, a) for a in n]
            BK = lambda *n: ['b%d_%s' % (h, a) for a in n]
            MK = lambda *n: ['sm%d_%s' % (h, a) for a in n]
            bA, bB, bC = (2, 3, 4) if h == 0 else (5, 6, 7)
            pA, pB, pC = pb[bA], pb[bB], pb[bC]
            kA, kB, kC = pk[bA], pk[bB], pk[bC]
            bcol = tok4[:, h:h + 1]; gcol = tok4[:, 2 + h:3 + h]
            P.op('dve', lambda e: e.tensor_tensor_scan(
                out=f['R1'][:], data0=onescol[:, 0:1].to_broadcast([128, 128]), data1=Grep[h][:, ss], initial=0.0,
                op0=ALU.mult, op1=ALU.add), R=[('Grep', h), 'onescol'], W=FK('R1'))
            P.mm(pA[:, 0:1], U[:], gcol, R=['U', 'tok4'], W=[kA])
            P.copy('act', sm['gcc'][:], pA[:, 0:1], R=[kA], W=MK('gcc'))
            P.copy('pool', sm['glast'][:], f['R1'][:, 127:128], R=FK('R1'), W=MK('glast'))
            yield
            P.ts('dve', f['DT'][:], f['R1'][:], sm['gcc'][:], ALU.subtract, 0.0, ALU.min, R=FK('R1') + MK('gcc'), W=FK('DT'))
            P.act(f['Gam'][:], f['DT'][:], AF.Exp, R=FK('DT'), W=FK('Gam'))
            P.tt('pool', f['GamU'][:], f['Gam'][:], U[:], ALU.mult, R=FK('Gam') + ['U'], W=FK('GamU'))
            P.tt('pool', f['BU'][:], Brep[h][:, ss], Us[:], ALU.mult, R=[('Brep', h), 'Us'], W=FK('BU'))
            P.act(f['egr'][:], f['R1'][:], AF.Exp, R=FK('R1'), W=FK('egr'))
            P.act(sm['egc'][:], sm['gcc'][:], AF.Exp, R=MK('gcc'), W=MK('egc'))
            P.tt('pool', sm['bege'][:], sm['egc'][:], bcol, ALU.mult, R=MK('egc') + ['tok4'], W=MK('bege'))
            P.act(sm['ekl'][:], sm['gcc'][:], AF.Exp, scale=-1.0, bias=sm['glast'][:], R=MK('gcc', 'glast'), W=MK('ekl'))
            P.act(sm['dec'][:], sm['glast'][:], AF.Exp, R=MK('glast'), W=MK('dec'))
            yield
            P.mm(pA[:, 0:128], knb[h][:, ss], knb[h][:, ss], R=[('knb', h)], W=[kA])
            P.mm(pA[:, 128:256], knb[h][:, ss], qnb[h][:, ss], R=[('knb', h), ('qnb', h)], W=[kA])
            P.transpose(pA[:, 256:384], knf[h][:, ss], ident[:], R=[('knf', h), 'ident'], W=[kA])
            P.transpose(pA[:, 384:512], pc[4 + h][:, ss], ident[:], R=[('pc', 4 + h), 'ident'], W=[kA])
            yield
            P.tt('dve', f['P0'][:], pA[:, 0:128], f['Gam'][:], ALU.mult, R=[kA] + FK('Gam'), W=FK('P0'))
            P.tt('dve', f['Pa'][:], f['P0'][:], f['BU'][:], ALU.mult, R=FK('P0', 'BU'), W=FK('Pa'))
            P.tt('dve', bq['Aqk'][:], pA[:, 128:256], f['GamU'][:], ALU.mult, R=[kA] + FK('GamU'), W=BK('Aqk'))
            P.copy('act', f['ktok'][:], pA[:, 256:384], R=[kA], W=FK('ktok'))
            P.copy('act', f['vtok'][:], pA[:, 384:512], R=[kA], W=FK('vtok'))
            P.ts('pool', bq['kbg'][:], f['ktok'][:], sm['bege'][:], ALU.mult, R=FK('ktok') + MK('bege'), W=BK('kbg'))
            P.ts('pool', bq['kst'][:], f['ktok'][:], sm['ekl'][:], ALU.mult, R=FK('ktok') + MK('ekl'), W=BK('kst'))
            P.ts('pool', bq['vb'][:], f['vtok'][:], bcol, ALU.mult, R=FK('vtok') + ['tok4'], W=BK('vb'))
            yield
            P.transpose(pB[:, 0:128], f['Pa'][:], ident[:], R=FK('Pa') + ['ident'], W=[kB])
            P.copy('act', f['Qa'][:], pB[:, 0:128], R=[kB], W=FK('Qa'))
            P.tt('pool', f['R'][:], ident[:], f['Pa'][:], ALU.subtract, R=['ident'] + FK('Pa'), W=FK('R'))
            yield
            Pc, Qc, Pn, Qn = 'Pa', 'Qa', 'Pb', 'Qb'
            for lvl in range(6):
                P.mm(pB[:, 128:256], f[Qc][:], f[Pc][:], R=FK(Qc, Pc), W=[kB])
                P.mm(pB[:, 256:384], f[Pc][:], f[Qc][:], R=FK(Qc, Pc), W=[kB])
                P.copy('act', f[Pn][:], pB[:, 128:256], R=[kB], W=FK(Pn))
                P.copy('act', f[Qn][:], pB[:, 256:384], R=[kB], W=FK(Qn))
                yield
                P.mm(pB[:, 384:512], f[Qn][:], f['R'][:], R=FK(Qn, 'R'), W=[kB])
                P.tt('dve', f['R'][:], f['R'][:], pB[:, 384:512], ALU.add, R=FK('R') + [kB], W=FK('R'))
                Pc, Qc, Pn, Qn = Pn, Qn, Pc, Qc
                yield
            P.copy('act', bq['Rb'][:], f['R'][:], R=FK('R'), W=BK('Rb'))
            P.mm(pC[:, 128:256], bq['kbg'][:], bq['Rb'][:], R=BK('kbg', 'Rb'), W=[kC])
            P.op('act', lambda e: e.activation(out=bq['nwc'][:], in_=pC[:, 128:256], func=AF.Copy, scale=-1.0),
                 R=[kC], W=BK('nwc'))
            yield
            P.mm(pC[:, 0:128], bq['Rb'][:], bq['vb'][:], start=True, stop=False, R=BK('Rb', 'vb'), W=[kC])
            P.mm(pC[:, 0:128], bq['nwc'][:], Sb[h][:], start=False, stop=True, R=BK('nwc') + [('Sb', h)], W=[kC])
            P.copy('act', bq['vnew'][:], pC[:, 0:128], R=[kC], W=BK('vnew'))
            P.tt('pool', bq['qdec'][:], qnb[h][:, ss], f['egr'][:], ALU.mult, R=[('qnb', h)] + FK('egr'), W=BK('qdec'))
            yield
            P.mm(pC[:, 256:384], bq['qdec'][:], Sb[h][:], start=True, stop=False, R=BK('qdec') + [('Sb', h)], W=[kC])
            P.mm(pC[:, 256:384], bq['Aqk'][:], bq['vnew'][:], start=False, stop=True, R=BK('Aqk', 'vnew'), W=[kC])
            P.mm(pC[:, 384:512], bq['kst'][:], bq['vnew'][:], R=BK('kst', 'vnew'), W=[kC])
            P.stt('dve', S[h][:], S[h][:], sm['dec'][:], pC[:, 384:512], ALU.mult, ALU.add,
                  R=[('S', h), kC] + MK('dec'), W=[('S', h)])
            P.copy('act', Sb[h][:], S[h][:], R=[('S', h)], W=[('Sb', h)])
            yield
            P.act(f['junk'][:], pC[:, 256:384], AF.Square, accum_out=sm['ss'][:], R=[kC], W=FK('junk') + MK('ss'))
            P.ts('dve', sm['rstd'][:], sm['ss'][:], 1.0 / 128, ALU.mult, 1e-6, ALU.add, R=MK('ss'), W=MK('rstd'))
            P.act(sm['rstd'][:], sm['rstd'][:], AF.Sqrt, R=MK('rstd'), W=MK('rstd'))
            P.op('dve', lambda e: e.reciprocal(out=sm['rstd'][:], in_=sm['rstd'][:]), R=MK('rstd'), W=MK('rstd'))
            P.transpose(pA[:, 0:128], pz[h][:, ss], ident[:], R=[('pz', h), 'ident'], W=[kA])
            P.act(f['zs'][:], pA[:, 0:128], AF.Silu, R=[kA], W=FK('zs'))
            P.tt('pool', f['zs'][:], f['zs'][:], ngrep[:], ALU.mult, R=FK('zs') + ['ngrep'], W=FK('zs'))
            yield
            P.stt('dve', f['osb'][:], pC[:, 256:384], sm['rstd'][:], f['zs'][:], ALU.mult, ALU.mult,
                  R=[kC] + MK('rstd') + FK('zs'), W=FK('osb'))
            P.transpose(pA[:, 128:256], f['osb'][:], ident[:], R=FK('osb') + ['ident'], W=[kA])
            P.copy('act', gobc[:, h, ss], pA[:, 128:256], R=[kA], W=['gobc'])

        for sb_ in range(4):
            if (c * CH + (sb_ + 1) * 128) <= PAD:
                continue
            ss = slice(sb_ * 128, (sb_ + 1) * 128)
            P.transpose(pb[1][:, 0:4], bg4[:, ss], ident[:4, :4], R=['bg4', 'ident'], W=[pk[1]])
            P.copy('act', tok4[:], pb[1][:, 0:4], R=[pk[1]], W=['tok4'])
            gens = [head_chain(0, sb_, ss), head_chain(1, sb_, ss)]
            while gens:
                for g in list(gens):
                    try:
                        next(g)
                    except StopIteration:
                        gens.remove(g)
        for h in range(2):
            P.dma(y_loc[c][h * 128:(h + 1) * 128, :], gobc[:, h, :], R=['gobc'], W=[('y_loc', c, h)], q='pool')
            ykeys.append(('y_loc', c, h))
        P.allgather(y_all[c], y_loc[c], groups, R=[('y_loc', c, k) for k in range(4)], W=[('y_all', c)])
    return ykeys


D = 2048
NE = 32
FF = 256
DN_ALPHA = (2.0 * 2) ** 0.25
LN_EPS = 1e-5
PAD_ = 496


def layer_norm_fm(P, v, N, gcol, bcol, pbA, pbB, kA, kB, ones_f, tmp, hb=None, vkey='v', hbkey='hb'):
    sq = tmp['sq']; mean = tmp['mean']; rstd = tmp['rstd']; m2 = tmp['m2']
    for dt in range(16):
        P.mm(pbA[:, :N], ones_f[:], v[:, dt, :N], start=(dt == 0), stop=(dt == 15), R=[(vkey, dt)], W=[kA])
    for dt in range(16):
        s = sq[dt % 2]
        P.act(s[:, :N], v[:, dt, :N], AF.Square, R=[(vkey, dt)], W=[('sq', dt % 2)])
        P.mm(pbB[:, :N], ones_f[:], s[:, :N], start=(dt == 0), stop=(dt == 15), R=[('sq', dt % 2)], W=[kB])
    P.act(mean[:, :N], pbA[:, :N], AF.Copy, scale=1.0 / D, R=[kA], W=['mean'])
    P.tt('dve', m2[:, :N], mean[:, :N], mean[:, :N], ALU.mult, R=['mean'], W=['m2'])
    P.ts('dve', rstd[:, :N], pbB[:, :N], 1.0 / D, ALU.mult, LN_EPS, ALU.add, R=[kB], W=['rstd'])
    P.tt('dve', rstd[:, :N], rstd[:, :N], m2[:, :N], ALU.subtract, R=['rstd', 'm2'], W=['rstd'])
    P.act(rstd[:, :N], rstd[:, :N], AF.Sqrt, R=['rstd'], W=['rstd'])
    P.op('dve', lambda e: e.reciprocal(out=rstd[:, :N], in_=rstd[:, :N]), R=['rstd'], W=['rstd'])
    for dt in range(16):
        eng = 'dve' if dt % 2 == 0 else 'pool'
        P.tt(eng, v[:, dt, :N], v[:, dt, :N], mean[:, :N], ALU.subtract, R=[(vkey, dt), 'mean'], W=[(vkey, dt)])
        P.tt(eng, v[:, dt, :N], v[:, dt, :N], rstd[:, :N], ALU.mult, R=[(vkey, dt), 'rstd'], W=[(vkey, dt)])
        P.ts(eng, v[:, dt, :N], v[:, dt, :N], gcol[:, dt:dt + 1], ALU.mult, bcol[:, dt:dt + 1], ALU.add,
             R=[(vkey, dt)], W=[(vkey, dt)])
        if hb is not None:
            P.copy('act', hb[:, dt, :N], v[:, dt, :N], R=[(vkey, dt)], W=[(hbkey, dt)])


def phase_post(P, pb, pk, glu, pre, y_all, hres, out32, outb, outb_all, groups, esel, NCH=6, N=342):
    T2 = NCH * N
    hT = hres
    outT = out32
    w_out = P.dram(pre + "w_out", [D, D])
    if glu:
        w_glu = P.dram(pre + "w_glu", [1024, 1024]); b_glu = P.dram(pre + "b_glu", [128, 8])
    lnp = P.dram(pre + "lnp", [128, 64])
    w_r = P.dram(pre + "w_r", [D, 36]); b_r = P.dram(pre + "b_r", [1, 36])
    w_gate = P.dram(pre + "w_gate", [NE, D, FF]); w_up = P.dram(pre + "w_up", [NE, D, FF]); w_down = P.dram(pre + "w_down", [NE, FF, D])
    c_ident = P.dram(pre + "c_ident", [128, 128]); c_sel = P.dram(pre + "c_sel", [32, NE * 128])

    ones_f = P.sb("ones_f", [128, 128]); ident = P.sb("ident", [128, 128]); selb = [P.sb("selb%d" % i, [32, 128]) for i in range(2)]
    lnp_sb = P.sb("lnp_sb", [128, 64]); wr_sb = P.sb("wr_sb", [128, 16, 36]); br_sb = P.sb("br_sb", [1, 36])
    if glu:
        bglu_sb = P.sb("bglu_sb", [128, 8])
    mst = P.sb("mst", [128, 8, N])
    yb = P.sb("yb", [128, 16, N], BF16)
    v = P.sb("v", [128, 16, N])
    hb = P.sb("hb", [128, 16, N], BF16)
    hh = P.sb("hh", [128, 2 * NE, N], BF16)
    hres = [P.sb("hres%d" % i, [128, N]) for i in range(2)]
    wst = [P.sb("wst%d" % i, [128, 8, 128]) for i in range(3)]
    wsm = [P.sb("wsm%d" % i, [128, 16, 128]) for i in range(1)]
    wsmb = [P.sb("wsmb%d" % i, [128, 16, 128], BF16) for i in range(2)]
    NWB = 4
    wb = [P.sb("wb%d" % i, [128, 16, 256], BF16) for i in range(NWB)]
    wdst = [P.sb("wdst%d" % i, [128, 1024]) for i in range(2)]
    NWD = 6
    wd = [P.sb("wd%d" % i, [128, 1, 1024], BF16) for i in range(NWD)]
    tmp = {'sq': [P.sb("sq%d" % i, [128, N]) for i in range(2)], 'mean': P.sb("mean", [128, N]),
           'rstd': P.sb("rstd", [128, N]), 'm2': P.sb("m2", [128, N])}
    sg = [P.sb("sg%d" % i, [128, N]) for i in range(2)]
    tq = [P.sb("tq%d" % i, [128, N]) for i in range(2)]
    combT = P.sb("combT", [32, N])
    rs = {k: P.sb("rs_" + k, [128, w]) for k, w in
          [('L', 36), ('gmax', 1), ('ngmax', 1), ('ohg', 4), ('eg', 4), ('se', 1), ('ptop', 1), ('lesel', 8),
           ('m1', 1), ('oh1', 8), ('msk', 8), ('m2', 1), ('oh2', 8), ('d', 1), ('ed', 1), ('w1', 1), ('w2', 1),
           ('ce', 8), ('comb', 32)]}
    idram = lambda n, sh: P.nc.dram_tensor(pre + n, sh, BF16, kind="Internal").ap()
    wc_gu = idram("wc_gu", [NE * 2, 128, 16 * 256]); wc_d = idram("wc_d", [2 * NE * 2, 128, 1024])
    wc_o = idram("wc_o", [16, 128, 16 * 128]); wc_g = idram("wc_g", [8, 128, 8 * 128])
    es_sb = P.sb("esel", [128, 4]); P.dma(es_sb[:], esel, W=['esel'])
    cand = [P.sb("cand%d" % i, [128, N], BF16) for i in range(4)]

    def select_into_mst(c, part):
        for j in range(8):
            row0 = (j // 2) * 512 + part + (j % 2) * 128
            for r in range(4):
                col0 = PAD_ + r * T2 + c * N
                pos = col0
                while pos < col0 + N:
                    yc = pos // 512
                    n = min(col0 + N - pos, (yc + 1) * 512 - pos)
                    P.dma(cand[r][:, pos - col0:pos - col0 + n], y_all[yc][row0:row0 + 128, pos - yc * 512:pos - yc * 512 + n],
                          W=[('cand', r)], q='sp')
                    pos += n
            P.ts('dve', mst[:, j, :], cand[0][:], es_sb[:, 0:1], ALU.mult, R=[('cand', 0), 'esel'], W=['mst'])
            for r in range(1, 4):
                P.stt('dve', mst[:, j, :], cand[r][:], es_sb[:, r:r + 1], mst[:, j, :], ALU.mult, ALU.add,
                      R=[('cand', r), 'esel', 'mst'], W=['mst'])

    P.memset('dve', ones_f[:], 1.0, W=['ones_f'])
    P.dma(ident[:], c_ident, W=['ident'])
    P.dma(lnp_sb[:], lnp, W=['lnp']); P.dma(br_sb[:], b_r, W=['br'])
    P.dma(wr_sb[:], w_r.rearrange("(t p) n -> p t n", p=128), W=['wr'])
    if glu:
        P.dma(bglu_sb[:], b_glu, W=['bglu'])

    cast_rr = [0]

    def cast(out, in_, R, W):
        eng = ('act', 'pool')[cast_rr[0] % 2]
        cast_rr[0] += 1
        P.copy(eng, out, in_, R=R, W=W)

    wsm_i = [0]

    def load_coltile(wdram, ktiles, col0, c, cache):
        i = wsm_i[0] % 2
        wsm_i[0] += 1
        cv = cache.rearrange("p (t n) -> p t n", t=ktiles)
        ck = ('wcache', cache.tensor.name, col0)
        if c == 0:
            P.dma(wsm[0][:, :ktiles, :], wdram[:, col0:col0 + 128].rearrange("(t p) n -> p t n", p=128),
                  W=[('wsm', 0)], q='sp')
            cast(wsmb[i][:, :ktiles, :], wsm[0][:, :ktiles, :], R=[('wsm', 0)], W=[('wsmb', i)])
            P.dma(cv, wsmb[i][:, :ktiles, :], R=[('wsmb', i)], W=[ck], q='pool')
        else:
            P.dma(wsmb[i][:, :ktiles, :], cv, R=[ck], W=[('wsmb', i)], q='sp')
        return wsmb[i], ('wsmb', i)

    for c in range(NCH):
        cs = slice(c * N, (c + 1) * N)
        select_into_mst(c, 0)
        if glu:
            for j in range(8):
                P.copy('act', yb[:, 8 + j, :], mst[:, j, :], R=['mst'], W=[('yb', 8 + j)])
            for j in range(8):
                wt, wk = load_coltile(w_glu, 8, j * 128, c, wc_g[j])
                for i in range(8):
                    P.mm(pb[6][:, :N], wt[:, i, :], yb[:, 8 + i, :], start=(i == 0), stop=(i == 7),
                         R=[wk, ('yb', 8 + i)], W=[pk[6]])
                P.act(sg[j % 2][:, :], pb[6][:, :N], AF.Sigmoid, bias=bglu_sb[:, j:j + 1], R=[pk[6], 'bglu'], W=[('sg', j % 2)])
                P.tt('dve', yb[:, j, :], mst[:, j, :], sg[j % 2][:, :], ALU.mult, R=['mst', ('sg', j % 2)], W=[('yb', j)])
        else:
            for j in range(8):
                P.copy('act', yb[:, j, :], mst[:, j, :], R=['mst'], W=[('yb', j)])
        select_into_mst(c, 256)
        for j in range(8):
            P.copy('act', yb[:, 8 + j, :], mst[:, j, :], R=['mst'], W=[('yb', 8 + j)])
        for dt in range(16):
            wt, wk = load_coltile(w_out, 16, dt * 128, c, wc_o[dt])
            hr = hres[dt % 2]
            P.dma(hr[:], hT[dt * 128:(dt + 1) * 128, cs], W=[('hres', dt % 2)], q='pool')
            bank = 6 + dt % 2
            for i in range(16):
                P.mm(pb[bank][:, :N], wt[:, i, :], yb[:, i, :], start=(i == 0), stop=(i == 15),
                     R=[wk, ('yb', i)], W=[pk[bank]])
            P.stt('dve', v[:, dt, :], hr[:], DN_ALPHA, pb[bank][:, :N], ALU.mult, ALU.add,
                  R=[('hres', dt % 2), pk[bank]], W=[('v', dt)])
        layer_norm_fm(P, v, N, lnp_sb[:, 0:16], lnp_sb[:, 16:32], pb[6], pb[7], pk[6], pk[7], ones_f, tmp, hb=hb)
        t0 = 0
        while t0 < N:
            M = min(128, N - t0)
            for dt in range(16):
                P.mm(pb[6][:M, :36], v[:, dt, t0:t0 + M], wr_sb[:, dt, :], start=(dt == 0), stop=False,
                     R=[('v', dt), 'wr'], W=[pk[6]])
            P.mm(pb[6][:M, :36], ones_f[0:1, :M], br_sb[:, :], start=False, stop=True, R=['ones_f', 'br'], W=[pk[6]])
            r = {k: t[:M, :] for k, t in rs.items()}
            K = lambda *names: ['rs_' + n for n in names]
            P.copy('dve', r['L'], pb[6][:M, :36], R=[pk[6]], W=K('L'))
            P.op('dve', lambda e, r=r: e.reduce_max(out=r['gmax'], in_=r['L'][:, 0:4], axis=AX.X), R=K('L'), W=K('gmax'))
            P.ts('dve', r['ohg'], r['L'][:, 0:4], r['gmax'], ALU.is_equal, R=K('L', 'gmax'), W=K('ohg'))
            P.ts('dve', r['ngmax'], r['gmax'], -1.0, ALU.mult, R=K('gmax'), W=K('ngmax'))
            P.act(r['eg'], r['L'][:, 0:4], AF.Exp, bias=r['ngmax'], R=K('L', 'ngmax'), W=K('eg'))
            P.op('dve', lambda e, r=r: e.reduce_sum(out=r['se'], in_=r['eg'], axis=AX.X), R=K('eg'), W=K('se'))
            P.op('dve', lambda e, r=r: e.reciprocal(out=r['ptop'], in_=r['se']), R=K('se'), W=K('ptop'))
            P.ts('dve', r['lesel'], r['L'][:, 4:12], r['ohg'][:, 0:1], ALU.mult, R=K('L', 'ohg'), W=K('lesel'))
            for g in range(1, 4):
                P.stt('dve', r['lesel'], r['L'][:, 4 + 8 * g:12 + 8 * g], r['ohg'][:, g:g + 1], r['lesel'],
                      ALU.mult, ALU.add, R=K('L', 'ohg', 'lesel'), W=K('lesel'))
            P.op('dve', lambda e, r=r: e.reduce_max(out=r['m1'], in_=r['lesel'], axis=AX.X), R=K('lesel'), W=K('m1'))
            P.ts('dve', r['oh1'], r['lesel'], r['m1'], ALU.is_equal, R=K('lesel', 'm1'), W=K('oh1'))
            P.stt('dve', r['msk'], r['oh1'], -1e30, r['lesel'], ALU.mult, ALU.add, R=K('oh1', 'lesel'), W=K('msk'))
            P.op('dve', lambda e, r=r: e.reduce_max(out=r['m2'], in_=r['msk'], axis=AX.X), R=K('msk'), W=K('m2'))
            P.ts('dve', r['oh2'], r['msk'], r['m2'], ALU.is_equal, R=K('msk', 'm2'), W=K('oh2'))
            P.tt('dve', r['d'], r['m2'], r['m1'], ALU.subtract, R=K('m1', 'm2'), W=K('d'))
            P.act(r['ed'], r['d'], AF.Exp, R=K('d'), W=K('ed'))
            P.ts('dve', r['w1'], r['ed'], 1.0, ALU.add, R=K('ed'), W=K('w1'))
            P.op('dve', lambda e, r=r: e.reciprocal(out=r['w1'], in_=r['w1']), R=K('w1'), W=K('w1'))
            P.tt('dve', r['w2'], r['ed'], r['w1'], ALU.mult, R=K('ed', 'w1'), W=K('w2'))
            P.tt('dve', r['w1'], r['w1'], r['ptop'], ALU.mult, R=K('w1', 'ptop'), W=K('w1'))
            P.tt('dve', r['w2'], r['w2'], r['ptop'], ALU.mult, R=K('w2', 'ptop'), W=K('w2'))
            P.ts('dve', r['ce'], r['oh1'], r['w1'], ALU.mult, R=K('oh1', 'w1'), W=K('ce'))
            P.stt('dve', r['ce'], r['oh2'], r['w2'], r['ce'], ALU.mult, ALU.add, R=K('oh2', 'w2', 'ce'), W=K('ce'))
            for g in range(4):
                P.ts('dve', r['comb'][:, 8 * g:8 * g + 8], r['ce'], r['ohg'][:, g:g + 1], ALU.mult,
                     R=K('ce', 'ohg', 'comb'), W=K('comb'))
            P.transpose(pb[7][:32, :M], r['comb'], ident[:M, :M], R=K('comb') + ['ident'], W=[pk[7]])
            P.copy('dve', combT[:, t0:t0 + M], pb[7][:32, :M], R=[pk[7], 'combT'], W=['combT'])
            t0 += M
        wst_i = 0
        for e in range(NE):
            cbk = pk[4 + e % 2]; cbp = pb[4 + e % 2]
            P.dma(selb[e % 2][:], c_sel[:, e * 128:(e + 1) * 128], W=[('selb', e % 2)], q='pool')
            P.mm(cbp[:, :N], selb[e % 2][:], combT[:, :], R=[('selb', e % 2), 'combT'], W=[cbk])
            for f in range(2):
                bi = (2 * e + f) % 2
                wi = (2 * e + f) % NWB
                wbt = wb[wi]; wbk = ('wb', wi)
                cgu = wc_gu[2 * e + f].rearrange("p (t n) -> p t n", t=16)
                if c == 0:
                    for gi, wsrc in enumerate((w_gate, w_up)):
                        for q2 in range(2):
                            st = wst[wst_i % 3]; sk = ('wst', wst_i % 3); wst_i += 1
                            P.dma(st[:], wsrc[e, q2 * 1024:(q2 + 1) * 1024, f * 128:(f + 1) * 128].rearrange(
                                "(t p) n -> p t n", p=128), W=[sk], q='sp')
                            cast(wbt[:, q2 * 8:(q2 + 1) * 8, gi * 128:(gi + 1) * 128], st[:], R=[sk, wbk], W=[wbk])
                    P.dma(cgu, wbt[:], R=[wbk], W=[('wc_gu', 2 * e + f)], q='pool')
                else:
                    P.dma(wbt[:], cgu, R=[('wc_gu', 2 * e + f)], W=[wbk], q='sp')
                pg = pb[bi]; pu = pb[2 + bi]
                for dt in range(16):
                    P.mm(pg[:, :N], wbt[:, dt, 0:128], hb[:, dt, :], start=(dt == 0), stop=(dt == 15),
                         R=[wbk, ('hb', dt)], W=[pk[bi]])
                for dt in range(16):
                    P.mm(pu[:, :N], wbt[:, dt, 128:256], hb[:, dt, :], start=(dt == 0),
                         stop=(dt == 15), R=[wbk, ('hb', dt)], W=[pk[2 + bi]])
                P.act(sg[bi][:, :], pg[:, :N], AF.Silu, R=[pk[bi]], W=[('sg', bi)])
                P.tt('dve', tq[bi][:, :], pu[:, :N], sg[bi][:, :], ALU.mult, R=[pk[2 + bi], ('sg', bi)], W=[('tq', bi)])
                P.tt('dve', hh[:, 2 * e + f, :], tq[bi][:, :], cbp[:, :N], ALU.mult, R=[('tq', bi), cbk],
                     W=[('hh', 2 * e + f)])
        for half in range(2):
            for e in range(NE):
                for ft in range(2):
                    i = (2 * e + ft) % 2
                    ci = (half * NE + e) * 2 + ft
                    wi = (2 * e + ft) % NWD
                    if c == 0:
                        P.dma(wdst[i][:], w_down[e, ft * 128:(ft + 1) * 128, half * 1024:(half + 1) * 1024],
                              W=[('wdst', i)], q='sp')
                        cast(wd[wi][:, 0, :], wdst[i][:], R=[('wdst', i)], W=[('wd', wi)])
                        P.dma(wc_d[ci], wd[wi][:, 0, :], R=[('wd', wi)], W=[('wc_d', ci)], q='pool')
                    else:
                        P.dma(wd[wi][:, 0, :], wc_d[ci], R=[('wc_d', ci)], W=[('wd', wi)], q='sp')
                    for j in range(8):
                        P.mm(pb[j][:, :N], wd[wi][:, 0, j * 128:(j + 1) * 128], hh[:, 2 * e + ft, :],
                             start=(e == 0 and ft == 0), stop=(e == NE - 1 and ft == 1),
                             R=[('wd', wi), ('hh', 2 * e + ft)], W=[pk[j]])
            for j in range(8):
                dt = half * 8 + j
                P.stt('dve', v[:, dt, :], v[:, dt, :], DN_ALPHA, pb[j][:, :N], ALU.mult, ALU.add,
                      R=[('v', dt), pk[j]], W=[('v', dt)])
        layer_norm_fm(P, v, N, lnp_sb[:, 32:48], lnp_sb[:, 48:64], pb[6], pb[7], pk[6], pk[7], ones_f, tmp, hb=None)
        P.dma(outT[:, cs].rearrange("(t p) n -> p t n", p=128), v[:], R=[('v', dt) for dt in range(16)], W=[('out32', c)], q='pool')
        if outb is not None:
            for dt in range(16):
                P.copy('act', hb[:, dt, :], v[:, dt, :], R=[('v', dt)], W=[('hb', dt)])
            for half in range(2):
                P.dma(outb[c][half].rearrange("(t p) n -> p t n", p=128), hb[:, 8 * half:8 * half + 8, :],
                      R=[('hb', dt) for dt in range(16)], W=[('outb', c, half)], q='pool')
                P.allgather(outb_all[c][half], outb[c][half], groups, R=[('outb', c, half)], W=[('outb_all', c, half)])

import numpy as np
PAD = 496

def c_consts():
    U = np.triu(np.ones((128, 128), np.float32))
    return {"c_U": U, "c_ident": np.eye(128, dtype=np.float32)}

def prep_ab(inp, j):
    w = inp['ab_w_in'][0]
    cols = np.concatenate([np.arange(256 * j, 256 * j + 256), 1024 + np.arange(128 * j, 128 * j + 128),
                           1536 + np.arange(128 * j, 128 * j + 128), 2048 + np.arange(256 * j, 256 * j + 256),
                           3088 + np.arange(256 * j, 256 * j + 256), np.arange(3072, 3088)])
    d = {"w_in": np.ascontiguousarray(w[:, cols])}
    a_re = inp['ab_s5_a_re'][0]; a_im = inp['ab_s5_a_im'][0]; ldt = inp['ab_s5_log_dt'][0]
    B = [inp['ab_s5_b_re'][0], inp['ab_s5_b_im'][0]]; C = [inp['ab_s5_c_re'][0], inp['ab_s5_c_im'][0]]
    par = np.zeros((128, 8, 3), np.float32)
    BT = np.zeros((128, 2, 8, 128), np.float32); CT = np.zeros((128, 2, 8, 128), np.float32)
    for st in range(8):
        for g2 in range(2):
            g = 16 * j + 2 * st + g2
            gl = (2 * st + g2) % 8
            ps = slice(g2 * 64, g2 * 64 + 64)
            par[ps, st, 0] = a_re[g]; par[ps, st, 1] = a_im[g]; par[ps, st, 2] = ldt[g]
            for ri in range(2):
                BT[gl * 16:(gl + 1) * 16, ri, st, ps] = B[ri][g].T
                CT[ps, ri, st, gl * 16:(gl + 1) * 16] = C[ri][g].T
    d["s5par"] = par; d["s5BT"] = BT; d["s5CT"] = CT
    d["s5d"] = np.ascontiguousarray(inp['ab_s5_d'][0][256 * j:256 * j + 256].reshape(2, 128).T)
    d["gla_wg"] = np.ascontiguousarray(inp['ab_gla_w_gate'][0][:, 128 * j:128 * j + 128])
    d["gla_bg"] = np.ascontiguousarray(inp['ab_gla_b_gate'][0][128 * j:128 * j + 128, None])
    d["gla_ng"] = np.ascontiguousarray(np.broadcast_to(inp['ab_gla_norm'][0][None, :], (128, 256)))
    d.update(c_consts())
    return d

def prep_hT(h, Tp):
    L = h.shape[0]
    out = np.zeros((h.shape[1], Tp), np.float32)
    out[:, Tp - L:] = h.T
    return out

def prep_cd(inp, j):
    w = inp['cd_w_in'][0]
    hs = [2 * j, 2 * j + 1]
    blk = lambda base, i: base + np.arange(i * 128, i * 128 + 128)
    tiles = [blk(0, hs[0]), blk(0, hs[1]), blk(1024, hs[0]), blk(1024, hs[1]), blk(2048, hs[0]), blk(2048, hs[1]),
             blk(4112, hs[0]), blk(4112, hs[1]), blk(3072, hs[0]), blk(3072, hs[1]), blk(5136, hs[0]), blk(5136, hs[1]),
             np.array([4096 + hs[0], 4096 + hs[1], 4104 + hs[0], 4104 + hs[1]])]
    d = {"w_in": np.ascontiguousarray(w[:, np.concatenate(tiles)])}
    cw = np.zeros((128, 8, 4), np.float32); cb = np.zeros((128, 8), np.float32)
    for i in range(6):
        cw[:, i, :] = inp['cd_conv_w'][0][:, tiles[i]].T
    for b in range(2):
        cw[:, 6 + b, :] = inp['cd_lru_conv_w'][0][:, blk(0, hs[b])].T
        cb[:, 6 + b] = inp['cd_lru_conv_b'][0][blk(0, hs[b])]
    d["convw"] = cw; d["convb"] = cb
    gp = np.zeros((4, 4), np.float32)
    for h in range(2):
        gp[2 + h, 0] = inp['cd_gdn_dt_bias'][0][hs[h]]; gp[2 + h, 1] = inp['cd_gdn_a_log'][0][hs[h]]
    gp[0:2, 2] = 1.0; gp[2:4, 3] = 1.0
    d["gpar"] = gp
    d["gdn_ng"] = np.ascontiguousarray(np.broadcast_to(inp['cd_gdn_norm'][0][None, :], (128, 128)))
    d["lwa"] = np.ascontiguousarray(np.stack([inp['cd_lru_w_a'][0][hs[b]] for b in range(2)], 1))
    d["lwx"] = np.ascontiguousarray(np.stack([inp['cd_lru_w_x'][0][hs[b]] for b in range(2)], 1))
    lp = np.zeros((128, 2, 3), np.float32)
    for b in range(2):
        lp[:, b, 0] = inp['cd_lru_b_a'][0][blk(0, hs[b])]; lp[:, b, 1] = inp['cd_lru_b_x'][0][blk(0, hs[b])]
        lp[:, b, 2] = inp['cd_lru_lambda'][0][blk(0, hs[b])]
    d["lpar"] = lp
    d.update(c_consts())
    d["c_Us"] = np.triu(np.ones((128, 128), np.float32), 1)
    sel4 = np.zeros((4, 512), np.float32)
    for r in range(4):
        sel4[r, r * 128:(r + 1) * 128] = 1.0
    d["c_sel4"] = sel4
    return d


N_META = 16
SEQ = 8192
NCHUNK = 17
TP = NCHUNK * CH
POST_NCH, POST_N = 6, 342
T2 = POST_NCH * POST_N
G4 = [[0, 1, 2, 3], [4, 5, 6, 7]]


def build_fused():
    P = Prog()
    nc = P.nc
    pb = [P.ps("pb%d" % i, [128, 512]) for i in range(8)]
    pk = ['pb%d' % i for i in range(8)]
    hT = P.dram("hT", [D, TP]); hq = P.dram("hq", [D, T2]); esel = P.dram("esel", [128, 4])
    outT = P.dram("outT", [D, T2], kind="ExternalOutput")
    idram = lambda n, sh, dt: nc.dram_tensor(n, sh, dt, kind="Internal").ap()
    y0_loc = [idram("y0_loc%d" % c, [512, CH], BF16) for c in range(NCHUNK)]
    y0_all = [idram("y0_all%d" % c, [4 * 512, CH], BF16) for c in range(NCHUNK)]
    y1_loc = [idram("y1_loc%d" % c, [512, CH], BF16) for c in range(NCHUNK)]
    y1_all = [idram("y1_all%d" % c, [4 * 512, CH], BF16) for c in range(NCHUNK)]
    h1_32 = idram("h1_32", [D, T2], F32)
    h1_b = [[idram("h1_b%d_%d" % (c, h), [1024, POST_N], BF16) for h in range(2)] for c in range(POST_NCH)]
    h1_all = [[idram("h1_all%d_%d" % (c, h), [4 * 1024, POST_N], BF16) for h in range(2)] for c in range(POST_NCH)]

    with P.scope():
        phase_ab(P, pb, pk, NCHUNK, hT, y0_loc, y0_all, G4)
    P.new_phase()
    with P.scope():
        phase_post(P, pb, pk, True, "p0_", y0_all, hq, h1_32, h1_b, h1_all, G4, esel, POST_NCH, POST_N)
    P.new_phase()
    with P.scope():
        phase_cd(P, pb, pk, NCHUNK, h1_all, y1_loc, y1_all, G4)
    P.new_phase()
    with P.scope():
        phase_post(P, pb, pk, False, "p1_", y1_all, h1_32, outT, None, None, G4, esel, POST_NCH, POST_N)
    return P.finalize(), P


def _post_weights(inp, layer, glu, pre):
    pt = lambda a: np.ascontiguousarray(a.reshape(-1, 128).T)
    d = {"w_out": (inp['ab_w_out'][0] if glu else inp['cd_w_out'][0]),
         "lnp": np.concatenate([pt(inp['ln_mix_g'][layer]), pt(inp['ln_mix_b'][layer]),
                                pt(inp['ln_ffn_g'][layer]), pt(inp['ln_ffn_b'][layer])], 1),
         "w_r": np.ascontiguousarray(np.concatenate([inp['moe_w_router_g'][layer],
                                                     inp['moe_w_router_e'][layer].reshape(D, 32)], 1)),
         "b_r": np.concatenate([inp['moe_b_router_g'][layer], inp['moe_b_router_e'][layer].reshape(32)])[None],
         "w_gate": inp['moe_w_gate'][layer].reshape(32, D, FF), "w_up": inp['moe_w_up'][layer].reshape(32, D, FF),
         "w_down": inp['moe_w_down'][layer].reshape(32, FF, D),
         "c_ident": np.eye(128, dtype=np.float32),
         "c_sel": np.ascontiguousarray(np.repeat(np.eye(32, dtype=np.float32), 128, axis=1))}
    if glu:
        d["w_glu"] = inp['ab_s5_w_glu'][0]
        d["b_glu"] = pt(inp['ab_s5_b_glu'][0])
    return {pre + k: v for k, v in d.items()}


def kernel(**inputs):
    inp = {k: np.asarray(v, dtype=np.float32) for k, v in inputs.items()}
    x = inp['x']
    B = x.shape[0]
    L = N_META + SEQ
    h0 = np.concatenate([np.broadcast_to(inp['meta_tokens'][None], (B, N_META, D)), x], axis=1)
    hTs = [prep_hT(h0[b], TP) for b in range(B)]
    pw0 = _post_weights(inp, 0, True, "p0_"); pw1 = _post_weights(inp, 1, False, "p1_")
    ab = [{"a_" + k: v for k, v in prep_ab(inp, j).items()} for j in range(4)]
    cd = [{"c_" + k: v for k, v in prep_cd(inp, j).items()} for j in range(4)]
    maps = []
    for i in range(8):
        b, j = i // 4, i % 4
        d = {"hT": hTs[b], "hq": np.ascontiguousarray(h0[b, j * T2:(j + 1) * T2].T)}
        es = np.zeros((128, 4), np.float32); es[:, j] = 1.0
        d["esel"] = es
        d.update(ab[j]); d.update(cd[j]); d.update(pw0); d.update(pw1)
        maps.append(d)
    nc, _ = build_fused()
    res = run_bass_kernel_spmd(nc, maps, core_ids=list(range(8)))
    out = np.zeros((B, L, D), np.float32)
    for i in range(8):
        b, j = i // 4, i % 4
        out[b, j * T2:(j + 1) * T2] = np.asarray(res.results[i]["outT"]).T
    return np.ascontiguousarray(out[:, N_META:])
```
